# Optimizing a Trainium2 kernel written in Bass

```python
import math
import jax, jax.numpy as jnp
from jax import lax
import numpy as np

D_MODEL = 1024
BATCH = 8
SEQ = 4096
DEPTH = 1

HG_HEADS = 4
HG_DK = 128
HG_DV = 128
HG_KEY = HG_HEADS * HG_DK
HG_WIDTH = HG_HEADS * HG_DV
HG_CHUNK = 64
DA_HEADS = 4
DA_HD = 64
DA_DV = 2 * DA_HD
DA_QK = DA_HEADS * 2 * DA_HD
DA_WIDTH = DA_HEADS * DA_DV
Q_BLOCK = 128
N_EXPERTS = 32
TOP_K = 4
D_FF = 1024
SWIGLU_LIMIT = 7.0
SWIGLU_ALPHA = 1.702
DN_ALPHA = (2.0 * DEPTH) ** 0.25
DN_BETA = (8.0 * DEPTH) ** -0.25
LN_EPS = 1e-5
NORM_EPS = 1e-6
IN_SIZES = (HG_KEY, HG_KEY, HG_WIDTH, HG_WIDTH, DA_QK, DA_QK, DA_WIDTH, D_MODEL, D_MODEL)
D_IN = sum(IN_SIZES)

kernel_name = "hybrid_hgrn2_diffattn_moe_deepnorm"


def layer_norm(x, g, b):
    xf = x.astype(jnp.float32)
    mu = jnp.mean(xf, axis=-1, keepdims=True)
    xc = xf - mu
    var = jnp.mean(xc * xc, axis=-1, keepdims=True)
    y = xc * lax.rsqrt(var + LN_EPS) * g.astype(jnp.float32) + b.astype(jnp.float32)
    return y.astype(x.dtype)


def head_rms_norm(o, g):
    ms = jnp.mean(o * o, axis=-1, keepdims=True)
    return o * lax.rsqrt(ms + NORM_EPS) * g.astype(jnp.float32)


def alibi_slopes(n):
    return jnp.asarray([2.0 ** (-8.0 * (h + 1) / n) for h in range(n)], dtype=jnp.float32)


def hgrn2_branch(q, f_logit, i, g, lb, norm_g):
    B, S, _ = q.shape
    nc = S // HG_CHUNK
    f32 = jnp.float32
    f = lb + (1.0 - lb) * jax.nn.sigmoid(f_logit.astype(f32))
    log_f = jnp.log(f)
    k = 1.0 - f
    qa = jax.nn.silu(q.astype(f32))
    v = i.astype(f32)

    def to_chunks(t, d):
        return t.reshape(B, nc, HG_CHUNK, HG_HEADS, d).transpose(1, 0, 3, 2, 4)

    qc, kc, lc = to_chunks(qa, HG_DK), to_chunks(k, HG_DK), to_chunks(log_f, HG_DK)
    vc = to_chunks(v, HG_DV)
    causal = jnp.tril(jnp.ones((HG_CHUNK, HG_CHUNK), dtype=bool))[None, None, :, :, None]

    def step(state, inp):
        q_, k_, v_, l_ = inp
        bcum = jnp.cumsum(l_, axis=2)
        o_inter = jnp.einsum('bhtk,bhkv->bhtv', q_ * jnp.exp(bcum), state)
        diff = bcum[:, :, :, None, :] - bcum[:, :, None, :, :]
        decay = jnp.exp(jnp.where(causal, diff, -jnp.inf))
        attn = jnp.einsum('bhtk,bhsk,bhtsk->bhts', q_, k_, decay)
        o_intra = jnp.einsum('bhts,bhsv->bhtv', attn, v_)
        b_last = bcum[:, :, -1:, :]
        k_dec = k_ * jnp.exp(b_last - bcum)
        new_state = jnp.exp(b_last[:, :, 0, :])[..., None] * state + jnp.einsum('bhsk,bhsv->bhkv', k_dec, v_)
        return new_state, o_inter + o_intra

    state0 = jnp.zeros((B, HG_HEADS, HG_DK, HG_DV), f32)
    _, o = lax.scan(step, state0, (qc, kc, vc, lc))
    o = o.transpose(1, 0, 3, 2, 4).reshape(B, S, HG_HEADS, HG_DV)
    o = head_rms_norm(o, norm_g).reshape(B, S, HG_WIDTH)
    o = o * jax.nn.sigmoid(g.astype(f32))
    return o.astype(q.dtype)


def diff_attention_branch(q, k, v, lam_params, lambda_init, norm_g):
    B, S, _ = q.shape
    nb = S // Q_BLOCK
    f32 = jnp.float32
    qh = q.reshape(B, S, DA_HEADS, 2, DA_HD).transpose(0, 2, 3, 1, 4) * (DA_HD ** -0.5)
    kh = k.reshape(B, S, DA_HEADS, 2, DA_HD).transpose(0, 2, 3, 1, 4)
    vh = v.reshape(B, S, DA_HEADS, DA_DV).transpose(0, 2, 1, 3)
    lp = lam_params.astype(f32)
    lam = jnp.exp(jnp.sum(lp[0] * lp[1])) - jnp.exp(jnp.sum(lp[2] * lp[3])) + lambda_init
    slopes = alibi_slopes(DA_HEADS)
    kpos = jnp.arange(S)
    qb = qh.reshape(B, DA_HEADS, 2, nb, Q_BLOCK, DA_HD).transpose(3, 0, 1, 2, 4, 5)

    def block(args):
        qblk, bi = args
        qpos = bi * Q_BLOCK + jnp.arange(Q_BLOCK)
        s = jnp.einsum('bhjqd,bhjkd->bhjqk', qblk, kh).astype(f32)
        dist = qpos[:, None] - kpos[None, :]
        bias = -slopes[:, None, None] * dist.astype(f32)
        s = jnp.where(dist >= 0, s + bias[None, :, None], -jnp.inf)
        p = jax.nn.softmax(s, axis=-1)
        a = p[:, :, 0] - lam * p[:, :, 1]
        return jnp.einsum('bhqk,bhkv->bhqv', a.astype(vh.dtype), vh)

    o = lax.map(block, (qb, jnp.arange(nb)))
    o = o.transpose(1, 0, 3, 2, 4).reshape(B, S, DA_HEADS, DA_DV).astype(f32)
    o = head_rms_norm(o, norm_g) * (1.0 - lambda_init)
    return o.reshape(B, S, DA_WIDTH).astype(q.dtype)


def moe_ffn(x, router_w, router_b, w_gate_up, b_gate_up, w_down, b_down):
    B, S, D = x.shape
    xt = x.reshape(B * S, D)
    logits = (xt @ router_w + router_b).astype(jnp.float32)
    top_vals, top_idx = lax.top_k(logits, TOP_K)
    top_w = jax.nn.softmax(top_vals, axis=-1)
    combine = jnp.sum(jax.nn.one_hot(top_idx, N_EXPERTS, dtype=jnp.float32) * top_w[..., None], axis=1)

    def expert_step(acc, params):
        wgu, bgu, wd, bd, cw = params
        h = xt @ wgu + bgu
        gate = jnp.minimum(h[:, 0::2], SWIGLU_LIMIT)
        up = jnp.clip(h[:, 1::2], -SWIGLU_LIMIT, SWIGLU_LIMIT)
        glu = gate * jax.nn.sigmoid(gate * SWIGLU_ALPHA)
        y = ((up + 1.0) * glu) @ wd + bd
        return acc + cw[:, None] * y.astype(jnp.float32), None

    acc0 = jnp.zeros((B * S, D), jnp.float32)
    acc, _ = lax.scan(expert_step, acc0, (w_gate_up, b_gate_up, w_down, b_down, combine.T))
    return acc.reshape(B, S, D).astype(x.dtype)


def setup_inputs(seed: int = 0) -> dict:
    key = jax.random.key(seed)
    ks = jax.random.split(key, 20)
    n = jax.random.normal
    f32 = jnp.float32
    return {
        "x": n(ks[0], (BATCH, SEQ, D_MODEL), f32),
        "w_in": n(ks[1], (DEPTH, D_MODEL, D_IN), f32) * D_MODEL ** -0.5,
        "hg_lb_logits": n(ks[2], (DEPTH + 1, HG_KEY), f32) * 0.5,
        "hg_norm_g": 1.0 + 0.02 * n(ks[3], (DEPTH, HG_HEADS, HG_DV), f32),
        "da_lambda": 0.1 * n(ks[4], (DEPTH, 4, DA_HD), f32),
        "da_norm_g": 1.0 + 0.02 * n(ks[5], (DEPTH, DA_HEADS, DA_DV), f32),
        "w_branch_a": n(ks[6], (DEPTH, HG_WIDTH, D_MODEL), f32) * HG_WIDTH ** -0.5,
        "w_branch_b": n(ks[7], (DEPTH, DA_WIDTH, D_MODEL), f32) * DA_WIDTH ** -0.5,
        "w_out": n(ks[8], (DEPTH, D_MODEL, D_MODEL), f32) * (D_MODEL ** -0.5) * DN_BETA,
        "ln1_g": 1.0 + 0.02 * n(ks[9], (DEPTH, D_MODEL), f32),
        "ln1_b": 0.02 * n(ks[10], (DEPTH, D_MODEL), f32),
        "router_w": n(ks[11], (DEPTH, D_MODEL, N_EXPERTS), f32) * D_MODEL ** -0.5,
        "router_b": 0.01 * n(ks[12], (DEPTH, N_EXPERTS), f32),
        "w_gate_up": n(ks[13], (DEPTH, N_EXPERTS, D_MODEL, 2 * D_FF), f32) * D_MODEL ** -0.5,
        "b_gate_up": 0.01 * n(ks[14], (DEPTH, N_EXPERTS, 2 * D_FF), f32),
        "w_down": n(ks[15], (DEPTH, N_EXPERTS, D_FF, D_MODEL), f32) * (D_FF ** -0.5) * DN_BETA,
        "b_down": 0.01 * n(ks[16], (DEPTH, N_EXPERTS, D_MODEL), f32),
        "ln2_g": 1.0 + 0.02 * n(ks[17], (DEPTH, D_MODEL), f32),
        "ln2_b": 0.02 * n(ks[18], (DEPTH, D_MODEL), f32),
    }


def reference(x, w_in, hg_lb_logits, hg_norm_g, da_lambda, da_norm_g, w_branch_a, w_branch_b,
              w_out, ln1_g, ln1_b, router_w, router_b, w_gate_up, b_gate_up, w_down, b_down,
              ln2_g, ln2_b):
    split_points = [int(s) for s in np.cumsum(IN_SIZES)[:-1]]
    lb_all = jnp.cumsum(jax.nn.softmax(hg_lb_logits.astype(jnp.float32), axis=0), axis=0)
    h = x
    for l in range(DEPTH):
        lambda_init = 0.8 - 0.6 * math.exp(-0.3 * l)
        proj = h @ w_in[l]
        hq, hf, hi, hg, dq, dk, dv, ga, gb = jnp.split(proj, split_points, axis=-1)
        o_a = hgrn2_branch(hq, hf, hi, hg, lb_all[l], hg_norm_g[l])
        o_b = diff_attention_branch(dq, dk, dv, da_lambda[l], lambda_init, da_norm_g[l])
        merged = jax.nn.sigmoid(ga) * (o_a @ w_branch_a[l]) + jax.nn.sigmoid(gb) * (o_b @ w_branch_b[l])
        mix = merged @ w_out[l]
        h = layer_norm(DN_ALPHA * h + mix, ln1_g[l], ln1_b[l])
        ffn = moe_ffn(h, router_w[l], router_b[l], w_gate_up[l], b_gate_up[l], w_down[l], b_down[l])
        h = layer_norm(DN_ALPHA * h + ffn, ln2_g[l], ln2_b[l])
    return h
```

```python
import math
from contextlib import ExitStack

import numpy as np
import concourse.bass as bass
import concourse.mybir as mybir
from concourse.bass_utils import run_bass_kernel_spmd

F32 = mybir.dt.float32
BF16 = mybir.dt.bfloat16
I32 = mybir.dt.int32
AF = mybir.ActivationFunctionType
ALU = mybir.AluOpType

S_LEN = 4096
D = 1024
NT = S_LEN // 128
NE = 32
DFF = 1024
LN_EPS = 1e-5
NORM_EPS = 1e-6
DN_ALPHA = 2.0 ** 0.25
LAMBDA_INIT = 0.8 - 0.6 * math.exp(0.0)
SLOPES = [2.0 ** (-8.0 * (h + 1) / 4) for h in range(4)]
NPASS = 4
PASS_TOK = S_LEN // NPASS


class Sched:
    ENG = ('pe', 'act', 'dve', 'pool', 'sp')

    def __init__(self, nc):
        self.nc = nc
        self.prog = {k: [] for k in self.ENG}
        self.cnt = {}
        self.sems = {}
        self.seen = {k: {} for k in self.ENG}
        self.res = {}
        for k in self.ENG:
            self._sem(k)

    def _sem(self, key):
        if key not in self.sems:
            self.sems[key] = self.nc.alloc_semaphore(name="s%d" % len(self.sems))
            self.cnt[key] = 0
        return self.sems[key]

    def emit(self, eng, fns, reads=(), writes=(), dma=None, partial=False):
        if callable(fns):
            fns = [fns]
        deps = {}

        def add(d):
            for s, v in d.items():
                if deps.get(s, 0) < v:
                    deps[s] = v
        for r in reads:
            st = self.res.get(r)
            if st:
                add(st['w'])
        for w in writes:
            st = self.res.get(w)
            if st:
                add(st['w'])
                add(st['r'])
        if dma is not None:
            self._sem(dma)
            if self.cnt[dma] > 0:
                add({dma: self.cnt[dma]})
        P = self.prog[eng]
        for s, v in deps.items():
            if s == eng and eng == 'pe':
                continue
            if self.seen[eng].get(s, 0) >= v:
                continue
            self.seen[eng][s] = v
            sem = self.sems[s]
            P.append(lambda e, sem=sem, v=v: e.wait_ge(sem, v))
        skey, inc = (dma, 16) if dma is not None else (eng, 1)
        self.cnt[skey] += inc
        val = self.cnt[skey]
        sem = self.sems[skey]
        n = len(fns)
        for i, f in enumerate(fns):
            if i == n - 1:
                P.append(lambda e, f=f, sem=sem, inc=inc: f(e).then_inc(sem, inc))
            else:
                P.append(lambda e, f=f: f(e))
        for r in reads:
            st = self.res.setdefault(r, {'w': {}, 'r': {}})
            if st['r'].get(skey, 0) < val:
                st['r'][skey] = val
        for w in writes:
            if partial and w in self.res:
                self.res[w]['w'][skey] = val
            else:
                self.res[w] = {'w': {skey: val}, 'r': {}}

    def barrier(self):
        allev = dict(self.cnt)
        for eng in self.ENG:
            for s, v in allev.items():
                if v == 0 or (s == eng and eng == 'pe'):
                    continue
                if self.seen[eng].get(s, 0) >= v:
                    continue
                self.seen[eng][s] = v
                sem = self.sems[s]
                self.prog[eng].append(lambda e, sem=sem, v=v: e.wait_ge(sem, v))
        self.res = {}

    def run(self):
        nc = self.nc
        with nc.Block() as block:
            @block.tensor
            def _(e):
                for f in self.prog['pe']:
                    f(e)

            @block.scalar
            def _(e):
                for f in self.prog['act']:
                    f(e)

            @block.vector
            def _(e):
                for f in self.prog['dve']:
                    f(e)

            @block.gpsimd
            def _(e):
                for f in self.prog['pool']:
                    f(e)

            @block.sync
            def _(e):
                for f in self.prog['sp']:
                    f(e)


def bcast_ap(dram_ap, n, offset=0, parts=128):
    return bass.AP(dram_ap.tensor, dram_ap.offset + offset, [[0, parts], [1, n]])


def build(stage=99, dbg=False, hstop=99, ne_decl=NE, heads=4, cut=99):
    nc = bass.Bass("TRN2", target_bir_lowering=False)

    def din(name, shape):
        return nc.dram_tensor(name, list(shape), F32, kind="ExternalInput").ap()

    x = din("x", [S_LEN, D])
    w_in = din("w_in", [D, 5632])
    hg_lb = din("hg_lb_logits", [2, 512])
    hg_ng = din("hg_norm_g", [4, 128])
    da_lam = din("da_lambda", [4, 64])
    da_ng = din("da_norm_g", [4, 128])
    w_a = din("w_branch_a", [512, D])
    w_b = din("w_branch_b", [512, D])
    w_out = din("w_out", [D, D])
    ln1_g = din("ln1_g", [D])
    ln1_b = din("ln1_b", [D])
    router_w = din("router_w", [D, NE])
    router_b = din("router_b", [NE])
    w_gu = din("w_gate_up", [ne_decl, D, 2 * DFF])
    b_gu = din("b_gate_up", [NE, 2 * DFF])
    w_dn = din("w_down", [ne_decl, DFF, D])
    b_dn = din("b_down", [NE, D])
    ln2_g = din("ln2_g", [D])
    ln2_b = din("ln2_b", [D])
    y = nc.dram_tensor("y", [S_LEN, D], F32, kind="ExternalOutput").ap()

    xT_d = nc.dram_tensor("xT_d", [8, 128, S_LEN], BF16, kind="Internal").ap()
    sg_d = [nc.dram_tensor("sg%d_d" % i, [8, 128, S_LEN], BF16, kind="Internal").ap() for i in range(2)]
    h1_d = nc.dram_tensor("h1_d", [S_LEN, D], F32, kind="Internal").ap()
    h1T_d = nc.dram_tensor("h1T_d", [8, 128, S_LEN], BF16, kind="Internal").ap()
    dbg_t = {}
    if dbg:
        dbg_t['oaT'] = nc.dram_tensor("dbg_oaT", [128, 4, S_LEN], BF16, kind="ExternalOutput").ap()
        dbg_t['obT'] = nc.dram_tensor("dbg_obT", [128, 4, S_LEN], BF16, kind="ExternalOutput").ap()
        dbg_t['h1'] = nc.dram_tensor("dbg_h1", [S_LEN, D], F32, kind="ExternalOutput").ap()
        dbg_t['comb'] = nc.dram_tensor("dbg_comb", [128, NT, NE], F32, kind="ExternalOutput").ap()

    S = Sched(nc)
    _uid = [0]

    def uq(name):
        _uid[0] += 1
        return "%s_u%d" % (name, _uid[0])
    with ExitStack() as G:
        def sbg(name, shape, dt):
            return G.enter_context(nc.sbuf_tensor(uq(name), list(shape), dt))
        PS = [G.enter_context(nc.psum_tensor("ps%d" % i, [128, 512], F32)) for i in range(8)]

        def psb(i):
            return PS[i][:].bitcast(BF16)

        identf = sbg("identf", [128, 128], F32)
        ident = sbg("ident", [128, 128], BF16)
        tri = sbg("tri", [128, 128], BF16)
        tribd = sbg("tribd", [128, 128], BF16)
        onesf = sbg("onesf", [128, 128], F32)
        scanmask = sbg("scanmask", [128, 512], F32)
        btab_i = sbg("btab_i", [128, 35], I32)
        btab = sbg("btab", [128, 4, 35], F32)
        comb = sbg("comb", [128, NT, NE], F32)
        MIX = ExitStack()

        def sbm(name, shape, dt):
            return MIX.enter_context(nc.sbuf_tensor(uq(name), list(shape), dt))
        o_aT = sbm("o_aT", [128, 4, S_LEN], BF16)
        o_bT = sbm("o_bT", [128, 4, S_LEN], BF16)
        wst = [sbm("wst%d" % i, [128, 2048], F32) for i in range(2)]

        S.emit('pool', lambda e: e.memset(identf[:], 0.0), writes=['identf'])
        S.emit('pool', lambda e: e.affine_select(out=identf[:], in_=identf[:], pattern=[[-1, 128]],
                                                  compare_op=ALU.not_equal, fill=1.0, base=0,
                                                  channel_multiplier=1),
               reads=['identf'], writes=['identf'])
        S.emit('dve', lambda e: e.tensor_copy(out=ident[:], in_=identf[:]), reads=['identf'], writes=['ident'])
        S.emit('pool', lambda e: e.memset(onesf[:], 1.0), writes=['onesf'])
        S.emit('pool', lambda e: e.affine_select(out=tri[:], in_=onesf[:], pattern=[[1, 128]],
                                                  compare_op=ALU.is_ge, fill=0.0, base=0,
                                                  channel_multiplier=-1),
               reads=['onesf'], writes=['tri'])
        S.emit('pool', lambda e: e.tensor_copy(out=tribd[:], in_=tri[:]), reads=['tri'], writes=['tribd'])
        S.emit('pool', lambda e: e.memset(tribd[0:64, 64:128], 0.0), reads=['tribd'], writes=['tribd'])
        S.emit('pool', lambda e: e.memset(scanmask[:], 1.0), writes=['scanmask'])
        S.emit('pool', lambda e: e.memset(scanmask[:].rearrange("p (c t) -> p c t", t=64)[:, :, 0:1], 0.0),
               reads=['scanmask'], writes=['scanmask'])
        S.emit('pool', lambda e: e.iota(btab_i[:], pattern=[[-128, 35]], base=384, channel_multiplier=1),
               writes=['btab_i'])
        for h in range(4):
            S.emit('dve', lambda e, h=h: e.tensor_scalar(out=btab[:, h, :], in0=btab_i[:], scalar1=float(SLOPES[h]),
                                                        scalar2=None, op0=ALU.mult),
                   reads=['btab_i'], writes=[('btab', h)])

        bank_rr = [0]

        def load_cast(dst_ap, src_ap, shape, key, cast_eng='pool'):
            a, b = shape
            step = max(1, 2048 // b)
            for a0 in range(0, a, step):
                a1 = min(a, a0 + step)
                i = bank_rr[0] % 2
                bank_rr[0] += 1
                st = wst[i][:, 0:(a1 - a0) * b].rearrange("p (a b) -> p a b", b=b)
                S.emit('sp', lambda e, st=st, a0=a0, a1=a1: e.dma_start(out=st, in_=src_ap[:, a0:a1, :]),
                       writes=[('wst', i)], dma=('dwst', i))
                if cast_eng == 'act':
                    S.emit('act', lambda e, st=st, a0=a0, a1=a1: e.copy(out=dst_ap[:, a0:a1, :], in_=st),
                           reads=[('wst', i)], writes=[key], partial=True)
                else:
                    S.emit(cast_eng, lambda e, st=st, a0=a0, a1=a1: e.tensor_copy(out=dst_ap[:, a0:a1, :], in_=st),
                           reads=[('wst', i)], writes=[key], partial=True)

        with ExitStack() as P:
            def sb(name, shape, dt):
                return P.enter_context(nc.sbuf_tensor(uq(name), list(shape), dt))
            xs = [sb("xs%d" % i, [128, D], F32) for i in range(2)]
            xb = [sb("xb%d" % i, [128, D], BF16) for i in range(2)]
            xTs = [sb("xTs%d" % i, [128, 8, 512], BF16) for i in range(2)]
            for t in range(NT):
                i = t % 2
                S.emit('sp', lambda e, t=t, i=i: e.dma_start(out=xs[i][:], in_=x[t * 128:(t + 1) * 128, :]),
                       writes=[('xs', i)], dma=('dxs', i))
                if i == 0:
                    S.emit('dve', lambda e, i=i: e.tensor_copy(out=xb[i][:], in_=xs[i][:]), reads=[('xs', i)], writes=[('xb', i)])
                else:
                    S.emit('act', lambda e, i=i: e.copy(out=xb[i][:], in_=xs[i][:]), reads=[('xs', i)], writes=[('xb', i)])
                bk = t % 2
                pv = psb(bk).rearrange("p (c n) -> p c n", n=128)
                S.emit('pe', [lambda e, c=c, i=i, pv=pv: e.transpose(out=pv[:, c, :], in_=xb[i][:, c * 128:(c + 1) * 128],
                                                                     identity=ident[:]) for c in range(8)],
                       reads=[('xb', i), 'ident'], writes=[('ps', bk)])
                g4 = (t // 4) % 2
                j = t % 4
                ce = 'dve' if i == 1 else 'act'
                if ce == 'dve':
                    S.emit('dve', lambda e, g4=g4, j=j, pv=pv: e.tensor_copy(out=xTs[g4][:, :, j * 128:(j + 1) * 128], in_=pv),
                           reads=[('ps', bk)], writes=[('xTs', g4)], partial=(j > 0))
                else:
                    S.emit('act', lambda e, g4=g4, j=j, pv=pv: e.copy(out=xTs[g4][:, :, j * 128:(j + 1) * 128], in_=pv),
                           reads=[('ps', bk)], writes=[('xTs', g4)], partial=(j > 0))
                if j == 3:
                    tt = t // 4
                    S.emit('sp', lambda e, g4=g4, tt=tt: e.dma_start(
                        out=xT_d[:, :, tt * 512:(tt + 1) * 512].rearrange("c p n -> p c n"), in_=xTs[g4][:]),
                        reads=[('xTs', g4)], dma=('dxT', g4))
            S.barrier()

        def make_xT_loader(P):
            bufs = [P.enter_context(nc.sbuf_tensor(uq("xTt%d" % i), [128, 8, 512], BF16)) for i in range(2)]
            cnt = [0]

            def load(tt):
                i = cnt[0] % 2
                cnt[0] += 1
                S.emit('sp', lambda e: e.dma_start(out=bufs[i][:],
                                                   in_=xT_d[:, :, tt * 512:(tt + 1) * 512].rearrange("c p n -> p c n")),
                       writes=[('xTt', i)], dma=('dxTt', i))
                return bufs[i], ('xTt', i)
            return load

        with ExitStack() as P:
            def sb(name, shape, dt):
                return P.enter_context(nc.sbuf_tensor(uq(name), list(shape), dt))
            wg = sb("wg", [128, 8, 2048], BF16)
            for q4 in range(4):
                load_cast(wg[:, :, q4 * 512:(q4 + 1) * 512],
                          w_in[:, 3584 + q4 * 512:3584 + (q4 + 1) * 512].rearrange("(c p) n -> p c n", p=128),
                          (8, 512), ('wg', q4))
            sgst = [sb("sgst%d" % i, [128, 8, 512], BF16) for i in range(2)]
            loadx = make_xT_loader(P)
            bk = 0
            for tt in range(8):
                xt, xk = loadx(tt)
                for gi in range(2):
                    for fc in range(8):
                        b = bk % 4
                        bk += 1
                        c0 = gi * 1024 + fc * 128
                        S.emit('pe', [lambda e, kc=kc, c0=c0, b=b, xt=xt: e.matmul(
                            PS[b][:], lhsT=wg[:, kc, c0:c0 + 128], rhs=xt[:, kc, :], start=(kc == 0), stop=(kc == 7))
                            for kc in range(8)],
                            reads=[xk, ('wg', c0 // 512)], writes=[('ps', b)])
                        S.emit('act', lambda e, gi=gi, fc=fc, b=b: e.activation(out=sgst[gi][:, fc, :], in_=PS[b][:],
                                                                               func=AF.Sigmoid),
                               reads=[('ps', b)], writes=[('sgst', gi)], partial=(fc > 0))
                    S.emit('sp', lambda e, gi=gi, tt=tt: e.dma_start(
                        out=sg_d[gi][:, :, tt * 512:(tt + 1) * 512].rearrange("c p n -> p c n"), in_=sgst[gi][:]),
                        reads=[('sgst', gi)], dma=('dsg', gi))
            S.barrier()

        def phase_H():
          with ExitStack() as P:
            def sb(name, shape, dt):
                return P.enter_context(nc.sbuf_tensor(uq(name), list(shape), dt))
            wq = sb("wq", [128, 8, 128], BF16)
            wf = sb("wf", [128, 8, 128], BF16)
            wig = sb("wig", [128, 8, 256], BF16)
            qeT = sb("qeT", [128, S_LEN], BF16)
            kdT = sb("kdT", [128, S_LEN], BF16)
            kd_tok = sb("kd_tok", [128, NT, 128], BF16)
            v_tok = sb("v_tok", [128, NT, 128], BF16)
            sgn = sb("sgn", [128, NT, 128], BF16)
            ebl = sb("ebl", [128, 64], F32)
            lbT8 = sb("lbT8", [8, 128], F32)
            lbp = sb("lbp", [128, 8], F32)
            lbv = sb("lbv", [128, 4], F32)
            omlb = sb("omlb", [128, 4], F32)
            ngA = sb("ngA", [128, 4, 128], F32)
            q32 = [sb("q32_%d" % i, [128, 512], F32) for i in range(2)]
            sg32 = [sb("sg32_%d" % i, [128, 512], F32) for i in range(2)]
            lf32 = [sb("lf32_%d" % i, [128, 512], F32) for i in range(2)]
            kk32 = [sb("kk32_%d" % i, [128, 512], F32) for i in range(2)]
            bc32 = [sb("bc32_%d" % i, [128, 512], F32) for i in range(2)]
            eb32 = [sb("eb32_%d" % i, [128, 512], F32) for i in range(2)]
            en32 = [sb("en32_%d" % i, [128, 512], F32) for i in range(2)]
            sgt = [sb("sgt%d" % i, [128, 128], F32) for i in range(2)]
            st32 = sb("st32", [128, 128], F32)
            tmp32 = sb("tmp32", [128, 128], F32)
            stbf = [sb("stbf%d" % i, [128, 128], BF16) for i in range(2)]
            attm = [sb("attm%d" % i, [128, 128], BF16) for i in range(2)]
            junk = sb("junk", [128, 128], BF16)
            ssA = sb("ssA", [128, 2], F32)
            rsA = sb("rsA", [128, 2], F32)
            oab = [sb("oab%d" % i, [128, 128], BF16) for i in range(2)]
            loadx = make_xT_loader(P)

            S.emit('sp', lambda e: e.dma_start(out=lbT8[:], in_=hg_lb.rearrange("r (h p) -> (r h) p", p=128)),
                   writes=['lbT8'], dma='dsmall0')
            S.emit('pe', lambda e: e.transpose(out=PS[7][:, 0:8], in_=lbT8[:], identity=identf[0:8, 0:8]),
                   reads=['lbT8', 'identf'], writes=[('ps', 7)])
            S.emit('dve', lambda e: e.tensor_copy(out=lbp[:], in_=PS[7][:, 0:8]), writes=['lbp', ('ps', 7)])
            S.emit('dve', lambda e: e.tensor_tensor(out=lbv[:], in0=lbp[:, 0:4], in1=lbp[:, 4:8], op=ALU.subtract),
                   reads=['lbp'], writes=['lbv'])
            S.emit('act', lambda e: e.activation(out=lbv[:], in_=lbv[:], func=AF.Sigmoid), reads=['lbv'], writes=['lbv'])
            S.emit('dve', lambda e: e.tensor_scalar(out=omlb[:], in0=lbv[:], scalar1=-1.0, scalar2=1.0, op0=ALU.mult,
                                                    op1=ALU.add), reads=['lbv'], writes=['omlb'])
            S.emit('sp', lambda e: e.dma_start(out=ngA[:].rearrange("p h n -> p (h n)"), in_=bcast_ap(hg_ng, 512)),
                   writes=['ngA'], dma='dsmall1')

            for h in range(heads if hstop >= 2 else 0):
                load_cast(wq[:], w_in[:, h * 128:(h + 1) * 128].rearrange("(c p) n -> p c n", p=128), (8, 128), 'wq')
                load_cast(wf[:], w_in[:, 512 + h * 128:512 + (h + 1) * 128].rearrange("(c p) n -> p c n", p=128),
                          (8, 128), 'wf')
                load_cast(wig[:, :, 0:128], w_in[:, 1024 + h * 128:1024 + (h + 1) * 128].rearrange("(c p) n -> p c n", p=128),
                          (8, 128), 'wig')
                load_cast(wig[:, :, 128:256], w_in[:, 1536 + h * 128:1536 + (h + 1) * 128].rearrange("(c p) n -> p c n", p=128),
                          (8, 128), 'wig')
                for tt in range(8):
                    xt, xk = loadx(tt)
                    i = tt % 2
                    tsl = slice(tt * 512, (tt + 1) * 512)
                    S.emit('pe', [lambda e, kc=kc, xt=xt: e.matmul(PS[0][:], lhsT=wq[:, kc, :], rhs=xt[:, kc, :],
                                                                  start=(kc == 0), stop=(kc == 7)) for kc in range(8)],
                           reads=[xk, 'wq'], writes=[('ps', 0)])
                    S.emit('pe', [lambda e, kc=kc, xt=xt: e.matmul(PS[1][:], lhsT=wf[:, kc, :], rhs=xt[:, kc, :],
                                                                  start=(kc == 0), stop=(kc == 7)) for kc in range(8)],
                           reads=[xk, 'wf'], writes=[('ps', 1)])
                    S.emit('act', lambda e, i=i: e.activation(out=q32[i][:], in_=PS[0][:], func=AF.Silu),
                           reads=[('ps', 0)], writes=[('q32', i)])
                    S.emit('act', lambda e, i=i: e.activation(out=sg32[i][:], in_=PS[1][:], func=AF.Sigmoid),
                           reads=[('ps', 1)], writes=[('sg32', i)])
                    if cut < 2:
                        continue
                    S.emit('dve', lambda e, i=i, h=h: e.tensor_scalar(out=sg32[i][:], in0=sg32[i][:],
                                                                      scalar1=omlb[:, h:h + 1], scalar2=lbv[:, h:h + 1],
                                                                      op0=ALU.mult, op1=ALU.add),
                           reads=[('sg32', i), 'omlb', 'lbv'], writes=[('sg32', i)])
                    S.emit('act', lambda e, i=i: e.activation(out=lf32[i][:], in_=sg32[i][:], func=AF.Ln),
                           reads=[('sg32', i)], writes=[('lf32', i)])
                    S.emit('pool', lambda e, i=i: e.tensor_scalar(out=kk32[i][:], in0=sg32[i][:], scalar1=-1.0, scalar2=1.0,
                                                                  op0=ALU.mult, op1=ALU.add),
                           reads=[('sg32', i)], writes=[('kk32', i)])
                    if cut < 3:
                        continue
                    S.emit('dve', lambda e, i=i: e.tensor_tensor_scan(out=bc32[i][:], data0=scanmask[:], data1=lf32[i][:],
                                                                      initial=0.0, op0=ALU.mult, op1=ALU.add),
                           reads=[('lf32', i), 'scanmask'], writes=[('bc32', i)])
                    S.emit('act', lambda e, i=i: e.activation(out=eb32[i][:], in_=bc32[i][:], func=AF.Exp),
                           reads=[('bc32', i)], writes=[('eb32', i)])
                    S.emit('act', lambda e, i=i: e.activation(out=en32[i][:], in_=bc32[i][:], func=AF.Exp, scale=-1.0),
                           reads=[('bc32', i)], writes=[('en32', i)])
                    if cut < 4:
                        continue
                    S.emit('dve', lambda e, i=i, tsl=tsl: e.tensor_tensor(out=qeT[:, tsl], in0=q32[i][:], in1=eb32[i][:],
                                                                          op=ALU.mult),
                           reads=[('q32', i), ('eb32', i)], writes=[('qeT', tt)])
                    S.emit('pool', lambda e, i=i, tsl=tsl: e.tensor_tensor(out=kdT[:, tsl], in0=kk32[i][:], in1=en32[i][:],
                                                                           op=ALU.mult),
                           reads=[('kk32', i), ('en32', i)], writes=[('kdT', tt)])
                    S.emit('pool', lambda e, i=i, tt=tt: e.tensor_copy(
                        out=ebl[:, tt * 8:(tt + 1) * 8],
                        in_=eb32[i][:].rearrange("p (c t) -> p c t", t=64)[:, :, 63]),
                        reads=[('eb32', i)], writes=[('ebl', tt)])
                    if cut < 5:
                        continue
                    for s4 in range(4):
                        p = tt * 4 + s4
                        b = 2 + (p % 2)
                        S.emit('pe', [lambda e, kc=kc, xt=xt, s4=s4, b=b: e.matmul(
                            PS[b][:, 0:256], lhsT=xt[:, kc, s4 * 128:(s4 + 1) * 128], rhs=wig[:, kc, :],
                            start=(kc == 0), stop=(kc == 7)) for kc in range(8)],
                            reads=[xk, 'wig'], writes=[('ps', b)])
                        S.emit('dve', lambda e, p=p, b=b: e.tensor_copy(out=v_tok[:, p, :], in_=PS[b][:, 0:128]),
                               writes=[('v_tok', p), ('ps', b)])
                        j = p % 2
                        S.emit('act', lambda e, j=j, b=b: e.activation(out=sgt[j][:], in_=PS[b][:, 128:256], func=AF.Sigmoid),
                               writes=[('sgt', j), ('ps', b)])
                        S.emit('pool', lambda e, j=j, p=p, h=h: e.tensor_tensor(out=sgn[:, p, :], in0=sgt[j][:],
                                                                                in1=ngA[:, h, :], op=ALU.mult),
                               reads=[('sgt', j), 'ngA'], writes=[('sgn', p)])
                    if cut < 6:
                        continue
                    pv = psb(4 + i).rearrange("p (c n) -> p c n", n=128)
                    S.emit('pe', [lambda e, s4=s4, tt=tt, pv=pv: e.transpose(
                        out=pv[:, s4, :], in_=kdT[:, tt * 512 + s4 * 128: tt * 512 + (s4 + 1) * 128], identity=ident[:])
                        for s4 in range(4)],
                        reads=[('kdT', tt), 'ident'], writes=[('ps', 4 + i)])
                    S.emit('dve', lambda e, tt=tt, pv=pv: e.tensor_copy(out=kd_tok[:, tt * 4:(tt + 1) * 4, :], in_=pv[:, 0:4, :]),
                           reads=[('ps', 4 + i)], writes=[('kd_tok', tt)])
                S.emit('dve', lambda e: e.memset(st32[:], 0.0), writes=['st32'])
                S.emit('dve', lambda e: e.memset(stbf[0][:], 0.0), writes=[('stbf', 0)])
                for p in range(NT if hstop >= 3 else 0):
                    tt = p // 4
                    psl = slice(p * 128, (p + 1) * 128)
                    ba = 6 + (p % 2)
                    bo = p % 2
                    A_ps = PS[ba][:, 0:128]
                    O_ps = PS[bo][:, 0:128]
                    S.emit('pe', lambda e, psl=psl, A_ps=A_ps: e.matmul(A_ps, lhsT=kdT[:, psl], rhs=qeT[:, psl],
                                                                       start=True, stop=True),
                           reads=[('kdT', tt), ('qeT', tt)], writes=[('ps', ba)])
                    am = attm[p % 2]
                    S.emit('dve', lambda e, am=am, A_ps=A_ps: e.tensor_tensor(out=am[:], in0=A_ps, in1=tribd[:], op=ALU.mult),
                           reads=['tribd'], writes=[('attm', p % 2), ('ps', ba)])
                    for hf in range(2):
                        c = 2 * p + hf
                        csl = slice(c * 64, (c + 1) * 64)
                        hs = slice(hf * 64, (hf + 1) * 64)
                        sb_cur = stbf[c % 2]
                        sb_nxt = stbf[(c + 1) % 2]
                        S.emit('pe', lambda e, csl=csl, hs=hs, sb_cur=sb_cur, bo=bo: e.matmul(
                            PS[bo][hs, 0:128], lhsT=qeT[:, csl], rhs=sb_cur[:], start=True, stop=False),
                            reads=[('qeT', tt), ('stbf', c % 2)], writes=[('ps', bo)])
                        ub = 4 + (c % 2)
                        S.emit('pe', lambda e, hs=hs, p=p, ub=ub: e.matmul(
                            PS[ub][:, 0:128], lhsT=kd_tok[hs, p, :], rhs=v_tok[hs, p, :], start=True, stop=True),
                            reads=[('kd_tok', tt), ('v_tok', p)], writes=[('ps', ub)])
                        S.emit('dve', lambda e, ub=ub: e.tensor_tensor(out=tmp32[:], in0=PS[ub][:, 0:128], in1=st32[:], op=ALU.add),
                               reads=['st32'], writes=['tmp32', ('ps', ub)])
                        S.emit('dve', lambda e, c=c: e.tensor_scalar(out=st32[:], in0=tmp32[:], scalar1=ebl[:, c:c + 1],
                                                                     scalar2=None, op0=ALU.mult),
                               reads=['tmp32', ('ebl', c // 8)], writes=['st32'])
                        S.emit('act', lambda e, c=c, sb_nxt=sb_nxt: e.activation(out=sb_nxt[:], in_=tmp32[:], func=AF.Copy,
                                                                               scale=ebl[:, c:c + 1]),
                               reads=['tmp32', ('ebl', c // 8)], writes=[('stbf', (c + 1) % 2)])
                    S.emit('pe', lambda e, am=am, p=p, O_ps=O_ps: e.matmul(O_ps, lhsT=am[:], rhs=v_tok[:, p, :],
                                                                         start=False, stop=True),
                           reads=[('attm', p % 2), ('v_tok', p)], writes=[('ps', bo)])
                    j = p % 2
                    S.emit('act', lambda e, O_ps=O_ps, j=j: e.activation(out=junk[:], in_=O_ps, func=AF.Square,
                                                                       accum_out=ssA[:, j:j + 1]),
                           writes=[('ssA', j), 'junk', ('ps', bo)])
                    S.emit('dve', lambda e, j=j: e.tensor_scalar(out=rsA[:, j:j + 1], in0=ssA[:, j:j + 1], scalar1=1.0 / 128,
                                                                 scalar2=NORM_EPS, op0=ALU.mult, op1=ALU.add),
                           reads=[('ssA', j)], writes=[('rsA', j)])
                    S.emit('act', lambda e, j=j: e.activation(out=rsA[:, j:j + 1], in_=rsA[:, j:j + 1], func=AF.Sqrt),
                           reads=[('rsA', j)], writes=[('rsA', j)])
                    S.emit('dve', lambda e, j=j: e.reciprocal(out=rsA[:, j:j + 1], in_=rsA[:, j:j + 1]),
                           reads=[('rsA', j)], writes=[('rsA', j)])
                    S.emit('dve', lambda e, j=j, p=p, O_ps=O_ps: e.scalar_tensor_tensor(
                        out=oab[j][:], in0=O_ps, scalar=rsA[:, j:j + 1], in1=sgn[:, p, :], op0=ALU.mult, op1=ALU.mult),
                        reads=[('rsA', j), ('sgn', p)], writes=[('oab', j), ('ps', bo)])
                    tb = 2 + j
                    tv = psb(tb)[:, 0:128]
                    S.emit('pe', lambda e, j=j, tv=tv: e.transpose(out=tv, in_=oab[j][:], identity=ident[:]),
                           reads=[('oab', j), 'ident'], writes=[('ps', tb)])
                    S.emit('act', lambda e, h=h, psl=psl, tv=tv: e.copy(out=o_aT[:, h, psl], in_=tv),
                           writes=[('o_aT', h, p), ('ps', tb)])
            S.barrier()
            if dbg:
                S.emit('sp', lambda e: e.dma_start(out=dbg_t['oaT'], in_=o_aT[:]), dma='ddbg0')

        if stage >= 1:
            phase_H()

        def phase_D():
          with ExitStack() as P:
            def sb(name, shape, dt):
                return P.enter_context(nc.sbuf_tensor(uq(name), list(shape), dt))
            wdq = sb("wdq", [128, 8, 128], BF16)
            wdk = sb("wdk", [128, 8, 128], BF16)
            wdv = sb("wdv", [128, 8, 128], BF16)
            dqT = sb("dqT", [128, S_LEN], BF16)
            dkT = sb("dkT", [128, S_LEN], BF16)
            dv_ext = sb("dv_ext", [128, NT, 129], BF16)
            ebuf = [sb("ebuf%d" % i, [128, 2, 512], BF16) for i in range(3)]
            lamt = sb("lamt", [128, 256], F32)
            lpr = sb("lpr", [128, 128], F32)
            lsm = sb("lsm", [128, 2], F32)
            neglam = sb("neglam", [128, 1], F32)
            ngB = sb("ngB", [128, 4, 128], F32)
            rz = [sb("rz%d" % i, [128, 2], F32) for i in range(2)]
            t1 = [sb("t1_%d" % i, [128, 128], F32) for i in range(2)]
            o32 = [sb("o32_%d" % i, [128, 128], F32) for i in range(2)]
            junk = sb("junkd", [128, 128], BF16)
            ssB = sb("ssB", [128, 2], F32)
            rsB = sb("rsB", [128, 2], F32)
            obb = [sb("obb%d" % i, [128, 128], BF16) for i in range(2)]
            loadx = make_xT_loader(P)

            S.emit('sp', lambda e: e.dma_start(out=lamt[:], in_=bcast_ap(da_lam, 256)), writes=['lamt'], dma='dsmall0')
            S.emit('sp', lambda e: e.dma_start(out=ngB[:].rearrange("p h n -> p (h n)"), in_=bcast_ap(da_ng, 512)),
                   writes=['ngB'], dma='dsmall1')
            S.emit('dve', lambda e: e.tensor_scalar(out=ngB[:], in0=ngB[:], scalar1=float(1.0 - LAMBDA_INIT), scalar2=None,
                                                    op0=ALU.mult), reads=['ngB'], writes=['ngB'])
            lv = lamt[:].rearrange("p (a n) -> p a n", n=64)
            S.emit('dve', lambda e: e.tensor_tensor(out=lpr[:, 0:64], in0=lv[:, 0, :], in1=lv[:, 1, :], op=ALU.mult),
                   reads=['lamt'], writes=['lpr'])
            S.emit('dve', lambda e: e.tensor_tensor(out=lpr[:, 64:128], in0=lv[:, 2, :], in1=lv[:, 3, :], op=ALU.mult),
                   reads=['lamt', 'lpr'], writes=['lpr'])
            S.emit('dve', lambda e: e.tensor_reduce(out=lsm[:], in_=lpr[:].rearrange("p (a n) -> p a n", n=64),
                                                    axis=mybir.AxisListType.X, op=ALU.add),
                   reads=['lpr'], writes=['lsm'])
            S.emit('act', lambda e: e.activation(out=lsm[:], in_=lsm[:], func=AF.Exp), reads=['lsm'], writes=['lsm'])
            S.emit('dve', lambda e: e.tensor_tensor(out=neglam[:], in0=lsm[:, 1:2], in1=lsm[:, 0:1], op=ALU.subtract),
                   reads=['lsm'], writes=['neglam'])
            S.emit('dve', lambda e: e.tensor_scalar(out=neglam[:], in0=neglam[:], scalar1=float(-LAMBDA_INIT), scalar2=None,
                                                    op0=ALU.add), reads=['neglam'], writes=['neglam'])
            S.emit('pool', lambda e: e.memset(dv_ext[:, :, 128:129], 1.0), writes=['dv_ones'])

            fin_cnt = [0]
            for h in range(heads):
                W = 128 if h == 0 else 512
                load_cast(wdq[:], w_in[:, 2048 + h * 128:2048 + (h + 1) * 128].rearrange("(c p) n -> p c n", p=128), (8, 128), 'wdq')
                load_cast(wdk[:], w_in[:, 2560 + h * 128:2560 + (h + 1) * 128].rearrange("(c p) n -> p c n", p=128), (8, 128), 'wdk')
                load_cast(wdv[:], w_in[:, 3072 + h * 128:3072 + (h + 1) * 128].rearrange("(c p) n -> p c n", p=128), (8, 128), 'wdv')
                for tt in range(8):
                    xt, xk = loadx(tt)
                    tsl = slice(tt * 512, (tt + 1) * 512)
                    S.emit('pe', [lambda e, kc=kc, xt=xt: e.matmul(PS[0][:], lhsT=wdq[:, kc, :], rhs=xt[:, kc, :],
                                                                  start=(kc == 0), stop=(kc == 7)) for kc in range(8)],
                           reads=[xk, 'wdq'], writes=[('ps', 0)])
                    S.emit('pe', [lambda e, kc=kc, xt=xt: e.matmul(PS[1][:], lhsT=wdk[:, kc, :], rhs=xt[:, kc, :],
                                                                  start=(kc == 0), stop=(kc == 7)) for kc in range(8)],
                           reads=[xk, 'wdk'], writes=[('ps', 1)])
                    S.emit('act', lambda e, tsl=tsl: e.activation(out=dqT[:, tsl], in_=PS[0][:], func=AF.Copy, scale=0.125),
                           writes=[('dqT', tt), ('ps', 0)])
                    S.emit('dve', lambda e, tsl=tsl: e.tensor_copy(out=dkT[:, tsl], in_=PS[1][:]),
                           writes=[('dkT', tt), ('ps', 1)])
                    for s4 in range(4):
                        p = tt * 4 + s4
                        b = 2 + (p % 2)
                        S.emit('pe', [lambda e, kc=kc, xt=xt, s4=s4, b=b: e.matmul(
                            PS[b][:, 0:128], lhsT=xt[:, kc, s4 * 128:(s4 + 1) * 128], rhs=wdv[:, kc, :],
                            start=(kc == 0), stop=(kc == 7)) for kc in range(8)],
                            reads=[xk, 'wdv'], writes=[('ps', b)])
                        S.emit('dve', lambda e, p=p, b=b: e.tensor_copy(out=dv_ext[:, p, 0:128], in_=PS[b][:, 0:128]),
                               reads=['dv_ones'], writes=[('dv', p), ('ps', b)])
                ecnt = 0
                for qt in range(8):
                    started = {}
                    nk = 4 * qt + 4
                    for kt in range(nk):
                        st2 = kt % 2
                        sbk = [2 * st2, 2 * st2 + 1]
                        i0 = max(0, kt - 4 * qt)
                        c0 = i0 * 128
                        eb = ebuf[ecnt % 3]
                        ek = ('ebuf', ecnt % 3)
                        ecnt += 1
                        for j in range(2):
                            js = slice(j * 64, (j + 1) * 64)
                            S.emit('pe', lambda e, j=j, js=js, kt=kt, qt=qt, c0=c0, sbk=sbk: e.matmul(
                                PS[sbk[j]][:, c0:512], lhsT=dkT[js, kt * 128:(kt + 1) * 128],
                                rhs=dqT[js, qt * 512 + c0:(qt + 1) * 512], start=True, stop=True),
                                reads=[('dkT', kt // 4), ('dqT', qt)], writes=[('ps', sbk[j])])
                        for j in range(2):
                            fns = []
                            if W == 512:
                                col = (4 * qt - kt) + 3
                                fns.append(lambda e, j=j, c0=c0, col=col, sbk=sbk, eb=eb, h=h: e.activation(
                                    out=eb[:, j, c0:512], in_=PS[sbk[j]][:, c0:512], func=AF.Exp,
                                    bias=btab[:, h, col:col + 1], scale=1.0))
                            else:
                                for i in range(i0, 4):
                                    col = (4 * qt + i - kt) + 3
                                    fns.append(lambda e, j=j, i=i, col=col, sbk=sbk, eb=eb, h=h: e.activation(
                                        out=eb[:, j, i * 128:(i + 1) * 128], in_=PS[sbk[j]][:, i * 128:(i + 1) * 128],
                                        func=AF.Exp, bias=btab[:, h, col:col + 1], scale=1.0))
                            S.emit('act', fns, reads=[('btab', h)], writes=[ek, ('ps', sbk[j])], partial=(j > 0))
                        if kt >= 4 * qt:
                            S.emit('dve', [lambda e, j=j, i0=i0, eb=eb: e.tensor_tensor(
                                out=eb[:, j, i0 * 128:(i0 + 1) * 128], in0=eb[:, j, i0 * 128:(i0 + 1) * 128], in1=tri[:],
                                op=ALU.mult) for j in range(2)],
                                reads=['tri', ek], writes=[ek])
                        fns = []
                        banks = set()
                        for i in range(i0, 4):
                            for j in range(2):
                                idx = i * 2 + j
                                bO = 4 + idx // 3
                                cc = (idx % 3) * 129
                                stt = bO not in started
                                started[bO] = True
                                banks.add(bO)
                                fns.append(lambda e, i=i, j=j, bO=bO, cc=cc, stt=stt, kt=kt, qt=qt, eb=eb: e.matmul(
                                    PS[bO][:, cc:cc + 129], lhsT=eb[:, j, i * 128:(i + 1) * 128], rhs=dv_ext[:, kt, :],
                                    start=stt, stop=(kt == 4 * qt + i), skip_group_check=True))
                        S.emit('pe', fns, reads=[ek, ('dv', kt), 'dv_ones'], writes=[('ps', b_) for b_ in sorted(banks)])
                    for i in range(4):
                        f2 = fin_cnt[0] % 2
                        fin_cnt[0] += 1
                        qs = qt * 4 + i
                        regs = []
                        for j in range(2):
                            idx = i * 2 + j
                            regs.append((4 + idx // 3, (idx % 3) * 129))
                        (b1, c1), (b2, c2) = regs
                        S.emit('dve', lambda e, f2=f2, b1=b1, c1=c1: e.reciprocal(out=rz[f2][:, 0:1], in_=PS[b1][:, c1 + 128:c1 + 129]),
                               writes=[('rz', f2), ('ps', b1)])
                        S.emit('dve', lambda e, f2=f2, b2=b2, c2=c2: e.reciprocal(out=rz[f2][:, 1:2], in_=PS[b2][:, c2 + 128:c2 + 129]),
                               reads=[('rz', f2)], writes=[('rz', f2), ('ps', b2)])
                        S.emit('dve', lambda e, f2=f2: e.tensor_tensor(out=rz[f2][:, 1:2], in0=rz[f2][:, 1:2], in1=neglam[:], op=ALU.mult),
                               reads=[('rz', f2), 'neglam'], writes=[('rz', f2)])
                        S.emit('dve', lambda e, f2=f2, b1=b1, c1=c1: e.tensor_scalar(out=t1[f2][:], in0=PS[b1][:, c1:c1 + 128],
                                                                                    scalar1=rz[f2][:, 0:1], scalar2=None, op0=ALU.mult),
                               reads=[('rz', f2)], writes=[('t1', f2), ('ps', b1)])
                        S.emit('dve', lambda e, f2=f2, b2=b2, c2=c2: e.scalar_tensor_tensor(
                            out=o32[f2][:], in0=PS[b2][:, c2:c2 + 128], scalar=rz[f2][:, 1:2], in1=t1[f2][:], op0=ALU.mult, op1=ALU.add),
                            reads=[('rz', f2), ('t1', f2)], writes=[('o32', f2), ('ps', b2)])
                        S.emit('act', lambda e, f2=f2: e.activation(out=junk[:], in_=o32[f2][:], func=AF.Square,
                                                                  accum_out=ssB[:, f2:f2 + 1]),
                               reads=[('o32', f2)], writes=[('ssB', f2), 'junkd'])
                        S.emit('dve', lambda e, f2=f2: e.tensor_scalar(out=rsB[:, f2:f2 + 1], in0=ssB[:, f2:f2 + 1], scalar1=1.0 / 128,
                                                                     scalar2=NORM_EPS, op0=ALU.mult, op1=ALU.add),
                               reads=[('ssB', f2)], writes=[('rsB', f2)])
                        S.emit('act', lambda e, f2=f2: e.activation(out=rsB[:, f2:f2 + 1], in_=rsB[:, f2:f2 + 1], func=AF.Sqrt),
                               reads=[('rsB', f2)], writes=[('rsB', f2)])
                        S.emit('dve', lambda e, f2=f2: e.reciprocal(out=rsB[:, f2:f2 + 1], in_=rsB[:, f2:f2 + 1]),
                               reads=[('rsB', f2)], writes=[('rsB', f2)])
                        S.emit('dve', lambda e, f2=f2, h=h: e.scalar_tensor_tensor(
                            out=obb[f2][:], in0=o32[f2][:], scalar=rsB[:, f2:f2 + 1], in1=ngB[:, h, :], op0=ALU.mult, op1=ALU.mult),
                            reads=[('o32', f2), ('rsB', f2), 'ngB'], writes=[('obb', f2)])
                        tv = psb(7)[:, 0:128]
                        S.emit('pe', lambda e, f2=f2, tv=tv: e.transpose(out=tv, in_=obb[f2][:], identity=ident[:]),
                               reads=[('obb', f2), 'ident'], writes=[('ps', 7)])
                        S.emit('act', lambda e, h=h, qs=qs, tv=tv: e.copy(out=o_bT[:, h, qs * 128:(qs + 1) * 128], in_=tv),
                               writes=[('o_bT', h, qs), ('ps', 7)])
            S.barrier()
            if dbg:
                S.emit('sp', lambda e: e.dma_start(out=dbg_t['obT'], in_=o_bT[:]), dma='ddbg1')

        if stage >= 2:
            phase_D()

        def layer_norm(tiles, src, src_key, dst, dst_key, gt, g_key, bt, b_key, tag):
            bnst, mv, rstd, ytmp = tiles
            yk = dst_key if ytmp is dst else tag + 'ytmp'
            S.emit('dve', [lambda e, k=k: e.bn_stats(out=bnst[:, k, :], in_=src[:, k * 512:(k + 1) * 512]) for k in range(2)],
                   reads=[src_key], writes=[tag + 'bnst'])
            S.emit('dve', lambda e: e.bn_aggr(out=mv[:], in_=bnst[:].rearrange("p a b -> p (a b)")),
                   reads=[tag + 'bnst'], writes=[tag + 'mv'])
            S.emit('dve', lambda e: e.tensor_scalar(out=rstd[:], in0=mv[:, 1:2], scalar1=LN_EPS, scalar2=None, op0=ALU.add),
                   reads=[tag + 'mv'], writes=[tag + 'rstd'])
            S.emit('act', lambda e: e.activation(out=rstd[:], in_=rstd[:], func=AF.Sqrt), reads=[tag + 'rstd'], writes=[tag + 'rstd'])
            S.emit('dve', lambda e: e.reciprocal(out=rstd[:], in_=rstd[:]), reads=[tag + 'rstd'], writes=[tag + 'rstd'])
            S.emit('dve', lambda e: e.tensor_scalar(out=ytmp[:], in0=src[:], scalar1=mv[:, 0:1], scalar2=rstd[:],
                                                    op0=ALU.subtract, op1=ALU.mult),
                   reads=[src_key, tag + 'mv', tag + 'rstd'], writes=[yk])
            S.emit('pool', lambda e: e.tensor_tensor(out=ytmp[:], in0=ytmp[:], in1=gt[:], op=ALU.mult),
                   reads=[yk, g_key], writes=[yk])
            S.emit('pool', lambda e: e.tensor_tensor(out=dst[:], in0=ytmp[:], in1=bt[:], op=ALU.add),
                   reads=[yk, b_key], writes=[dst_key])

        def phase_M():
          with ExitStack() as P:
            def sb(name, shape, dt):
                return P.enter_context(nc.sbuf_tensor(uq(name), list(shape), dt))
            wa = sb("wa", [128, 4, 1024], BF16)
            wb_ = sb("wb", [128, 4, 1024], BF16)
            wo = sb("wo", [128, 8, 1024], BF16)
            rw = sb("rw", [128, 8, NE], F32)
            rbt = sb("rbt", [128, NE], F32)
            g1 = sb("g1", [128, D], F32)
            b1 = sb("b1", [128, D], F32)
            sga = [sb("sga0", [128, 8, 512], BF16)] * 2
            sgb = [sb("sgb0", [128, 8, 512], BF16)] * 2
            mT = [sb("mT0", [128, 8, 512], BF16)] * 2
            tA = [sb("tA%d" % i, [128, 512], F32) for i in range(2)]
            tB = [sb("tB%d" % i, [128, 512], F32) for i in range(2)]
            xres = [sb("xres%d" % i, [128, D], F32) for i in range(2)]
            rr = [sb("rr%d" % i, [128, D], F32) for i in range(2)]
            h1t = [sb("h1t%d" % i, [128, D], F32) for i in range(2)]
            bnst = sb("bnst", [128, 2, 6], F32)
            mv = sb("mv", [128, 2], F32)
            rstd = sb("rstd", [128, 1], F32)
            ytmp = sb("ytmp", [128, D], F32)
            h1T32 = sb("h1T32", [128, 8, 128], F32)
            h1Tb = [sb("h1Tb%d" % i, [128, 8, 128], BF16) for i in range(2)]
            lg = sb("lg", [128, NE], F32)
            top8 = sb("top8", [128, 8], F32)
            msk = sb("msk", [128, NE], F32)
            ex = sb("ex", [128, NE], F32)
            nm = sb("nm", [128, 1], F32)
            zs = sb("zs", [128, 1], F32)

            load_cast(wa[:], w_a.rearrange("(c p) n -> p c n", p=128), (4, 1024), 'wa')
            load_cast(wb_[:], w_b.rearrange("(c p) n -> p c n", p=128), (4, 1024), 'wb')
            load_cast(wo[:, 0:4, :], w_out[0:512, :].rearrange("(c p) n -> p c n", p=128), (4, 1024), ('wo', 0))
            load_cast(wo[:, 4:8, :], w_out[512:1024, :].rearrange("(c p) n -> p c n", p=128), (4, 1024), ('wo', 1))
            S.emit('sp', lambda e: e.dma_start(out=rw[:], in_=router_w.rearrange("(c p) n -> p c n", p=128)), writes=['rw'], dma='dsmall0')
            S.emit('sp', lambda e: e.dma_start(out=rbt[:], in_=bcast_ap(router_b, NE)), writes=['rbt'], dma='dsmall1')
            S.emit('sp', lambda e: e.dma_start(out=g1[:], in_=bcast_ap(ln1_g, D)), writes=['l1g'], dma='dsmall2')
            S.emit('sp', lambda e: e.dma_start(out=b1[:], in_=bcast_ap(ln1_b, D)), writes=['l1b'], dma='dsmall3')
            for tt in range(8):
                i2 = 0
                tsl = slice(tt * 512, (tt + 1) * 512)
                S.emit('sp', lambda e, i2=i2, tsl=tsl: e.dma_start(out=sga[i2][:], in_=sg_d[0][:, :, tsl].rearrange("c p n -> p c n")),
                       writes=[('sga', i2)], dma=('dsga', i2))
                S.emit('sp', lambda e, i2=i2, tsl=tsl: e.dma_start(out=sgb[i2][:], in_=sg_d[1][:, :, tsl].rearrange("c p n -> p c n")),
                       writes=[('sgb', i2)], dma=('dsgb', i2))
                for fc in range(8):
                    k2 = fc % 2
                    fsl = slice(fc * 128, (fc + 1) * 128)
                    S.emit('pe', [lambda e, kc=kc, fsl=fsl, tsl=tsl: e.matmul(PS[0][:], lhsT=wa[:, kc, fsl], rhs=o_aT[:, kc, tsl],
                                                                             start=(kc == 0), stop=(kc == 3)) for kc in range(4)],
                           reads=['wa'], writes=[('ps', 0)])
                    S.emit('pe', [lambda e, kc=kc, fsl=fsl, tsl=tsl: e.matmul(PS[1][:], lhsT=wb_[:, kc, fsl], rhs=o_bT[:, kc, tsl],
                                                                             start=(kc == 0), stop=(kc == 3)) for kc in range(4)],
                           reads=['wb'], writes=[('ps', 1)])
                    S.emit('dve', lambda e, k2=k2, i2=i2, fc=fc: e.tensor_tensor(out=tA[k2][:], in0=PS[0][:], in1=sga[i2][:, fc, :], op=ALU.mult),
                           reads=[('sga', i2)], writes=[('tA', k2), ('ps', 0)])
                    S.emit('dve', lambda e, k2=k2, i2=i2, fc=fc: e.tensor_tensor(out=tB[k2][:], in0=PS[1][:], in1=sgb[i2][:, fc, :], op=ALU.mult),
                           reads=[('sgb', i2)], writes=[('tB', k2), ('ps', 1)])
                    S.emit('pool', lambda e, k2=k2, i2=i2, fc=fc: e.tensor_tensor(out=mT[i2][:, fc, :], in0=tA[k2][:], in1=tB[k2][:], op=ALU.add),
                           reads=[('tA', k2), ('tB', k2)], writes=[('mT', i2)], partial=(fc > 0))
                for s4 in range(4):
                    p = tt * 4 + s4
                    j = p % 2
                    S.emit('sp', lambda e, p=p, j=j: e.dma_start(out=xres[j][:], in_=x[p * 128:(p + 1) * 128, :]),
                           writes=[('xres', j)], dma=('dxres', j))
                    for hf in range(2):
                        S.emit('pe', [lambda e, kc=kc, i2=i2, s4=s4, hf=hf: e.matmul(
                            PS[2 + hf][:], lhsT=mT[i2][:, kc, s4 * 128:(s4 + 1) * 128], rhs=wo[:, kc, hf * 512:(hf + 1) * 512],
                            start=(kc == 0), stop=(kc == 7)) for kc in range(8)],
                            reads=[('mT', i2), ('wo', 0), ('wo', 1)], writes=[('ps', 2 + hf)])
                        S.emit('dve', lambda e, j=j, hf=hf: e.scalar_tensor_tensor(
                            out=rr[j][:, hf * 512:(hf + 1) * 512], in0=xres[j][:, hf * 512:(hf + 1) * 512], scalar=float(DN_ALPHA),
                            in1=PS[2 + hf][:], op0=ALU.mult, op1=ALU.add),
                            reads=[('xres', j)], writes=[('rr', j), ('ps', 2 + hf)], partial=(hf > 0))
                    layer_norm((bnst, mv, rstd, ytmp), rr[j], ('rr', j), h1t[j], ('h1t', j), g1, 'l1g', b1, 'l1b', 'm_')
                    S.emit('sp', lambda e, p=p, j=j: e.dma_start(out=h1_d[p * 128:(p + 1) * 128, :], in_=h1t[j][:]),
                           reads=[('h1t', j)], dma=('dh1', j))
                    for g in range(2):
                        S.emit('pe', [lambda e, c=c, j=j, g=g: e.transpose(out=PS[4 + g][:, (c % 4) * 128:(c % 4 + 1) * 128],
                                                                          in_=h1t[j][:, c * 128:(c + 1) * 128], identity=identf[:])
                                      for c in range(4 * g, 4 * g + 4)],
                               reads=[('h1t', j), 'identf'], writes=[('ps', 4 + g)])
                        S.emit('act', lambda e, j=j, g=g: e.copy(out=h1Tb[j][:, 4 * g:4 * g + 4, :],
                                                                 in_=PS[4 + g][:].rearrange("p (c n) -> p c n", n=128)),
                               writes=[('h1Tb', j), ('ps', 4 + g)], partial=(g > 0))
                        S.emit('dve', lambda e, g=g: e.tensor_copy(out=h1T32[:, 4 * g:4 * g + 4, :],
                                                                   in_=PS[4 + g][:].rearrange("p (c n) -> p c n", n=128)),
                               writes=['h1T32', ('ps', 4 + g)], partial=(g > 0))
                    S.emit('sp', lambda e, p=p, j=j: e.dma_start(
                        out=h1T_d[:, :, p * 128:(p + 1) * 128].rearrange("c p n -> p c n"), in_=h1Tb[j][:]),
                        reads=[('h1Tb', j)], dma=('dh1T', j))
                    S.emit('pe', [lambda e, c=c: e.matmul(PS[6][:, 0:NE], lhsT=h1T32[:, c, :], rhs=rw[:, c, :],
                                                          start=(c == 0), stop=(c == 7)) for c in range(8)],
                           reads=['h1T32', 'rw'], writes=[('ps', 6)])
                    S.emit('dve', lambda e: e.tensor_tensor(out=lg[:], in0=PS[6][:, 0:NE], in1=rbt[:], op=ALU.add),
                           reads=['rbt'], writes=['lg', ('ps', 6)])
                    S.emit('dve', lambda e: e.max(out=top8[:], in_=lg[:]), reads=['lg'], writes=['top8'])
                    S.emit('dve', lambda e: e.tensor_scalar(out=msk[:], in0=lg[:], scalar1=top8[:, 3:4], scalar2=None, op0=ALU.is_ge),
                           reads=['lg', 'top8'], writes=['msk'])
                    S.emit('dve', lambda e: e.tensor_scalar(out=nm[:], in0=top8[:, 0:1], scalar1=-1.0, scalar2=None, op0=ALU.mult),
                           reads=['top8'], writes=['nm'])
                    S.emit('act', lambda e: e.activation(out=ex[:], in_=lg[:], func=AF.Exp, bias=nm[:], scale=1.0),
                           reads=['lg', 'nm'], writes=['ex'])
                    S.emit('dve', lambda e: e.tensor_tensor(out=ex[:], in0=ex[:], in1=msk[:], op=ALU.mult),
                           reads=['ex', 'msk'], writes=['ex'])
                    S.emit('dve', lambda e: e.tensor_reduce(out=zs[:], in_=ex[:], axis=mybir.AxisListType.X, op=ALU.add),
                           reads=['ex'], writes=['zs'])
                    S.emit('dve', lambda e: e.reciprocal(out=zs[:], in_=zs[:]), reads=['zs'], writes=['zs'])
                    S.emit('dve', lambda e, p=p: e.tensor_scalar(out=comb[:, p, :], in0=ex[:], scalar1=zs[:], scalar2=None, op0=ALU.mult),
                           reads=['ex', 'zs'], writes=[('comb', p)])
            S.barrier()
            if dbg:
                S.emit('sp', lambda e: e.dma_start(out=dbg_t['h1'], in_=h1_d), dma='ddbg2')
                S.emit('sp', lambda e: e.dma_start(out=dbg_t['comb'], in_=comb[:]), dma='ddbg3')
                S.barrier()

        if stage >= 3:
            phase_M()
        MIX.close()

        def phase_E():
          with ExitStack() as P:
            def sb(name, shape, dt):
                return P.enter_context(nc.sbuf_tensor(uq(name), list(shape), dt))
            h1T = sb("h1T", [128, 8, PASS_TOK], BF16)
            acc = sb("acc", [128, 8, D], F32)
            wguh = [sb("wguh%d" % i, [128, 8, 2, 512], BF16) for i in range(3)]
            wdh = [sb("wdh%d" % i, [128, 4, D], BF16) for i in range(3)]
            est = [sb("est%d" % i, [128, 2048], F32) for i in range(3)]
            bias_all = sb("bias_all", [128, 2, 8, NE], F32)
            bd = sb("bd", [NE, D], F32)
            combT = [sb("combT%d" % i, [NE, 128], F32) for i in range(2)]
            g_t = [sb("g_t%d" % i, [128, 512], F32) for i in range(2)]
            s_t = [sb("s_t%d" % i, [128, 512], F32) for i in range(2)]
            u_t = [sb("u_t%d" % i, [128, 512], F32) for i in range(2)]
            actT = [sb("actT%d" % i, [128, 4, 512], BF16) for i in range(2)]
            h1r = sb("h1r", [128, D], F32)
            r2 = sb("r2", [128, D], F32)
            yout = sb("yout", [128, D], F32)
            g2 = sb("g2", [128, D], F32)
            b2 = sb("b2", [128, D], F32)
            bnst = sb("bnst2", [128, 2, 6], F32)
            mv = sb("mv2", [128, 2], F32)
            rstd = sb("rstd2", [128, 1], F32)
            bgu32 = est[2][0:NE, :]

            S.emit('sp', lambda e: e.dma_start(out=bgu32, in_=b_gu), writes=[('est', 2)], dma='dsmall0')
            S.emit('sp', lambda e: e.dma_start(out=bd[:], in_=b_dn), writes=['bd'], dma='dsmall1')
            S.emit('sp', lambda e: e.dma_start(out=g2[:], in_=bcast_ap(ln2_g, D)), writes=['l2g'], dma='dsmall2')
            S.emit('sp', lambda e: e.dma_start(out=b2[:], in_=bcast_ap(ln2_b, D)), writes=['l2b'], dma='dsmall3')
            S.emit('pe', [lambda e, g=g, fc=fc: e.transpose(
                out=PS[7][:, (g * 8 + fc) * NE:(g * 8 + fc + 1) * NE],
                in_=est[2][0:NE, fc * 256 + g:(fc + 1) * 256:2], identity=identf[0:NE, 0:NE])
                for g in range(2) for fc in range(8)],
                reads=[('est', 2), 'identf'], writes=[('ps', 7)])
            S.emit('dve', lambda e: e.tensor_copy(out=bias_all[:].rearrange("p a b c -> p (a b c)"), in_=PS[7][:]),
                   writes=['bias_all', ('ps', 7)])
            S.emit('dve', lambda e: e.tensor_scalar(out=bias_all[:, 1, :, :], in0=bias_all[:, 1, :, :], scalar1=1.0, scalar2=None,
                                                    op0=ALU.add), reads=['bias_all'], writes=['bias_all'])

            ecnt = [0]
            ccnt = [0]

            def load_unit(ps_, e_, hfu, slot):
                first = True
                for q in range(4):
                    i = ecnt[0] % 3
                    ecnt[0] += 1
                    S.emit('sp', lambda e, i=i, q=q: e.dma_start(
                        out=est[i][:].rearrange("p (c n) -> p c n", n=1024),
                        in_=w_gu[e_, q * 256:(q + 1) * 256, hfu * 1024:(hfu + 1) * 1024].rearrange("(c p) n -> p c n", p=128)),
                        writes=[('est', i)], dma=('dest', i))
                    for c in range(2):
                        kc = q * 2 + c
                        ce = 'pool'
                        ccnt[0] += 1
                        src = est[i][:, c * 1024:(c + 1) * 1024].rearrange("p (f g) -> p g f", g=2)
                        if ce == 'act':
                            S.emit('act', lambda e, kc=kc, src=src: e.copy(out=wguh[slot][:, kc, :, :], in_=src),
                                   reads=[('est', i)], writes=[('wguh', slot)], partial=(not first))
                        else:
                            S.emit('pool', lambda e, kc=kc, src=src: e.tensor_copy(out=wguh[slot][:, kc, :, :], in_=src),
                                   reads=[('est', i)], writes=[('wguh', slot)], partial=(not first))
                        first = False
                for q in range(2):
                    i = ecnt[0] % 3
                    ecnt[0] += 1
                    S.emit('sp', lambda e, i=i, q=q: e.dma_start(
                        out=est[i][:].rearrange("p (c n) -> p c n", n=1024),
                        in_=w_dn[e_, hfu * 512 + q * 256:hfu * 512 + (q + 1) * 256, :].rearrange("(c p) n -> p c n", p=128)),
                        writes=[('est', i)], dma=('dest', i))
                    ce = 'pool'
                    ccnt[0] += 1
                    src = est[i][:].rearrange("p (c n) -> p c n", n=1024)
                    if ce == 'act':
                        S.emit('act', lambda e, q=q, src=src: e.copy(out=wdh[slot][:, 2 * q:2 * q + 2, :], in_=src),
                               reads=[('est', i)], writes=[('wdh', slot)], partial=(q > 0))
                    else:
                        S.emit('pool', lambda e, q=q, src=src: e.tensor_copy(out=wdh[slot][:, 2 * q:2 * q + 2, :], in_=src),
                               reads=[('est', i)], writes=[('wdh', slot)], partial=(q > 0))

            gcnt = [0]
            ycnt = [0]

            def emit_gu(ps_, e_, hfu, slot, tile, ai):
                tl = slice(tile * 512, (tile + 1) * 512)

                def finish(fcl, k):
                    S.emit('dve', lambda e, k=k: e.tensor_tensor(out=g_t[k][:], in0=g_t[k][:], in1=s_t[k][:], op=ALU.mult),
                           reads=[('g_t', k), ('s_t', k)], writes=[('g_t', k)])
                    S.emit('dve', lambda e, k=k, fcl=fcl: e.scalar_tensor_tensor(
                        out=actT[ai][:, fcl, :], in0=u_t[k][:], scalar=-6.0, in1=g_t[k][:], op0=ALU.max, op1=ALU.mult),
                        reads=[('u_t', k), ('g_t', k)], writes=[('actT', ai)], partial=(fcl > 0))
                pend = None
                for fcl in range(4):
                    fc = hfu * 4 + fcl
                    k = gcnt[0] % 2
                    gcnt[0] += 1
                    bg, bu = 2 * k, 2 * k + 1
                    fs = slice(fcl * 128, (fcl + 1) * 128)
                    S.emit('pe', [lambda e, kc=kc, fs=fs, tl=tl, bg=bg: e.matmul(
                        PS[bg][:], lhsT=wguh[slot][:, kc, 0, fs], rhs=h1T[:, kc, tl], start=(kc == 0), stop=(kc == 7))
                        for kc in range(8)], reads=[('wguh', slot), 'h1T'], writes=[('ps', bg)])
                    S.emit('pe', [lambda e, kc=kc, fs=fs, tl=tl, bu=bu: e.matmul(
                        PS[bu][:], lhsT=wguh[slot][:, kc, 1, fs], rhs=h1T[:, kc, tl], start=(kc == 0), stop=(kc == 7))
                        for kc in range(8)], reads=[('wguh', slot), 'h1T'], writes=[('ps', bu)])
                    S.emit('dve', lambda e, k=k, bg=bg, fc=fc: e.tensor_scalar(
                        out=g_t[k][:], in0=PS[bg][:], scalar1=bias_all[:, 0, fc, e_:e_ + 1], scalar2=7.0, op0=ALU.add, op1=ALU.min),
                        reads=['bias_all'], writes=[('g_t', k), ('ps', bg)])
                    S.emit('act', lambda e, k=k: e.activation(out=s_t[k][:], in_=g_t[k][:], func=AF.Sigmoid, scale=1.702),
                           reads=[('g_t', k)], writes=[('s_t', k)])
                    S.emit('dve', lambda e, k=k, bu=bu, fc=fc: e.tensor_scalar(
                        out=u_t[k][:], in0=PS[bu][:], scalar1=bias_all[:, 1, fc, e_:e_ + 1], scalar2=8.0, op0=ALU.add, op1=ALU.min),
                        reads=['bias_all'], writes=[('u_t', k), ('ps', bu)])
                    if pend is not None:
                        finish(*pend)
                    pend = (fcl, k)
                finish(*pend)

            def emit_down(ps_, e_, hfu, slot, tile, ai):
                for s4 in range(4):
                    st = tile * 4 + s4
                    p = ps_ * 8 + st
                    for d2 in range(2):
                        by = 4 + (ycnt[0] % 2)
                        ycnt[0] += 1
                        ds = slice(d2 * 512, (d2 + 1) * 512)
                        S.emit('pe', [lambda e, fcl=fcl, s4=s4, ds=ds, by=by: e.matmul(
                            PS[by][:], lhsT=actT[ai][:, fcl, s4 * 128:(s4 + 1) * 128], rhs=wdh[slot][:, fcl, ds],
                            start=(fcl == 0), stop=(fcl == 3)) for fcl in range(4)],
                            reads=[('actT', ai), ('wdh', slot)], writes=[('ps', by)])
                        S.emit('dve', lambda e, st=st, p=p, ds=ds, by=by: e.scalar_tensor_tensor(
                            out=acc[:, st, ds], in0=PS[by][:], scalar=comb[:, p, e_:e_ + 1], in1=acc[:, st, ds],
                            op0=ALU.mult, op1=ALU.add),
                            reads=[('acc', st, d2)], writes=[('acc', st, d2), ('ps', by)])

            units = [(ps_, e_, hfu) for ps_ in range(NPASS) for e_ in range(NE) for hfu in range(2)]
            if stage == 4 and ne_decl < NE:
                units = [(ps_, e_, hfu) for ps_ in range(NPASS) for e_ in range(ne_decl) for hfu in range(2)]
            nun = len(units)
            per_pass = nun // NPASS
            for u0 in range(min(2, nun)):
                load_unit(*units[u0], u0 % 3)
            prev = None
            tcount = 0
            for u, (ps_, e_, hfu) in enumerate(units):
                slot = u % 3
                if u % per_pass == 0:
                    tok0 = ps_ * PASS_TOK
                    S.emit('sp', lambda e, tok0=tok0: e.dma_start(
                        out=h1T[:], in_=h1T_d[:, :, tok0:tok0 + PASS_TOK].rearrange("c p n -> p c n")),
                        writes=['h1T'], dma='dh1Tp')
                    for st in range(8):
                        p = ps_ * 8 + st
                        ci = st % 2
                        S.emit('pe', lambda e, p=p: e.transpose(out=PS[7][0:NE, 0:128], in_=comb[:, p, :], identity=identf[:]),
                               reads=['identf'], writes=[('ps', 7)])
                        S.emit('dve', lambda e, ci=ci: e.tensor_copy(out=combT[ci][:], in_=PS[7][0:NE, 0:128]),
                               writes=[('combT', ci), ('ps', 7)])
                        for d2 in range(2):
                            ds = slice(d2 * 512, (d2 + 1) * 512)
                            S.emit('pe', lambda e, ci=ci, ds=ds: e.matmul(PS[6][:], lhsT=combT[ci][:], rhs=bd[:, ds], start=True, stop=True),
                                   reads=[('combT', ci), 'bd'], writes=[('ps', 6)])
                            S.emit('act', lambda e, st=st, ds=ds: e.copy(out=acc[:, st, ds], in_=PS[6][:]),
                                   writes=[('acc', st, d2), ('ps', 6)])
                for tile in range(2):
                    ai = tcount % 2
                    tcount += 1
                    emit_gu(ps_, e_, hfu, slot, tile, ai)
                    if prev is not None:
                        emit_down(*prev)
                    prev = (ps_, e_, hfu, slot, tile, ai)
                    if tile == 0 and u + 2 < nun:
                        load_unit(*units[u + 2], (u + 2) % 3)
                if (u + 1) % per_pass == 0:
                    emit_down(*prev)
                    prev = None
                    for st in range(8):
                        p = ps_ * 8 + st
                        S.emit('sp', lambda e, p=p: e.dma_start(out=h1r[:], in_=h1_d[p * 128:(p + 1) * 128, :]),
                               writes=['h1r'], dma='dh1r')
                        S.emit('dve', lambda e, st=st: e.scalar_tensor_tensor(
                            out=r2[:], in0=h1r[:], scalar=float(DN_ALPHA), in1=acc[:, st, :], op0=ALU.mult, op1=ALU.add),
                            reads=['h1r', ('acc', st, 0), ('acc', st, 1)], writes=['r2'])
                        layer_norm((bnst, mv, rstd, yout), r2, 'r2', yout, 'yout', g2, 'l2g', b2, 'l2b', 'f_')
                        S.emit('sp', lambda e, p=p: e.dma_start(out=y[p * 128:(p + 1) * 128, :], in_=yout[:]),
                               reads=['yout'], writes=['yout_dma'], dma='dyout')
            S.barrier()

        if stage >= 4:
            phase_E()

        S.barrier()
        S.run()
    return nc


_NC_CACHE = {}


def _get_nc():
    if 'nc' not in _NC_CACHE:
        _NC_CACHE['nc'] = build()
    return _NC_CACHE['nc']


def kernel(**inputs):
    nc = _get_nc()
    n = 8
    names = ["w_in", "hg_lb_logits", "hg_norm_g", "da_lambda", "da_norm_g", "w_branch_a", "w_branch_b", "w_out",
             "ln1_g", "ln1_b", "router_w", "router_b", "w_gate_up", "b_gate_up", "w_down", "b_down", "ln2_g", "ln2_b"]
    shared = {}
    for k in names:
        a = np.ascontiguousarray(np.asarray(inputs[k], dtype=np.float32))
        shared[k] = a[0] if k != "hg_lb_logits" else a
    xx = np.asarray(inputs["x"], dtype=np.float32)
    in_maps = []
    for c in range(n):
        m = dict(shared)
        m["x"] = np.ascontiguousarray(xx[c])
        in_maps.append(m)
    res = run_bass_kernel_spmd(nc, in_maps, core_ids=list(range(n)))
    return np.stack([np.asarray(r["y"]) for r in res.results], axis=0).astype(np.float32)
```

```python
import math
from contextlib import ExitStack

import numpy as np
import concourse.bass as bass
import concourse.mybir as mybir
from concourse.bass_utils import run_bass_kernel_spmd

F32 = mybir.dt.float32
BF16 = mybir.dt.bfloat16
I32 = mybir.dt.int32
AF = mybir.ActivationFunctionType
ALU = mybir.AluOpType

S_LEN = 4096
D = 1024
NT = S_LEN // 128
NE = 32
DFF = 1024
LN_EPS = 1e-5
NORM_EPS = 1e-6
DN_ALPHA = 2.0 ** 0.25
LAMBDA_INIT = 0.8 - 0.6 * math.exp(0.0)
SLOPES = [2.0 ** (-8.0 * (h + 1) / 4) for h in range(4)]
NPASS = 4
PASS_TOK = S_LEN // NPASS


class Sched:
    ENG = ('pe', 'act', 'dve', 'pool', 'sp')

    def __init__(self, nc):
        self.nc = nc
        self.prog = {k: [] for k in self.ENG}
        self.cnt = {}
        self.sems = {}
        self.seen = {k: {} for k in self.ENG}
        self.res = {}
        for k in self.ENG:
            self._sem(k)

    def _sem(self, key):
        if key not in self.sems:
            self.sems[key] = self.nc.alloc_semaphore(name="s%d" % len(self.sems))
            self.cnt[key] = 0
        return self.sems[key]

    def emit(self, eng, fns, reads=(), writes=(), dma=None, partial=False):
        if callable(fns):
            fns = [fns]
        deps = {}

        def add(d):
            for s, v in d.items():
                if deps.get(s, 0) < v:
                    deps[s] = v
        for r in reads:
            st = self.res.get(r)
            if st:
                add(st['w'])
        for w in writes:
            st = self.res.get(w)
            if st:
                add(st['w'])
                add(st['r'])
        if dma is not None:
            self._sem(dma)
            if self.cnt[dma] > 0:
                add({dma: self.cnt[dma]})
        P = self.prog[eng]
        for s, v in deps.items():
            if s == eng and eng == 'pe':
                continue
            if self.seen[eng].get(s, 0) >= v:
                continue
            self.seen[eng][s] = v
            sem = self.sems[s]
            P.append(lambda e, sem=sem, v=v: e.wait_ge(sem, v))
        skey, inc = (dma, 16) if dma is not None else (eng, 1)
        self.cnt[skey] += inc
        val = self.cnt[skey]
        sem = self.sems[skey]
        n = len(fns)
        for i, f in enumerate(fns):
            if i == n - 1:
                P.append(lambda e, f=f, sem=sem, inc=inc: f(e).then_inc(sem, inc))
            else:
                P.append(lambda e, f=f: f(e))
        for r in reads:
            st = self.res.setdefault(r, {'w': {}, 'r': {}})
            if st['r'].get(skey, 0) < val:
                st['r'][skey] = val
        for w in writes:
            if partial and w in self.res:
                self.res[w]['w'][skey] = val
            else:
                self.res[w] = {'w': {skey: val}, 'r': {}}

    def barrier(self):
        allev = dict(self.cnt)
        for eng in self.ENG:
            for s, v in allev.items():
                if v == 0 or (s == eng and eng == 'pe'):
                    continue
                if self.seen[eng].get(s, 0) >= v:
                    continue
                self.seen[eng][s] = v
                sem = self.sems[s]
                self.prog[eng].append(lambda e, sem=sem, v=v: e.wait_ge(sem, v))
        self.res = {}

    def run(self):
        nc = self.nc
        with nc.Block() as block:
            @block.tensor
            def _(e):
                for f in self.prog['pe']:
                    f(e)

            @block.scalar
            def _(e):
                for f in self.prog['act']:
                    f(e)

            @block.vector
            def _(e):
                for f in self.prog['dve']:
                    f(e)

            @block.gpsimd
            def _(e):
                for f in self.prog['pool']:
                    f(e)

            @block.sync
            def _(e):
                for f in self.prog['sp']:
                    f(e)


def bcast_ap(dram_ap, n, offset=0, parts=128):
    return bass.AP(dram_ap.tensor, dram_ap.offset + offset, [[0, parts], [1, n]])


def build(stage=99, dbg=False, hstop=99, ne_decl=NE, heads=4, cut=99):
    nc = bass.Bass("TRN2", target_bir_lowering=False)

    def din(name, shape):
        return nc.dram_tensor(name, list(shape), F32, kind="ExternalInput").ap()

    x = din("x", [S_LEN, D])
    w_in = din("w_in", [D, 5632])
    hg_lb = din("hg_lb_logits", [2, 512])
    hg_ng = din("hg_norm_g", [4, 128])
    da_lam = din("da_lambda", [4, 64])
    da_ng = din("da_norm_g", [4, 128])
    w_a = din("w_branch_a", [512, D])
    w_b = din("w_branch_b", [512, D])
    w_out = din("w_out", [D, D])
    ln1_g = din("ln1_g", [D])
    ln1_b = din("ln1_b", [D])
    router_w = din("router_w", [D, NE])
    router_b = din("router_b", [NE])
    w_gu = din("w_gate_up", [ne_decl, D, 2 * DFF])
    b_gu = din("b_gate_up", [NE, 2 * DFF])
    w_dn = din("w_down", [ne_decl, DFF, D])
    b_dn = din("b_down", [NE, D])
    ln2_g = din("ln2_g", [D])
    ln2_b = din("ln2_b", [D])
    y = nc.dram_tensor("y", [S_LEN, D], F32, kind="ExternalOutput").ap()

    xT_d = nc.dram_tensor("xT_d", [8, 128, S_LEN], BF16, kind="Internal").ap()
    sg_d = [nc.dram_tensor("sg%d_d" % i, [8, 128, S_LEN], BF16, kind="Internal").ap() for i in range(2)]
    h1_d = nc.dram_tensor("h1_d", [S_LEN, D], F32, kind="Internal").ap()
    h1T_d = nc.dram_tensor("h1T_d", [8, 128, S_LEN], BF16, kind="Internal").ap()
    dbg_t = {}
    if dbg:
        dbg_t['oaT'] = nc.dram_tensor("dbg_oaT", [128, 4, S_LEN], BF16, kind="ExternalOutput").ap()
        dbg_t['obT'] = nc.dram_tensor("dbg_obT", [128, 4, S_LEN], BF16, kind="ExternalOutput").ap()
        dbg_t['h1'] = nc.dram_tensor("dbg_h1", [S_LEN, D], F32, kind="ExternalOutput").ap()
        dbg_t['comb'] = nc.dram_tensor("dbg_comb", [128, NT, NE], F32, kind="ExternalOutput").ap()

    S = Sched(nc)
    _uid = [0]

    def uq(name):
        _uid[0] += 1
        return "%s_u%d" % (name, _uid[0])
    with ExitStack() as G:
        def sbg(name, shape, dt):
            return G.enter_context(nc.sbuf_tensor(uq(name), list(shape), dt))
        PS = [G.enter_context(nc.psum_tensor("ps%d" % i, [128, 512], F32)) for i in range(8)]

        def psb(i):
            return PS[i][:].bitcast(BF16)

        identf = sbg("identf", [128, 128], F32)
        ident = sbg("ident", [128, 128], BF16)
        tri = sbg("tri", [128, 128], BF16)
        tribd = sbg("tribd", [128, 128], BF16)
        onesf = sbg("onesf", [128, 128], F32)
        scanmask = sbg("scanmask", [128, 512], F32)
        btab_i = sbg("btab_i", [128, 35], I32)
        btab = sbg("btab", [128, 4, 35], F32)
        comb = sbg("comb", [128, NT, NE], F32)
        MIX = ExitStack()

        def sbm(name, shape, dt):
            return MIX.enter_context(nc.sbuf_tensor(uq(name), list(shape), dt))
        o_aT = sbm("o_aT", [128, 4, S_LEN], BF16)
        o_bT = sbm("o_bT", [128, 4, S_LEN], BF16)
        wst = [sbm("wst%d" % i, [128, 2048], F32) for i in range(2)]

        S.emit('pool', lambda e: e.memset(identf[:], 0.0), writes=['identf'])
        S.emit('pool', lambda e: e.affine_select(out=identf[:], in_=identf[:], pattern=[[-1, 128]],
                                                  compare_op=ALU.not_equal, fill=1.0, base=0,
                                                  channel_multiplier=1),
               reads=['identf'], writes=['identf'])
        S.emit('dve', lambda e: e.tensor_copy(out=ident[:], in_=identf[:]), reads=['identf'], writes=['ident'])
        S.emit('pool', lambda e: e.memset(onesf[:], 1.0), writes=['onesf'])
        S.emit('pool', lambda e: e.affine_select(out=tri[:], in_=onesf[:], pattern=[[1, 128]],
                                                  compare_op=ALU.is_ge, fill=0.0, base=0,
                                                  channel_multiplier=-1),
               reads=['onesf'], writes=['tri'])
        S.emit('pool', lambda e: e.tensor_copy(out=tribd[:], in_=tri[:]), reads=['tri'], writes=['tribd'])
        S.emit('pool', lambda e: e.memset(tribd[0:64, 64:128], 0.0), reads=['tribd'], writes=['tribd'])
        S.emit('pool', lambda e: e.memset(scanmask[:], 1.0), writes=['scanmask'])
        S.emit('pool', lambda e: e.memset(scanmask[:].rearrange("p (c t) -> p c t", t=64)[:, :, 0:1], 0.0),
               reads=['scanmask'], writes=['scanmask'])
        S.emit('pool', lambda e: e.iota(btab_i[:], pattern=[[-128, 35]], base=384, channel_multiplier=1),
               writes=['btab_i'])
        for h in range(4):
            S.emit('dve', lambda e, h=h: e.tensor_scalar(out=btab[:, h, :], in0=btab_i[:], scalar1=float(SLOPES[h]),
                                                        scalar2=None, op0=ALU.mult),
                   reads=['btab_i'], writes=[('btab', h)])

        bank_rr = [0]

        def load_cast(dst_ap, src_ap, shape, key, cast_eng='pool'):
            a, b = shape
            step = max(1, 2048 // b)
            for a0 in range(0, a, step):
                a1 = min(a, a0 + step)
                i = bank_rr[0] % 2
                bank_rr[0] += 1
                st = wst[i][:, 0:(a1 - a0) * b].rearrange("p (a b) -> p a b", b=b)
                S.emit('sp', lambda e, st=st, a0=a0, a1=a1: e.dma_start(out=st, in_=src_ap[:, a0:a1, :]),
                       writes=[('wst', i)], dma=('dwst', i))
                if cast_eng == 'act':
                    S.emit('act', lambda e, st=st, a0=a0, a1=a1: e.copy(out=dst_ap[:, a0:a1, :], in_=st),
                           reads=[('wst', i)], writes=[key], partial=True)
                else:
                    S.emit(cast_eng, lambda e, st=st, a0=a0, a1=a1: e.tensor_copy(out=dst_ap[:, a0:a1, :], in_=st),
                           reads=[('wst', i)], writes=[key], partial=True)

        with ExitStack() as P:
            def sb(name, shape, dt):
                return P.enter_context(nc.sbuf_tensor(uq(name), list(shape), dt))
            xs = [sb("xs%d" % i, [128, D], F32) for i in range(2)]
            xb = [sb("xb%d" % i, [128, D], BF16) for i in range(2)]
            xTs = [sb("xTs%d" % i, [128, 8, 512], BF16) for i in range(2)]
            for t in range(NT):
                i = t % 2
                S.emit('sp', lambda e, t=t, i=i: e.dma_start(out=xs[i][:], in_=x[t * 128:(t + 1) * 128, :]),
                       writes=[('xs', i)], dma=('dxs', i))
                if i == 0:
                    S.emit('dve', lambda e, i=i: e.tensor_copy(out=xb[i][:], in_=xs[i][:]), reads=[('xs', i)], writes=[('xb', i)])
                else:
                    S.emit('act', lambda e, i=i: e.copy(out=xb[i][:], in_=xs[i][:]), reads=[('xs', i)], writes=[('xb', i)])
                bk = t % 2
                pv = psb(bk).rearrange("p (c n) -> p c n", n=128)
                S.emit('pe', [lambda e, c=c, i=i, pv=pv: e.transpose(out=pv[:, c, :], in_=xb[i][:, c * 128:(c + 1) * 128],
                                                                     identity=ident[:]) for c in range(8)],
                       reads=[('xb', i), 'ident'], writes=[('ps', bk)])
                g4 = (t // 4) % 2
                j = t % 4
                ce = 'dve' if i == 1 else 'act'
                if ce == 'dve':
                    S.emit('dve', lambda e, g4=g4, j=j, pv=pv: e.tensor_copy(out=xTs[g4][:, :, j * 128:(j + 1) * 128], in_=pv),
                           reads=[('ps', bk)], writes=[('xTs', g4)], partial=(j > 0))
                else:
                    S.emit('act', lambda e, g4=g4, j=j, pv=pv: e.copy(out=xTs[g4][:, :, j * 128:(j + 1) * 128], in_=pv),
                           reads=[('ps', bk)], writes=[('xTs', g4)], partial=(j > 0))
                if j == 3:
                    tt = t // 4
                    S.emit('sp', lambda e, g4=g4, tt=tt: e.dma_start(
                        out=xT_d[:, :, tt * 512:(tt + 1) * 512].rearrange("c p n -> p c n"), in_=xTs[g4][:]),
                        reads=[('xTs', g4)], dma=('dxT', g4))
            S.barrier()

        def make_xT_loader(P):
            bufs = [P.enter_context(nc.sbuf_tensor(uq("xTt%d" % i), [128, 8, 512], BF16)) for i in range(2)]
            cnt = [0]

            def load(tt):
                i = cnt[0] % 2
                cnt[0] += 1
                S.emit('sp', lambda e: e.dma_start(out=bufs[i][:],
                                                   in_=xT_d[:, :, tt * 512:(tt + 1) * 512].rearrange("c p n -> p c n")),
                       writes=[('xTt', i)], dma=('dxTt', i))
                return bufs[i], ('xTt', i)
            return load

        with ExitStack() as P:
            def sb(name, shape, dt):
                return P.enter_context(nc.sbuf_tensor(uq(name), list(shape), dt))
            wg = sb("wg", [128, 8, 2048], BF16)
            for q4 in range(4):
                load_cast(wg[:, :, q4 * 512:(q4 + 1) * 512],
                          w_in[:, 3584 + q4 * 512:3584 + (q4 + 1) * 512].rearrange("(c p) n -> p c n", p=128),
                          (8, 512), ('wg', q4))
            sgst = [sb("sgst%d" % i, [128, 8, 512], BF16) for i in range(2)]
            loadx = make_xT_loader(P)
            bk = 0
            for tt in range(8):
                xt, xk = loadx(tt)
                for gi in range(2):
                    for fc in range(8):
                        b = bk % 4
                        bk += 1
                        c0 = gi * 1024 + fc * 128
                        S.emit('pe', [lambda e, kc=kc, c0=c0, b=b, xt=xt: e.matmul(
                            PS[b][:], lhsT=wg[:, kc, c0:c0 + 128], rhs=xt[:, kc, :], start=(kc == 0), stop=(kc == 7))
                            for kc in range(8)],
                            reads=[xk, ('wg', c0 // 512)], writes=[('ps', b)])
                        S.emit('act', lambda e, gi=gi, fc=fc, b=b: e.activation(out=sgst[gi][:, fc, :], in_=PS[b][:],
                                                                               func=AF.Sigmoid),
                               reads=[('ps', b)], writes=[('sgst', gi)], partial=(fc > 0))
                    S.emit('sp', lambda e, gi=gi, tt=tt: e.dma_start(
                        out=sg_d[gi][:, :, tt * 512:(tt + 1) * 512].rearrange("c p n -> p c n"), in_=sgst[gi][:]),
                        reads=[('sgst', gi)], dma=('dsg', gi))
            S.barrier()

        def phase_H():
          with ExitStack() as P:
            def sb(name, shape, dt):
                return P.enter_context(nc.sbuf_tensor(uq(name), list(shape), dt))
            wq = sb("wq", [128, 8, 128], BF16)
            wf = sb("wf", [128, 8, 128], BF16)
            wig = sb("wig", [128, 8, 256], BF16)
            qeT = sb("qeT", [128, S_LEN], BF16)
            kdT = sb("kdT", [128, S_LEN], BF16)
            kd_tok = sb("kd_tok", [128, NT, 128], BF16)
            v_tok = sb("v_tok", [128, NT, 128], BF16)
            sgn = sb("sgn", [128, NT, 128], BF16)
            ebl = sb("ebl", [128, 64], F32)
            lbT8 = sb("lbT8", [8, 128], F32)
            lbp = sb("lbp", [128, 8], F32)
            lbv = sb("lbv", [128, 4], F32)
            omlb = sb("omlb", [128, 4], F32)
            ngA = sb("ngA", [128, 4, 128], F32)
            q32 = [sb("q32_%d" % i, [128, 512], F32) for i in range(2)]
            sg32 = [sb("sg32_%d" % i, [128, 512], F32) for i in range(2)]
            lf32 = [sb("lf32_%d" % i, [128, 512], F32) for i in range(2)]
            kk32 = [sb("kk32_%d" % i, [128, 512], F32) for i in range(2)]
            bc32 = [sb("bc32_%d" % i, [128, 512], F32) for i in range(2)]
            eb32 = [sb("eb32_%d" % i, [128, 512], F32) for i in range(2)]
            en32 = [sb("en32_%d" % i, [128, 512], F32) for i in range(2)]
            sgt = [sb("sgt%d" % i, [128, 128], F32) for i in range(2)]
            st32 = sb("st32", [128, 128], F32)
            tmp32 = sb("tmp32", [128, 128], F32)
            stbf = [sb("stbf%d" % i, [128, 128], BF16) for i in range(2)]
            attm = [sb("attm%d" % i, [128, 128], BF16) for i in range(2)]
            junk = sb("junk", [128, 128], BF16)
            ssA = sb("ssA", [128, 2], F32)
            rsA = sb("rsA", [128, 2], F32)
            oab = [sb("oab%d" % i, [128, 128], BF16) for i in range(2)]
            loadx = make_xT_loader(P)

            S.emit('sp', lambda e: e.dma_start(out=lbT8[:], in_=hg_lb.rearrange("r (h p) -> (r h) p", p=128)),
                   writes=['lbT8'], dma='dsmall0')
            S.emit('pe', lambda e: e.transpose(out=PS[7][:, 0:8], in_=lbT8[:], identity=identf[0:8, 0:8]),
                   reads=['lbT8', 'identf'], writes=[('ps', 7)])
            S.emit('dve', lambda e: e.tensor_copy(out=lbp[:], in_=PS[7][:, 0:8]), writes=['lbp', ('ps', 7)])
            S.emit('dve', lambda e: e.tensor_tensor(out=lbv[:], in0=lbp[:, 0:4], in1=lbp[:, 4:8], op=ALU.subtract),
                   reads=['lbp'], writes=['lbv'])
            S.emit('act', lambda e: e.activation(out=lbv[:], in_=lbv[:], func=AF.Sigmoid), reads=['lbv'], writes=['lbv'])
            S.emit('dve', lambda e: e.tensor_scalar(out=omlb[:], in0=lbv[:], scalar1=-1.0, scalar2=1.0, op0=ALU.mult,
                                                    op1=ALU.add), reads=['lbv'], writes=['omlb'])
            S.emit('sp', lambda e: e.dma_start(out=ngA[:].rearrange("p h n -> p (h n)"), in_=bcast_ap(hg_ng, 512)),
                   writes=['ngA'], dma='dsmall1')

            for h in range(heads if hstop >= 2 else 0):
                load_cast(wq[:], w_in[:, h * 128:(h + 1) * 128].rearrange("(c p) n -> p c n", p=128), (8, 128), 'wq')
                load_cast(wf[:], w_in[:, 512 + h * 128:512 + (h + 1) * 128].rearrange("(c p) n -> p c n", p=128),
                          (8, 128), 'wf')
                load_cast(wig[:, :, 0:128], w_in[:, 1024 + h * 128:1024 + (h + 1) * 128].rearrange("(c p) n -> p c n", p=128),
                          (8, 128), 'wig')
                load_cast(wig[:, :, 128:256], w_in[:, 1536 + h * 128:1536 + (h + 1) * 128].rearrange("(c p) n -> p c n", p=128),
                          (8, 128), 'wig')
                for tt in range(8):
                    xt, xk = loadx(tt)
                    i = tt % 2
                    tsl = slice(tt * 512, (tt + 1) * 512)
                    S.emit('pe', [lambda e, kc=kc, xt=xt: e.matmul(PS[0][:], lhsT=wq[:, kc, :], rhs=xt[:, kc, :],
                                                                  start=(kc == 0), stop=(kc == 7)) for kc in range(8)],
                           reads=[xk, 'wq'], writes=[('ps', 0)])
                    S.emit('pe', [lambda e, kc=kc, xt=xt: e.matmul(PS[1][:], lhsT=wf[:, kc, :], rhs=xt[:, kc, :],
                                                                  start=(kc == 0), stop=(kc == 7)) for kc in range(8)],
                           reads=[xk, 'wf'], writes=[('ps', 1)])
                    S.emit('act', lambda e, i=i: e.activation(out=q32[i][:], in_=PS[0][:], func=AF.Silu),
                           reads=[('ps', 0)], writes=[('q32', i)])
                    S.emit('act', lambda e, i=i: e.activation(out=sg32[i][:], in_=PS[1][:], func=AF.Sigmoid),
                           reads=[('ps', 1)], writes=[('sg32', i)])
                    if cut < 2:
                        continue
                    S.emit('dve', lambda e, i=i, h=h: e.tensor_scalar(out=sg32[i][:], in0=sg32[i][:],
                                                                      scalar1=omlb[:, h:h + 1], scalar2=lbv[:, h:h + 1],
                                                                      op0=ALU.mult, op1=ALU.add),
                           reads=[('sg32', i), 'omlb', 'lbv'], writes=[('sg32', i)])
                    S.emit('act', lambda e, i=i: e.activation(out=lf32[i][:], in_=sg32[i][:], func=AF.Ln),
                           reads=[('sg32', i)], writes=[('lf32', i)])
                    S.emit('pool', lambda e, i=i: e.tensor_scalar(out=kk32[i][:], in0=sg32[i][:], scalar1=-1.0, scalar2=1.0,
                                                                  op0=ALU.mult, op1=ALU.add),
                           reads=[('sg32', i)], writes=[('kk32', i)])
                    if cut < 3:
                        continue
                    S.emit('dve', lambda e, i=i: e.tensor_tensor_scan(out=bc32[i][:], data0=scanmask[:], data1=lf32[i][:],
                                                                      initial=0.0, op0=ALU.mult, op1=ALU.add),
                           reads=[('lf32', i), 'scanmask'], writes=[('bc32', i)])
                    S.emit('act', lambda e, i=i: e.activation(out=eb32[i][:], in_=bc32[i][:], func=AF.Exp),
                           reads=[('bc32', i)], writes=[('eb32', i)])
                    S.emit('act', lambda e, i=i: e.activation(out=en32[i][:], in_=bc32[i][:], func=AF.Exp, scale=-1.0),
                           reads=[('bc32', i)], writes=[('en32', i)])
                    if cut < 4:
                        continue
                    S.emit('dve', lambda e, i=i, tsl=tsl: e.tensor_tensor(out=qeT[:, tsl], in0=q32[i][:], in1=eb32[i][:],
                                                                          op=ALU.mult),
                           reads=[('q32', i), ('eb32', i)], writes=[('qeT', tt)])
                    S.emit('pool', lambda e, i=i, tsl=tsl: e.tensor_tensor(out=kdT[:, tsl], in0=kk32[i][:], in1=en32[i][:],
                                                                           op=ALU.mult),
                           reads=[('kk32', i), ('en32', i)], writes=[('kdT', tt)])
                    S.emit('pool', lambda e, i=i, tt=tt: e.tensor_copy(
                        out=ebl[:, tt * 8:(tt + 1) * 8],
                        in_=eb32[i][:].rearrange("p (c t) -> p c t", t=64)[:, :, 63]),
                        reads=[('eb32', i)], writes=[('ebl', tt)])
                    if cut < 5:
                        continue
                    for s4 in range(4):
                        p = tt * 4 + s4
                        b = 2 + (p % 2)
                        S.emit('pe', [lambda e, kc=kc, xt=xt, s4=s4, b=b: e.matmul(
                            PS[b][:, 0:256], lhsT=xt[:, kc, s4 * 128:(s4 + 1) * 128], rhs=wig[:, kc, :],
                            start=(kc == 0), stop=(kc == 7)) for kc in range(8)],
                            reads=[xk, 'wig'], writes=[('ps', b)])
                        S.emit('dve', lambda e, p=p, b=b: e.tensor_copy(out=v_tok[:, p, :], in_=PS[b][:, 0:128]),
                               writes=[('v_tok', p), ('ps', b)])
                        j = p % 2
                        S.emit('act', lambda e, j=j, b=b: e.activation(out=sgt[j][:], in_=PS[b][:, 128:256], func=AF.Sigmoid),
                               writes=[('sgt', j), ('ps', b)])
                        S.emit('pool', lambda e, j=j, p=p, h=h: e.tensor_tensor(out=sgn[:, p, :], in0=sgt[j][:],
                                                                                in1=ngA[:, h, :], op=ALU.mult),
                               reads=[('sgt', j), 'ngA'], writes=[('sgn', p)])
                    if cut < 6:
                        continue
                    pv = psb(4 + i).rearrange("p (c n) -> p c n", n=128)
                    S.emit('pe', [lambda e, s4=s4, tt=tt, pv=pv: e.transpose(
                        out=pv[:, s4, :], in_=kdT[:, tt * 512 + s4 * 128: tt * 512 + (s4 + 1) * 128], identity=ident[:])
                        for s4 in range(4)],
                        reads=[('kdT', tt), 'ident'], writes=[('ps', 4 + i)])
                    S.emit('dve', lambda e, tt=tt, pv=pv: e.tensor_copy(out=kd_tok[:, tt * 4:(tt + 1) * 4, :], in_=pv[:, 0:4, :]),
                           reads=[('ps', 4 + i)], writes=[('kd_tok', tt)])
                S.emit('dve', lambda e: e.memset(st32[:], 0.0), writes=['st32'])
                S.emit('dve', lambda e: e.memset(stbf[0][:], 0.0), writes=[('stbf', 0)])
                for p in range(NT if hstop >= 3 else 0):
                    tt = p // 4
                    psl = slice(p * 128, (p + 1) * 128)
                    ba = 6 + (p % 2)
                    bo = p % 2
                    A_ps = PS[ba][:, 0:128]
                    O_ps = PS[bo][:, 0:128]
                    S.emit('pe', lambda e, psl=psl, A_ps=A_ps: e.matmul(A_ps, lhsT=kdT[:, psl], rhs=qeT[:, psl],
                                                                       start=True, stop=True),
                           reads=[('kdT', tt), ('qeT', tt)], writes=[('ps', ba)])
                    am = attm[p % 2]
                    S.emit('dve', lambda e, am=am, A_ps=A_ps: e.tensor_tensor(out=am[:], in0=A_ps, in1=tribd[:], op=ALU.mult),
                           reads=['tribd'], writes=[('attm', p % 2), ('ps', ba)])
                    for hf in range(2):
                        c = 2 * p + hf
                        csl = slice(c * 64, (c + 1) * 64)
                        hs = slice(hf * 64, (hf + 1) * 64)
                        sb_cur = stbf[c % 2]
                        sb_nxt = stbf[(c + 1) % 2]
                        S.emit('pe', lambda e, csl=csl, hs=hs, sb_cur=sb_cur, bo=bo: e.matmul(
                            PS[bo][hs, 0:128], lhsT=qeT[:, csl], rhs=sb_cur[:], start=True, stop=False),
                            reads=[('qeT', tt), ('stbf', c % 2)], writes=[('ps', bo)])
                        ub = 4 + (c % 2)
                        S.emit('pe', lambda e, hs=hs, p=p, ub=ub: e.matmul(
                            PS[ub][:, 0:128], lhsT=kd_tok[hs, p, :], rhs=v_tok[hs, p, :], start=True, stop=True),
                            reads=[('kd_tok', tt), ('v_tok', p)], writes=[('ps', ub)])
                        S.emit('dve', lambda e, ub=ub: e.tensor_tensor(out=tmp32[:], in0=PS[ub][:, 0:128], in1=st32[:], op=ALU.add),
                               reads=['st32'], writes=['tmp32', ('ps', ub)])
                        S.emit('dve', lambda e, c=c: e.tensor_scalar(out=st32[:], in0=tmp32[:], scalar1=ebl[:, c:c + 1],
                                                                     scalar2=None, op0=ALU.mult),
                               reads=['tmp32', ('ebl', c // 8)], writes=['st32'])
                        S.emit('act', lambda e, c=c, sb_nxt=sb_nxt: e.activation(out=sb_nxt[:], in_=tmp32[:], func=AF.Copy,
                                                                               scale=ebl[:, c:c + 1]),
                               reads=['tmp32', ('ebl', c // 8)], writes=[('stbf', (c + 1) % 2)])
                    S.emit('pe', lambda e, am=am, p=p, O_ps=O_ps: e.matmul(O_ps, lhsT=am[:], rhs=v_tok[:, p, :],
                                                                         start=False, stop=True),
                           reads=[('attm', p % 2), ('v_tok', p)], writes=[('ps', bo)])
                    j = p % 2
                    S.emit('act', lambda e, O_ps=O_ps, j=j: e.activation(out=junk[:], in_=O_ps, func=AF.Square,
                                                                       accum_out=ssA[:, j:j + 1]),
                           writes=[('ssA', j), 'junk', ('ps', bo)])
                    S.emit('dve', lambda e, j=j: e.tensor_scalar(out=rsA[:, j:j + 1], in0=ssA[:, j:j + 1], scalar1=1.0 / 128,
                                                                 scalar2=NORM_EPS, op0=ALU.mult, op1=ALU.add),
                           reads=[('ssA', j)], writes=[('rsA', j)])
                    S.emit('act', lambda e, j=j: e.activation(out=rsA[:, j:j + 1], in_=rsA[:, j:j + 1], func=AF.Sqrt),
                           reads=[('rsA', j)], writes=[('rsA', j)])
                    S.emit('dve', lambda e, j=j: e.reciprocal(out=rsA[:, j:j + 1], in_=rsA[:, j:j + 1]),
                           reads=[('rsA', j)], writes=[('rsA', j)])
                    S.emit('dve', lambda e, j=j, p=p, O_ps=O_ps: e.scalar_tensor_tensor(
                        out=oab[j][:], in0=O_ps, scalar=rsA[:, j:j + 1], in1=sgn[:, p, :], op0=ALU.mult, op1=ALU.mult),
                        reads=[('rsA', j), ('sgn', p)], writes=[('oab', j), ('ps', bo)])
                    tb = 2 + j
                    tv = psb(tb)[:, 0:128]
                    S.emit('pe', lambda e, j=j, tv=tv: e.transpose(out=tv, in_=oab[j][:], identity=ident[:]),
                           reads=[('oab', j), 'ident'], writes=[('ps', tb)])
                    S.emit('act', lambda e, h=h, psl=psl, tv=tv: e.copy(out=o_aT[:, h, psl], in_=tv),
                           writes=[('o_aT', h, p), ('ps', tb)])
            S.barrier()
            if dbg:
                S.emit('sp', lambda e: e.dma_start(out=dbg_t['oaT'], in_=o_aT[:]), dma='ddbg0')

        if stage >= 1:
            phase_H()

        def phase_D():
          with ExitStack() as P:
            def sb(name, shape, dt):
                return P.enter_context(nc.sbuf_tensor(uq(name), list(shape), dt))
            wdq = sb("wdq", [128, 8, 128], BF16)
            wdk = sb("wdk", [128, 8, 128], BF16)
            wdv = sb("wdv", [128, 8, 128], BF16)
            dqT = sb("dqT", [128, S_LEN], BF16)
            dkT = sb("dkT", [128, S_LEN], BF16)
            dv_ext = sb("dv_ext", [128, NT, 129], BF16)
            ebuf = [sb("ebuf%d" % i, [128, 2, 512], BF16) for i in range(3)]
            lamt = sb("lamt", [128, 256], F32)
            lpr = sb("lpr", [128, 128], F32)
            lsm = sb("lsm", [128, 2], F32)
            neglam = sb("neglam", [128, 1], F32)
            ngB = sb("ngB", [128, 4, 128], F32)
            rz = [sb("rz%d" % i, [128, 2], F32) for i in range(2)]
            t1 = [sb("t1_%d" % i, [128, 128], F32) for i in range(2)]
            o32 = [sb("o32_%d" % i, [128, 128], F32) for i in range(2)]
            junk = sb("junkd", [128, 128], BF16)
            ssB = sb("ssB", [128, 2], F32)
            rsB = sb("rsB", [128, 2], F32)
            obb = [sb("obb%d" % i, [128, 128], BF16) for i in range(2)]
            loadx = make_xT_loader(P)

            S.emit('sp', lambda e: e.dma_start(out=lamt[:], in_=bcast_ap(da_lam, 256)), writes=['lamt'], dma='dsmall0')
            S.emit('sp', lambda e: e.dma_start(out=ngB[:].rearrange("p h n -> p (h n)"), in_=bcast_ap(da_ng, 512)),
                   writes=['ngB'], dma='dsmall1')
            S.emit('dve', lambda e: e.tensor_scalar(out=ngB[:], in0=ngB[:], scalar1=float(1.0 - LAMBDA_INIT), scalar2=None,
                                                    op0=ALU.mult), reads=['ngB'], writes=['ngB'])
            lv = lamt[:].rearrange("p (a n) -> p a n", n=64)
            S.emit('dve', lambda e: e.tensor_tensor(out=lpr[:, 0:64], in0=lv[:, 0, :], in1=lv[:, 1, :], op=ALU.mult),
                   reads=['lamt'], writes=['lpr'])
            S.emit('dve', lambda e: e.tensor_tensor(out=lpr[:, 64:128], in0=lv[:, 2, :], in1=lv[:, 3, :], op=ALU.mult),
                   reads=['lamt', 'lpr'], writes=['lpr'])
            S.emit('dve', lambda e: e.tensor_reduce(out=lsm[:], in_=lpr[:].rearrange("p (a n) -> p a n", n=64),
                                                    axis=mybir.AxisListType.X, op=ALU.add),
                   reads=['lpr'], writes=['lsm'])
            S.emit('act', lambda e: e.activation(out=lsm[:], in_=lsm[:], func=AF.Exp), reads=['lsm'], writes=['lsm'])
            S.emit('dve', lambda e: e.tensor_tensor(out=neglam[:], in0=lsm[:, 1:2], in1=lsm[:, 0:1], op=ALU.subtract),
                   reads=['lsm'], writes=['neglam'])
            S.emit('dve', lambda e: e.tensor_scalar(out=neglam[:], in0=neglam[:], scalar1=float(-LAMBDA_INIT), scalar2=None,
                                                    op0=ALU.add), reads=['neglam'], writes=['neglam'])
            S.emit('pool', lambda e: e.memset(dv_ext[:, :, 128:129], 1.0), writes=['dv_ones'])

            fin_cnt = [0]
            for h in range(heads):
                W = 128 if h == 0 else 512
                load_cast(wdq[:], w_in[:, 2048 + h * 128:2048 + (h + 1) * 128].rearrange("(c p) n -> p c n", p=128), (8, 128), 'wdq')
                load_cast(wdk[:], w_in[:, 2560 + h * 128:2560 + (h + 1) * 128].rearrange("(c p) n -> p c n", p=128), (8, 128), 'wdk')
                load_cast(wdv[:], w_in[:, 3072 + h * 128:3072 + (h + 1) * 128].rearrange("(c p) n -> p c n", p=128), (8, 128), 'wdv')
                for tt in range(8):
                    xt, xk = loadx(tt)
                    tsl = slice(tt * 512, (tt + 1) * 512)
                    S.emit('pe', [lambda e, kc=kc, xt=xt: e.matmul(PS[0][:], lhsT=wdq[:, kc, :], rhs=xt[:, kc, :],
                                                                  start=(kc == 0), stop=(kc == 7)) for kc in range(8)],
                           reads=[xk, 'wdq'], writes=[('ps', 0)])
                    S.emit('pe', [lambda e, kc=kc, xt=xt: e.matmul(PS[1][:], lhsT=wdk[:, kc, :], rhs=xt[:, kc, :],
                                                                  start=(kc == 0), stop=(kc == 7)) for kc in range(8)],
                           reads=[xk, 'wdk'], writes=[('ps', 1)])
                    S.emit('act', lambda e, tsl=tsl: e.activation(out=dqT[:, tsl], in_=PS[0][:], func=AF.Copy, scale=0.125),
                           writes=[('dqT', tt), ('ps', 0)])
                    S.emit('dve', lambda e, tsl=tsl: e.tensor_copy(out=dkT[:, tsl], in_=PS[1][:]),
                           writes=[('dkT', tt), ('ps', 1)])
                    for s4 in range(4):
                        p = tt * 4 + s4
                        b = 2 + (p % 2)
                        S.emit('pe', [lambda e, kc=kc, xt=xt, s4=s4, b=b: e.matmul(
                            PS[b][:, 0:128], lhsT=xt[:, kc, s4 * 128:(s4 + 1) * 128], rhs=wdv[:, kc, :],
                            start=(kc == 0), stop=(kc == 7)) for kc in range(8)],
                            reads=[xk, 'wdv'], writes=[('ps', b)])
                        S.emit('dve', lambda e, p=p, b=b: e.tensor_copy(out=dv_ext[:, p, 0:128], in_=PS[b][:, 0:128]),
                               reads=['dv_ones'], writes=[('dv', p), ('ps', b)])
                ecnt = 0
                for qt in range(8):
                    started = {}
                    nk = 4 * qt + 4
                    for kt in range(nk):
                        st2 = kt % 2
                        sbk = [2 * st2, 2 * st2 + 1]
                        i0 = max(0, kt - 4 * qt)
                        c0 = i0 * 128
                        eb = ebuf[ecnt % 3]
                        ek = ('ebuf', ecnt % 3)
                        ecnt += 1
                        for j in range(2):
                            js = slice(j * 64, (j + 1) * 64)
                            S.emit('pe', lambda e, j=j, js=js, kt=kt, qt=qt, c0=c0, sbk=sbk: e.matmul(
                                PS[sbk[j]][:, c0:512], lhsT=dkT[js, kt * 128:(kt + 1) * 128],
                                rhs=dqT[js, qt * 512 + c0:(qt + 1) * 512], start=True, stop=True),
                                reads=[('dkT', kt // 4), ('dqT', qt)], writes=[('ps', sbk[j])])
                        for j in range(2):
                            fns = []
                            if W == 512:
                                col = (4 * qt - kt) + 3
                                fns.append(lambda e, j=j, c0=c0, col=col, sbk=sbk, eb=eb, h=h: e.activation(
                                    out=eb[:, j, c0:512], in_=PS[sbk[j]][:, c0:512], func=AF.Exp,
                                    bias=btab[:, h, col:col + 1], scale=1.0))
                            else:
                                for i in range(i0, 4):
                                    col = (4 * qt + i - kt) + 3
                                    fns.append(lambda e, j=j, i=i, col=col, sbk=sbk, eb=eb, h=h: e.activation(
                                        out=eb[:, j, i * 128:(i + 1) * 128], in_=PS[sbk[j]][:, i * 128:(i + 1) * 128],
                                        func=AF.Exp, bias=btab[:, h, col:col + 1], scale=1.0))
                            S.emit('act', fns, reads=[('btab', h)], writes=[ek, ('ps', sbk[j])], partial=(j > 0))
                        if kt >= 4 * qt:
                            S.emit('dve', [lambda e, j=j, i0=i0, eb=eb: e.tensor_tensor(
                                out=eb[:, j, i0 * 128:(i0 + 1) * 128], in0=eb[:, j, i0 * 128:(i0 + 1) * 128], in1=tri[:],
                                op=ALU.mult) for j in range(2)],
                                reads=['tri', ek], writes=[ek])
                        fns = []
                        banks = set()
                        for i in range(i0, 4):
                            for j in range(2):
                                idx = i * 2 + j
                                bO = 4 + idx // 3
                                cc = (idx % 3) * 129
                                stt = bO not in started
                                started[bO] = True
                                banks.add(bO)
                                fns.append(lambda e, i=i, j=j, bO=bO, cc=cc, stt=stt, kt=kt, qt=qt, eb=eb: e.matmul(
                                    PS[bO][:, cc:cc + 129], lhsT=eb[:, j, i * 128:(i + 1) * 128], rhs=dv_ext[:, kt, :],
                                    start=stt, stop=(kt == 4 * qt + i), skip_group_check=True))
                        S.emit('pe', fns, reads=[ek, ('dv', kt), 'dv_ones'], writes=[('ps', b_) for b_ in sorted(banks)])
                    for i in range(4):
                        f2 = fin_cnt[0] % 2
                        fin_cnt[0] += 1
                        qs = qt * 4 + i
                        regs = []
                        for j in range(2):
                            idx = i * 2 + j
                            regs.append((4 + idx // 3, (idx % 3) * 129))
                        (b1, c1), (b2, c2) = regs
                        S.emit('dve', lambda e, f2=f2, b1=b1, c1=c1: e.reciprocal(out=rz[f2][:, 0:1], in_=PS[b1][:, c1 + 128:c1 + 129]),
                               writes=[('rz', f2), ('ps', b1)])
                        S.emit('dve', lambda e, f2=f2, b2=b2, c2=c2: e.reciprocal(out=rz[f2][:, 1:2], in_=PS[b2][:, c2 + 128:c2 + 129]),
                               reads=[('rz', f2)], writes=[('rz', f2), ('ps', b2)])
                        S.emit('dve', lambda e, f2=f2: e.tensor_tensor(out=rz[f2][:, 1:2], in0=rz[f2][:, 1:2], in1=neglam[:], op=ALU.mult),
                               reads=[('rz', f2), 'neglam'], writes=[('rz', f2)])
                        S.emit('dve', lambda e, f2=f2, b1=b1, c1=c1: e.tensor_scalar(out=t1[f2][:], in0=PS[b1][:, c1:c1 + 128],
                                                                                    scalar1=rz[f2][:, 0:1], scalar2=None, op0=ALU.mult),
                               reads=[('rz', f2)], writes=[('t1', f2), ('ps', b1)])
                        S.emit('dve', lambda e, f2=f2, b2=b2, c2=c2: e.scalar_tensor_tensor(
                            out=o32[f2][:], in0=PS[b2][:, c2:c2 + 128], scalar=rz[f2][:, 1:2], in1=t1[f2][:], op0=ALU.mult, op1=ALU.add),
                            reads=[('rz', f2), ('t1', f2)], writes=[('o32', f2), ('ps', b2)])
                        S.emit('act', lambda e, f2=f2: e.activation(out=junk[:], in_=o32[f2][:], func=AF.Square,
                                                                  accum_out=ssB[:, f2:f2 + 1]),
                               reads=[('o32', f2)], writes=[('ssB', f2), 'junkd'])
                        S.emit('dve', lambda e, f2=f2: e.tensor_scalar(out=rsB[:, f2:f2 + 1], in0=ssB[:, f2:f2 + 1], scalar1=1.0 / 128,
                                                                     scalar2=NORM_EPS, op0=ALU.mult, op1=ALU.add),
                               reads=[('ssB', f2)], writes=[('rsB', f2)])
                        S.emit('act', lambda e, f2=f2: e.activation(out=rsB[:, f2:f2 + 1], in_=rsB[:, f2:f2 + 1], func=AF.Sqrt),
                               reads=[('rsB', f2)], writes=[('rsB', f2)])
                        S.emit('dve', lambda e, f2=f2: e.reciprocal(out=rsB[:, f2:f2 + 1], in_=rsB[:, f2:f2 + 1]),
                               reads=[('rsB', f2)], writes=[('rsB', f2)])
                        S.emit('dve', lambda e, f2=f2, h=h: e.scalar_tensor_tensor(
                            out=obb[f2][:], in0=o32[f2][:], scalar=rsB[:, f2:f2 + 1], in1=ngB[:, h, :], op0=ALU.mult, op1=ALU.mult),
                            reads=[('o32', f2), ('rsB', f2), 'ngB'], writes=[('obb', f2)])
                        tv = psb(7)[:, 0:128]
                        S.emit('pe', lambda e, f2=f2, tv=tv: e.transpose(out=tv, in_=obb[f2][:], identity=ident[:]),
                               reads=[('obb', f2), 'ident'], writes=[('ps', 7)])
                        S.emit('act', lambda e, h=h, qs=qs, tv=tv: e.copy(out=o_bT[:, h, qs * 128:(qs + 1) * 128], in_=tv),
                               writes=[('o_bT', h, qs), ('ps', 7)])
            S.barrier()
            if dbg:
                S.emit('sp', lambda e: e.dma_start(out=dbg_t['obT'], in_=o_bT[:]), dma='ddbg1')

        if stage >= 2:
            phase_D()

        def layer_norm(tiles, src, src_key, dst, dst_key, gt, g_key, bt, b_key, tag):
            bnst, mv, rstd, ytmp = tiles
            yk = dst_key if ytmp is dst else tag + 'ytmp'
            S.emit('dve', [lambda e, k=k: e.bn_stats(out=bnst[:, k, :], in_=src[:, k * 512:(k + 1) * 512]) for k in range(2)],
                   reads=[src_key], writes=[tag + 'bnst'])
            S.emit('dve', lambda e: e.bn_aggr(out=mv[:], in_=bnst[:].rearrange("p a b -> p (a b)")),
                   reads=[tag + 'bnst'], writes=[tag + 'mv'])
            S.emit('dve', lambda e: e.tensor_scalar(out=rstd[:], in0=mv[:, 1:2], scalar1=LN_EPS, scalar2=None, op0=ALU.add),
                   reads=[tag + 'mv'], writes=[tag + 'rstd'])
            S.emit('act', lambda e: e.activation(out=rstd[:], in_=rstd[:], func=AF.Sqrt), reads=[tag + 'rstd'], writes=[tag + 'rstd'])
            S.emit('dve', lambda e: e.reciprocal(out=rstd[:], in_=rstd[:]), reads=[tag + 'rstd'], writes=[tag + 'rstd'])
            S.emit('dve', lambda e: e.tensor_scalar(out=ytmp[:], in0=src[:], scalar1=mv[:, 0:1], scalar2=rstd[:],
                                                    op0=ALU.subtract, op1=ALU.mult),
                   reads=[src_key, tag + 'mv', tag + 'rstd'], writes=[yk])
            S.emit('pool', lambda e: e.tensor_tensor(out=ytmp[:], in0=ytmp[:], in1=gt[:], op=ALU.mult),
                   reads=[yk, g_key], writes=[yk])
            S.emit('pool', lambda e: e.tensor_tensor(out=dst[:], in0=ytmp[:], in1=bt[:], op=ALU.add),
                   reads=[yk, b_key], writes=[dst_key])

        def phase_M():
          with ExitStack() as P:
            def sb(name, shape, dt):
                return P.enter_context(nc.sbuf_tensor(uq(name), list(shape), dt))
            wa = sb("wa", [128, 4, 1024], BF16)
            wb_ = sb("wb", [128, 4, 1024], BF16)
            wo = sb("wo", [128, 8, 1024], BF16)
            rw = sb("rw", [128, 8, NE], F32)
            rbt = sb("rbt", [128, NE], F32)
            g1 = sb("g1", [128, D], F32)
            b1 = sb("b1", [128, D], F32)
            sga = [sb("sga0", [128, 8, 512], BF16)] * 2
            sgb = [sb("sgb0", [128, 8, 512], BF16)] * 2
            mT = [sb("mT0", [128, 8, 512], BF16)] * 2
            tA = [sb("tA%d" % i, [128, 512], F32) for i in range(2)]
            tB = [sb("tB%d" % i, [128, 512], F32) for i in range(2)]
            xres = [sb("xres%d" % i, [128, D], F32) for i in range(2)]
            rr = [sb("rr%d" % i, [128, D], F32) for i in range(2)]
            h1t = [sb("h1t%d" % i, [128, D], F32) for i in range(2)]
            bnst = sb("bnst", [128, 2, 6], F32)
            mv = sb("mv", [128, 2], F32)
            rstd = sb("rstd", [128, 1], F32)
            ytmp = sb("ytmp", [128, D], F32)
            h1T32 = sb("h1T32", [128, 8, 128], F32)
            h1Tb = [sb("h1Tb%d" % i, [128, 8, 128], BF16) for i in range(2)]
            lg = sb("lg", [128, NE], F32)
            top8 = sb("top8", [128, 8], F32)
            msk = sb("msk", [128, NE], F32)
            ex = sb("ex", [128, NE], F32)
            nm = sb("nm", [128, 1], F32)
            zs = sb("zs", [128, 1], F32)

            load_cast(wa[:], w_a.rearrange("(c p) n -> p c n", p=128), (4, 1024), 'wa')
            load_cast(wb_[:], w_b.rearrange("(c p) n -> p c n", p=128), (4, 1024), 'wb')
            load_cast(wo[:, 0:4, :], w_out[0:512, :].rearrange("(c p) n -> p c n", p=128), (4, 1024), ('wo', 0))
            load_cast(wo[:, 4:8, :], w_out[512:1024, :].rearrange("(c p) n -> p c n", p=128), (4, 1024), ('wo', 1))
            S.emit('sp', lambda e: e.dma_start(out=rw[:], in_=router_w.rearrange("(c p) n -> p c n", p=128)), writes=['rw'], dma='dsmall0')
            S.emit('sp', lambda e: e.dma_start(out=rbt[:], in_=bcast_ap(router_b, NE)), writes=['rbt'], dma='dsmall1')
            S.emit('sp', lambda e: e.dma_start(out=g1[:], in_=bcast_ap(ln1_g, D)), writes=['l1g'], dma='dsmall2')
            S.emit('sp', lambda e: e.dma_start(out=b1[:], in_=bcast_ap(ln1_b, D)), writes=['l1b'], dma='dsmall3')
            for tt in range(8):
                i2 = 0
                tsl = slice(tt * 512, (tt + 1) * 512)
                S.emit('sp', lambda e, i2=i2, tsl=tsl: e.dma_start(out=sga[i2][:], in_=sg_d[0][:, :, tsl].rearrange("c p n -> p c n")),
                       writes=[('sga', i2)], dma=('dsga', i2))
                S.emit('sp', lambda e, i2=i2, tsl=tsl: e.dma_start(out=sgb[i2][:], in_=sg_d[1][:, :, tsl].rearrange("c p n -> p c n")),
                       writes=[('sgb', i2)], dma=('dsgb', i2))
                for fc in range(8):
                    k2 = fc % 2
                    fsl = slice(fc * 128, (fc + 1) * 128)
                    S.emit('pe', [lambda e, kc=kc, fsl=fsl, tsl=tsl: e.matmul(PS[0][:], lhsT=wa[:, kc, fsl], rhs=o_aT[:, kc, tsl],
                                                                             start=(kc == 0), stop=(kc == 3)) for kc in range(4)],
                           reads=['wa'], writes=[('ps', 0)])
                    S.emit('pe', [lambda e, kc=kc, fsl=fsl, tsl=tsl: e.matmul(PS[1][:], lhsT=wb_[:, kc, fsl], rhs=o_bT[:, kc, tsl],
                                                                             start=(kc == 0), stop=(kc == 3)) for kc in range(4)],
                           reads=['wb'], writes=[('ps', 1)])
                    S.emit('dve', lambda e, k2=k2, i2=i2, fc=fc: e.tensor_tensor(out=tA[k2][:], in0=PS[0][:], in1=sga[i2][:, fc, :], op=ALU.mult),
                           reads=[('sga', i2)], writes=[('tA', k2), ('ps', 0)])
                    S.emit('dve', lambda e, k2=k2, i2=i2, fc=fc: e.tensor_tensor(out=tB[k2][:], in0=PS[1][:], in1=sgb[i2][:, fc, :], op=ALU.mult),
                           reads=[('sgb', i2)], writes=[('tB', k2), ('ps', 1)])
                    S.emit('pool', lambda e, k2=k2, i2=i2, fc=fc: e.tensor_tensor(out=mT[i2][:, fc, :], in0=tA[k2][:], in1=tB[k2][:], op=ALU.add),
                           reads=[('tA', k2), ('tB', k2)], writes=[('mT', i2)], partial=(fc > 0))
                for s4 in range(4):
                    p = tt * 4 + s4
                    j = p % 2
                    S.emit('sp', lambda e, p=p, j=j: e.dma_start(out=xres[j][:], in_=x[p * 128:(p + 1) * 128, :]),
                           writes=[('xres', j)], dma=('dxres', j))
                    for hf in range(2):
                        S.emit('pe', [lambda e, kc=kc, i2=i2, s4=s4, hf=hf: e.matmul(
                            PS[2 + hf][:], lhsT=mT[i2][:, kc, s4 * 128:(s4 + 1) * 128], rhs=wo[:, kc, hf * 512:(hf + 1) * 512],
                            start=(kc == 0), stop=(kc == 7)) for kc in range(8)],
                            reads=[('mT', i2), ('wo', 0), ('wo', 1)], writes=[('ps', 2 + hf)])
                        S.emit('dve', lambda e, j=j, hf=hf: e.scalar_tensor_tensor(
                            out=rr[j][:, hf * 512:(hf + 1) * 512], in0=xres[j][:, hf * 512:(hf + 1) * 512], scalar=float(DN_ALPHA),
                            in1=PS[2 + hf][:], op0=ALU.mult, op1=ALU.add),
                            reads=[('xres', j)], writes=[('rr', j), ('ps', 2 + hf)], partial=(hf > 0))
                    layer_norm((bnst, mv, rstd, ytmp), rr[j], ('rr', j), h1t[j], ('h1t', j), g1, 'l1g', b1, 'l1b', 'm_')
                    S.emit('sp', lambda e, p=p, j=j: e.dma_start(out=h1_d[p * 128:(p + 1) * 128, :], in_=h1t[j][:]),
                           reads=[('h1t', j)], dma=('dh1', j))
                    for g in range(2):
                        S.emit('pe', [lambda e, c=c, j=j, g=g: e.transpose(out=PS[4 + g][:, (c % 4) * 128:(c % 4 + 1) * 128],
                                                                          in_=h1t[j][:, c * 128:(c + 1) * 128], identity=identf[:])
                                      for c in range(4 * g, 4 * g + 4)],
                               reads=[('h1t', j), 'identf'], writes=[('ps', 4 + g)])
                        S.emit('act', lambda e, j=j, g=g: e.copy(out=h1Tb[j][:, 4 * g:4 * g + 4, :],
                                                                 in_=PS[4 + g][:].rearrange("p (c n) -> p c n", n=128)),
                               writes=[('h1Tb', j), ('ps', 4 + g)], partial=(g > 0))
                        S.emit('dve', lambda e, g=g: e.tensor_copy(out=h1T32[:, 4 * g:4 * g + 4, :],
                                                                   in_=PS[4 + g][:].rearrange("p (c n) -> p c n", n=128)),
                               writes=['h1T32', ('ps', 4 + g)], partial=(g > 0))
                    S.emit('sp', lambda e, p=p, j=j: e.dma_start(
                        out=h1T_d[:, :, p * 128:(p + 1) * 128].rearrange("c p n -> p c n"), in_=h1Tb[j][:]),
                        reads=[('h1Tb', j)], dma=('dh1T', j))
                    S.emit('pe', [lambda e, c=c: e.matmul(PS[6][:, 0:NE], lhsT=h1T32[:, c, :], rhs=rw[:, c, :],
                                                          start=(c == 0), stop=(c == 7)) for c in range(8)],
                           reads=['h1T32', 'rw'], writes=[('ps', 6)])
                    S.emit('dve', lambda e: e.tensor_tensor(out=lg[:], in0=PS[6][:, 0:NE], in1=rbt[:], op=ALU.add),
                           reads=['rbt'], writes=['lg', ('ps', 6)])
                    S.emit('dve', lambda e: e.max(out=top8[:], in_=lg[:]), reads=['lg'], writes=['top8'])
                    S.emit('dve', lambda e: e.tensor_scalar(out=msk[:], in0=lg[:], scalar1=top8[:, 3:4], scalar2=None, op0=ALU.is_ge),
                           reads=['lg', 'top8'], writes=['msk'])
                    S.emit('dve', lambda e: e.tensor_scalar(out=nm[:], in0=top8[:, 0:1], scalar1=-1.0, scalar2=None, op0=ALU.mult),
                           reads=['top8'], writes=['nm'])
                    S.emit('act', lambda e: e.activation(out=ex[:], in_=lg[:], func=AF.Exp, bias=nm[:], scale=1.0),
                           reads=['lg', 'nm'], writes=['ex'])
                    S.emit('dve', lambda e: e.tensor_tensor(out=ex[:], in0=ex[:], in1=msk[:], op=ALU.mult),
                           reads=['ex', 'msk'], writes=['ex'])
                    S.emit('dve', lambda e: e.tensor_reduce(out=zs[:], in_=ex[:], axis=mybir.AxisListType.X, op=ALU.add),
                           reads=['ex'], writes=['zs'])
                    S.emit('dve', lambda e: e.reciprocal(out=zs[:], in_=zs[:]), reads=['zs'], writes=['zs'])
                    S.emit('dve', lambda e, p=p: e.tensor_scalar(out=comb[:, p, :], in0=ex[:], scalar1=zs[:], scalar2=None, op0=ALU.mult),
                           reads=['ex', 'zs'], writes=[('comb', p)])
            S.barrier()
            if dbg:
                S.emit('sp', lambda e: e.dma_start(out=dbg_t['h1'], in_=h1_d), dma='ddbg2')
                S.emit('sp', lambda e: e.dma_start(out=dbg_t['comb'], in_=comb[:]), dma='ddbg3')
                S.barrier()

        if stage >= 3:
            phase_M()
        MIX.close()

        def phase_E():
          with ExitStack() as P:
            def sb(name, shape, dt):
                return P.enter_context(nc.sbuf_tensor(uq(name), list(shape), dt))
            h1T = sb("h1T", [128, 8, PASS_TOK], BF16)
            acc = sb("acc", [128, 8, D], F32)
            wguh = [sb("wguh%d" % i, [128, 8, 2, 512], BF16) for i in range(3)]
            wdh = [sb("wdh%d" % i, [128, 4, D], BF16) for i in range(3)]
            est = [sb("est%d" % i, [128, 2048], F32) for i in range(3)]
            bias_all = sb("bias_all", [128, 2, 8, NE], F32)
            bd = sb("bd", [NE, D], F32)
            combT = [sb("combT%d" % i, [NE, 128], F32) for i in range(2)]
            g_t = [sb("g_t%d" % i, [128, 512], F32) for i in range(2)]
            s_t = [sb("s_t%d" % i, [128, 512], F32) for i in range(2)]
            u_t = [sb("u_t%d" % i, [128, 512], F32) for i in range(2)]
            actT = [sb("actT%d" % i, [128, 4, 512], BF16) for i in range(2)]
            comb_s = sb("comb_s", [128, NT, NE], F32)
            h1r = sb("h1r", [128, D], F32)
            r2 = sb("r2", [128, D], F32)
            yout = sb("yout", [128, D], F32)
            g2 = sb("g2", [128, D], F32)
            b2 = sb("b2", [128, D], F32)
            bnst = sb("bnst2", [128, 2, 6], F32)
            mv = sb("mv2", [128, 2], F32)
            rstd = sb("rstd2", [128, 1], F32)
            bgu32 = est[2][0:NE, :]

            S.emit('sp', lambda e: e.dma_start(out=bgu32, in_=b_gu), writes=[('est', 2)], dma='dsmall0')
            S.emit('sp', lambda e: e.dma_start(out=bd[:], in_=b_dn), writes=['bd'], dma='dsmall1')
            S.emit('sp', lambda e: e.dma_start(out=g2[:], in_=bcast_ap(ln2_g, D)), writes=['l2g'], dma='dsmall2')
            S.emit('sp', lambda e: e.dma_start(out=b2[:], in_=bcast_ap(ln2_b, D)), writes=['l2b'], dma='dsmall3')
            S.emit('pe', [lambda e, g=g, fc=fc: e.transpose(
                out=PS[7][:, (g * 8 + fc) * NE:(g * 8 + fc + 1) * NE],
                in_=est[2][0:NE, fc * 256 + g:(fc + 1) * 256:2], identity=identf[0:NE, 0:NE])
                for g in range(2) for fc in range(8)],
                reads=[('est', 2), 'identf'], writes=[('ps', 7)])
            S.emit('dve', lambda e: e.tensor_copy(out=bias_all[:].rearrange("p a b c -> p (a b c)"), in_=PS[7][:]),
                   writes=['bias_all', ('ps', 7)])
            S.emit('dve', lambda e: e.tensor_scalar(out=bias_all[:, 1, :, :], in0=bias_all[:, 1, :, :], scalar1=1.0, scalar2=None,
                                                    op0=ALU.add), reads=['bias_all'], writes=['bias_all'])

            S.emit('dve', lambda e: e.tensor_scalar(out=comb_s[:], in0=comb[:], scalar1=float(1.0 / 1.702), scalar2=None, op0=ALU.mult),
                   writes=['comb_s'])
            jobs = []
            ptr = {'d': 0, 'c': 0}

            def make_jobs(e_, hfu, slot):
                for q in range(4):
                    jobs.append(('gu', e_, hfu, slot, q))
                for q in range(2):
                    jobs.append(('dn', e_, hfu, slot, q))

            def emit_dma(k):
                kind, e_, hfu, slot, q = jobs[k]
                i = k % 3
                if kind == 'gu':
                    src = w_gu[e_, q * 256:(q + 1) * 256, hfu * 1024:(hfu + 1) * 1024].rearrange("(c p) n -> p c n", p=128)
                else:
                    src = w_dn[e_, hfu * 512 + q * 256:hfu * 512 + (q + 1) * 256, :].rearrange("(c p) n -> p c n", p=128)
                S.emit('sp', lambda e, i=i, src=src: e.dma_start(out=est[i][:].rearrange("p (c n) -> p c n", n=1024), in_=src),
                       writes=[('est', i)], dma=('dest', i))

            def emit_cast(k):
                kind, e_, hfu, slot, q = jobs[k]
                i = k % 3
                if kind == 'gu':
                    for c in range(2):
                        kc = q * 2 + c
                        src = est[i][:, c * 1024:(c + 1) * 1024].rearrange("p (f g) -> p g f", g=2)
                        S.emit('act', lambda e, kc=kc, src=src, slot=slot: e.copy(out=wguh[slot][:, kc, :, :], in_=src),
                               reads=[('est', i)], writes=[('wguh', slot)], partial=not (q == 0 and c == 0))
                else:
                    src = est[i][:].rearrange("p (c n) -> p c n", n=1024)
                    S.emit('act', lambda e, q=q, src=src, slot=slot: e.copy(out=wdh[slot][:, 2 * q:2 * q + 2, :], in_=src),
                           reads=[('est', i)], writes=[('wdh', slot)], partial=(q > 0))

            def pump(n):
                for _ in range(n):
                    if ptr['c'] < len(jobs) and ptr['c'] < ptr['d']:
                        emit_cast(ptr['c'])
                        ptr['c'] += 1
                    while ptr['d'] < min(len(jobs), ptr['c'] + 3):
                        emit_dma(ptr['d'])
                        ptr['d'] += 1

            gcnt = [0]
            ycnt = [0]

            def emit_gu(ps_, e_, hfu, slot, tile, ai):
                tl = slice(tile * 512, (tile + 1) * 512)

                def finish(fcl, k):
                    S.emit('dve', lambda e, k=k, fcl=fcl: e.scalar_tensor_tensor(
                        out=actT[ai][:, fcl, :], in0=u_t[k][:], scalar=-6.0, in1=s_t[k][:], op0=ALU.max, op1=ALU.mult),
                        reads=[('u_t', k), ('s_t', k)], writes=[('actT', ai)], partial=(fcl > 0))
                pend = None
                for fcl in range(4):
                    fc = hfu * 4 + fcl
                    k = gcnt[0] % 2
                    gcnt[0] += 1
                    bg, bu = 2 * k, 2 * k + 1
                    fs = slice(fcl * 128, (fcl + 1) * 128)
                    S.emit('pe', [lambda e, kc=kc, fs=fs, tl=tl, bg=bg: e.matmul(
                        PS[bg][:], lhsT=wguh[slot][:, kc, 0, fs], rhs=h1T[:, kc, tl], start=(kc == 0), stop=(kc == 7))
                        for kc in range(8)], reads=[('wguh', slot), 'h1T'], writes=[('ps', bg)])
                    S.emit('pe', [lambda e, kc=kc, fs=fs, tl=tl, bu=bu: e.matmul(
                        PS[bu][:], lhsT=wguh[slot][:, kc, 1, fs], rhs=h1T[:, kc, tl], start=(kc == 0), stop=(kc == 7))
                        for kc in range(8)], reads=[('wguh', slot), 'h1T'], writes=[('ps', bu)])
                    S.emit('dve', lambda e, k=k, bg=bg, fc=fc: e.tensor_scalar(
                        out=g_t[k][:], in0=PS[bg][:], scalar1=bias_all[:, 0, fc, e_:e_ + 1], scalar2=7.0, op0=ALU.add, op1=ALU.min),
                        reads=['bias_all'], writes=[('g_t', k), ('ps', bg)])
                    S.emit('act', lambda e, k=k: e.activation(out=s_t[k][:], in_=g_t[k][:], func=AF.Silu, scale=1.702),
                           reads=[('g_t', k)], writes=[('s_t', k)])
                    S.emit('dve', lambda e, k=k, bu=bu, fc=fc: e.tensor_scalar(
                        out=u_t[k][:], in0=PS[bu][:], scalar1=bias_all[:, 1, fc, e_:e_ + 1], scalar2=8.0, op0=ALU.add, op1=ALU.min),
                        reads=['bias_all'], writes=[('u_t', k), ('ps', bu)])
                    if pend is not None:
                        finish(*pend)
                    pend = (fcl, k)
                    if tile == 0 or fcl < 2:
                        pump(1)
                finish(*pend)

            def emit_down(ps_, e_, hfu, slot, tile, ai):
                for s4 in range(4):
                    st = tile * 4 + s4
                    p = ps_ * 8 + st
                    for d2 in range(2):
                        by = 4 + (ycnt[0] % 2)
                        ycnt[0] += 1
                        ds = slice(d2 * 512, (d2 + 1) * 512)
                        S.emit('pe', [lambda e, fcl=fcl, s4=s4, ds=ds, by=by: e.matmul(
                            PS[by][:], lhsT=actT[ai][:, fcl, s4 * 128:(s4 + 1) * 128], rhs=wdh[slot][:, fcl, ds],
                            start=(fcl == 0), stop=(fcl == 3)) for fcl in range(4)],
                            reads=[('actT', ai), ('wdh', slot)], writes=[('ps', by)])
                        S.emit('dve', lambda e, st=st, p=p, ds=ds, by=by: e.scalar_tensor_tensor(
                            out=acc[:, st, ds], in0=PS[by][:], scalar=comb_s[:, p, e_:e_ + 1], in1=acc[:, st, ds],
                            op0=ALU.mult, op1=ALU.add),
                            reads=[('acc', st, d2)], writes=[('acc', st, d2), ('ps', by)])

            units = [(ps_, e_, hfu) for ps_ in range(NPASS) for e_ in range(NE) for hfu in range(2)]
            if stage == 4 and ne_decl < NE:
                units = [(ps_, e_, hfu) for ps_ in range(NPASS) for e_ in range(ne_decl) for hfu in range(2)]
            nun = len(units)
            per_pass = nun // NPASS
            for u0 in range(min(2, nun)):
                make_jobs(units[u0][1], units[u0][2], u0 % 3)
            pump(len(jobs) + 1)
            prev = None
            tcount = 0
            for u, (ps_, e_, hfu) in enumerate(units):
                slot = u % 3
                if u % per_pass == 0:
                    tok0 = ps_ * PASS_TOK
                    S.emit('sp', lambda e, tok0=tok0: e.dma_start(
                        out=h1T[:], in_=h1T_d[:, :, tok0:tok0 + PASS_TOK].rearrange("c p n -> p c n")),
                        writes=['h1T'], dma='dh1Tp')
                    for st in range(8):
                        p = ps_ * 8 + st
                        ci = st % 2
                        S.emit('pe', lambda e, p=p: e.transpose(out=PS[7][0:NE, 0:128], in_=comb[:, p, :], identity=identf[:]),
                               reads=['identf'], writes=[('ps', 7)])
                        S.emit('dve', lambda e, ci=ci: e.tensor_copy(out=combT[ci][:], in_=PS[7][0:NE, 0:128]),
                               writes=[('combT', ci), ('ps', 7)])
                        for d2 in range(2):
                            ds = slice(d2 * 512, (d2 + 1) * 512)
                            S.emit('pe', lambda e, ci=ci, ds=ds: e.matmul(PS[6][:], lhsT=combT[ci][:], rhs=bd[:, ds], start=True, stop=True),
                                   reads=[('combT', ci), 'bd'], writes=[('ps', 6)])
                            S.emit('act', lambda e, st=st, ds=ds: e.copy(out=acc[:, st, ds], in_=PS[6][:]),
                                   writes=[('acc', st, d2), ('ps', 6)])
                if u + 2 < nun:
                    make_jobs(units[u + 2][1], units[u + 2][2], (u + 2) % 3)
                for tile in range(2):
                    ai = tcount % 2
                    tcount += 1
                    emit_gu(ps_, e_, hfu, slot, tile, ai)
                    if prev is not None:
                        emit_down(*prev)
                    prev = (ps_, e_, hfu, slot, tile, ai)
                if (u + 1) % per_pass == 0:
                    emit_down(*prev)
                    prev = None
                    for st in range(8):
                        p = ps_ * 8 + st
                        S.emit('sp', lambda e, p=p: e.dma_start(out=h1r[:], in_=h1_d[p * 128:(p + 1) * 128, :]),
                               writes=['h1r'], dma='dh1r')
                        S.emit('dve', lambda e, st=st: e.scalar_tensor_tensor(
                            out=r2[:], in0=h1r[:], scalar=float(DN_ALPHA), in1=acc[:, st, :], op0=ALU.mult, op1=ALU.add),
                            reads=['h1r', ('acc', st, 0), ('acc', st, 1)], writes=['r2'])
                        layer_norm((bnst, mv, rstd, yout), r2, 'r2', yout, 'yout', g2, 'l2g', b2, 'l2b', 'f_')
                        S.emit('sp', lambda e, p=p: e.dma_start(out=y[p * 128:(p + 1) * 128, :], in_=yout[:]),
                               reads=['yout'], writes=['yout_dma'], dma='dyout')
            S.barrier()

        if stage >= 4:
            phase_E()

        S.barrier()
        S.run()
    return nc


_NC_CACHE = {}


def _get_nc():
    if 'nc' not in _NC_CACHE:
        _NC_CACHE['nc'] = build()
    return _NC_CACHE['nc']


def kernel(**inputs):
    nc = _get_nc()
    n = 8
    names = ["w_in", "hg_lb_logits", "hg_norm_g", "da_lambda", "da_norm_g", "w_branch_a", "w_branch_b", "w_out",
             "ln1_g", "ln1_b", "router_w", "router_b", "w_gate_up", "b_gate_up", "w_down", "b_down", "ln2_g", "ln2_b"]
    shared = {}
    for k in names:
        a = np.ascontiguousarray(np.asarray(inputs[k], dtype=np.float32))
        shared[k] = a[0] if k != "hg_lb_logits" else a
    xx = np.asarray(inputs["x"], dtype=np.float32)
    in_maps = []
    for c in range(n):
        m = dict(shared)
        m["x"] = np.ascontiguousarray(xx[c])
        in_maps.append(m)
    res = run_bass_kernel_spmd(nc, in_maps, core_ids=list(range(n)))
    return np.stack([np.asarray(r["y"]) for r in res.results], axis=0).astype(np.float32)
```

```python
import math
from contextlib import ExitStack

import numpy as np
import concourse.bass as bass
import concourse.mybir as mybir
from concourse.bass_utils import run_bass_kernel_spmd

F32 = mybir.dt.float32
BF16 = mybir.dt.bfloat16
I32 = mybir.dt.int32
AF = mybir.ActivationFunctionType
ALU = mybir.AluOpType

S_LEN = 4096
D = 1024
NT = S_LEN // 128
NE = 32
DFF = 1024
LN_EPS = 1e-5
NORM_EPS = 1e-6
DN_ALPHA = 2.0 ** 0.25
LAMBDA_INIT = 0.8 - 0.6 * math.exp(0.0)
SLOPES = [2.0 ** (-8.0 * (h + 1) / 4) for h in range(4)]
NPASS = 4
PASS_TOK = S_LEN // NPASS


class Sched:
    ENG = ('pe', 'act', 'dve', 'pool', 'sp')

    def __init__(self, nc):
        self.nc = nc
        self.prog = {k: [] for k in self.ENG}
        self.cnt = {}
        self.sems = {}
        self.seen = {k: {} for k in self.ENG}
        self.res = {}
        for k in self.ENG:
            self._sem(k)

    def _sem(self, key):
        if key not in self.sems:
            self.sems[key] = self.nc.alloc_semaphore(name="s%d" % len(self.sems))
            self.cnt[key] = 0
        return self.sems[key]

    def emit(self, eng, fns, reads=(), writes=(), dma=None, partial=False):
        if callable(fns):
            fns = [fns]
        deps = {}

        def add(d):
            for s, v in d.items():
                if deps.get(s, 0) < v:
                    deps[s] = v
        for r in reads:
            st = self.res.get(r)
            if st:
                add(st['w'])
        for w in writes:
            st = self.res.get(w)
            if st:
                add(st['w'])
                add(st['r'])
        if dma is not None:
            self._sem(dma)
            if self.cnt[dma] > 0:
                add({dma: self.cnt[dma]})
        P = self.prog[eng]
        for s, v in deps.items():
            if s == eng and eng == 'pe':
                continue
            if self.seen[eng].get(s, 0) >= v:
                continue
            self.seen[eng][s] = v
            sem = self.sems[s]
            P.append(lambda e, sem=sem, v=v: e.wait_ge(sem, v))
        skey, inc = (dma, 16) if dma is not None else (eng, 1)
        self.cnt[skey] += inc
        val = self.cnt[skey]
        sem = self.sems[skey]
        n = len(fns)
        for i, f in enumerate(fns):
            if i == n - 1:
                P.append(lambda e, f=f, sem=sem, inc=inc: f(e).then_inc(sem, inc))
            else:
                P.append(lambda e, f=f: f(e))
        for r in reads:
            st = self.res.setdefault(r, {'w': {}, 'r': {}})
            if st['r'].get(skey, 0) < val:
                st['r'][skey] = val
        for w in writes:
            if partial and w in self.res:
                self.res[w]['w'][skey] = val
            else:
                self.res[w] = {'w': {skey: val}, 'r': {}}

    def barrier(self):
        allev = dict(self.cnt)
        for eng in self.ENG:
            for s, v in allev.items():
                if v == 0 or (s == eng and eng == 'pe'):
                    continue
                if self.seen[eng].get(s, 0) >= v:
                    continue
                self.seen[eng][s] = v
                sem = self.sems[s]
                self.prog[eng].append(lambda e, sem=sem, v=v: e.wait_ge(sem, v))
        self.res = {}

    def run(self):
        nc = self.nc
        with nc.Block() as block:
            @block.tensor
            def _(e):
                for f in self.prog['pe']:
                    f(e)

            @block.scalar
            def _(e):
                for f in self.prog['act']:
                    f(e)

            @block.vector
            def _(e):
                for f in self.prog['dve']:
                    f(e)

            @block.gpsimd
            def _(e):
                for f in self.prog['pool']:
                    f(e)

            @block.sync
            def _(e):
                for f in self.prog['sp']:
                    f(e)


def bcast_ap(dram_ap, n, offset=0, parts=128):
    return bass.AP(dram_ap.tensor, dram_ap.offset + offset, [[0, parts], [1, n]])


def build(stage=99, dbg=False, hstop=99, ne_decl=NE, heads=4, cut=99):
    nc = bass.Bass("TRN2", target_bir_lowering=False)

    def din(name, shape):
        return nc.dram_tensor(name, list(shape), F32, kind="ExternalInput").ap()

    x = din("x", [S_LEN, D])
    w_in = din("w_in", [D, 5632])
    hg_lb = din("hg_lb_logits", [2, 512])
    hg_ng = din("hg_norm_g", [4, 128])
    da_lam = din("da_lambda", [4, 64])
    da_ng = din("da_norm_g", [4, 128])
    w_a = din("w_branch_a", [512, D])
    w_b = din("w_branch_b", [512, D])
    w_out = din("w_out", [D, D])
    ln1_g = din("ln1_g", [D])
    ln1_b = din("ln1_b", [D])
    router_w = din("router_w", [D, NE])
    router_b = din("router_b", [NE])
    w_gu = din("w_gate_up", [ne_decl, D, 2 * DFF])
    b_gu = din("b_gate_up", [NE, 2 * DFF])
    w_dn = din("w_down", [ne_decl, DFF, D])
    b_dn = din("b_down", [NE, D])
    ln2_g = din("ln2_g", [D])
    ln2_b = din("ln2_b", [D])
    y = nc.dram_tensor("y", [S_LEN, D], F32, kind="ExternalOutput").ap()

    xT_d = nc.dram_tensor("xT_d", [8, 128, S_LEN], BF16, kind="Internal").ap()
    sg_d = [nc.dram_tensor("sg%d_d" % i, [8, 128, S_LEN], BF16, kind="Internal").ap() for i in range(2)]
    h1_d = nc.dram_tensor("h1_d", [S_LEN, D], F32, kind="Internal").ap()
    h1T_d = nc.dram_tensor("h1T_d", [8, 128, S_LEN], BF16, kind="Internal").ap()
    dbg_t = {}
    if dbg:
        dbg_t['oaT'] = nc.dram_tensor("dbg_oaT", [128, 4, S_LEN], BF16, kind="ExternalOutput").ap()
        dbg_t['obT'] = nc.dram_tensor("dbg_obT", [128, 4, S_LEN], BF16, kind="ExternalOutput").ap()
        dbg_t['h1'] = nc.dram_tensor("dbg_h1", [S_LEN, D], F32, kind="ExternalOutput").ap()
        dbg_t['comb'] = nc.dram_tensor("dbg_comb", [128, NT, NE], F32, kind="ExternalOutput").ap()

    S = Sched(nc)
    _uid = [0]

    def uq(name):
        _uid[0] += 1
        return "%s_u%d" % (name, _uid[0])
    with ExitStack() as G:
        def sbg(name, shape, dt):
            return G.enter_context(nc.sbuf_tensor(uq(name), list(shape), dt))
        PS = [G.enter_context(nc.psum_tensor("ps%d" % i, [128, 512], F32)) for i in range(8)]

        def psb(i):
            return PS[i][:].bitcast(BF16)

        identf = sbg("identf", [128, 128], F32)
        ident = sbg("ident", [128, 128], BF16)
        tri = sbg("tri", [128, 128], BF16)
        tribd = sbg("tribd", [128, 128], BF16)
        onesf = sbg("onesf", [128, 128], F32)
        nhalf = sbg("nhalf", [128, 8], F32)
        scanmask = sbg("scanmask", [128, 512], F32)
        btab_i = sbg("btab_i", [128, 35], I32)
        btab = sbg("btab", [128, 4, 35], F32)
        comb = sbg("comb", [128, NT, NE], F32)
        MIX = ExitStack()

        def sbm(name, shape, dt):
            return MIX.enter_context(nc.sbuf_tensor(uq(name), list(shape), dt))
        o_aT = sbm("o_aT", [128, 4, S_LEN], BF16)
        o_bT = sbm("o_bT", [128, 4, S_LEN], BF16)
        wst = [sbm("wst%d" % i, [128, 2048], F32) for i in range(2)]

        S.emit('pool', lambda e: e.memset(identf[:], 0.0), writes=['identf'])
        S.emit('pool', lambda e: e.affine_select(out=identf[:], in_=identf[:], pattern=[[-1, 128]],
                                                  compare_op=ALU.not_equal, fill=1.0, base=0,
                                                  channel_multiplier=1),
               reads=['identf'], writes=['identf'])
        S.emit('dve', lambda e: e.tensor_copy(out=ident[:], in_=identf[:]), reads=['identf'], writes=['ident'])
        S.emit('pool', lambda e: e.memset(onesf[:], 1.0), writes=['onesf'])
        S.emit('pool', lambda e: e.memset(nhalf[:], -0.5), writes=['nhalf'])
        S.emit('pool', lambda e: e.affine_select(out=tri[:], in_=onesf[:], pattern=[[1, 128]],
                                                  compare_op=ALU.is_ge, fill=0.0, base=0,
                                                  channel_multiplier=-1),
               reads=['onesf'], writes=['tri'])
        S.emit('pool', lambda e: e.tensor_copy(out=tribd[:], in_=tri[:]), reads=['tri'], writes=['tribd'])
        S.emit('pool', lambda e: e.memset(tribd[0:64, 64:128], 0.0), reads=['tribd'], writes=['tribd'])
        S.emit('pool', lambda e: e.memset(scanmask[:], 1.0), writes=['scanmask'])
        S.emit('pool', lambda e: e.memset(scanmask[:].rearrange("p (c t) -> p c t", t=64)[:, :, 0:1], 0.0),
               reads=['scanmask'], writes=['scanmask'])
        S.emit('pool', lambda e: e.iota(btab_i[:], pattern=[[-128, 35]], base=384, channel_multiplier=1),
               writes=['btab_i'])
        for h in range(4):
            S.emit('dve', lambda e, h=h: e.tensor_scalar(out=btab[:, h, :], in0=btab_i[:], scalar1=float(SLOPES[h]),
                                                        scalar2=None, op0=ALU.mult),
                   reads=['btab_i'], writes=[('btab', h)])

        bank_rr = [0]

        def load_cast(dst_ap, src_ap, shape, key, cast_eng='act'):
            a, b = shape
            step = max(1, 2048 // b)
            for a0 in range(0, a, step):
                a1 = min(a, a0 + step)
                i = bank_rr[0] % 2
                bank_rr[0] += 1
                st = wst[i][:, 0:(a1 - a0) * b].rearrange("p (a b) -> p a b", b=b)
                S.emit('sp', lambda e, st=st, a0=a0, a1=a1: e.dma_start(out=st, in_=src_ap[:, a0:a1, :]),
                       writes=[('wst', i)], dma=('dwst', i))
                if cast_eng == 'act':
                    S.emit('act', lambda e, st=st, a0=a0, a1=a1: e.copy(out=dst_ap[:, a0:a1, :], in_=st),
                           reads=[('wst', i)], writes=[key], partial=True)
                else:
                    S.emit(cast_eng, lambda e, st=st, a0=a0, a1=a1: e.tensor_copy(out=dst_ap[:, a0:a1, :], in_=st),
                           reads=[('wst', i)], writes=[key], partial=True)

        with ExitStack() as P:
            def sb(name, shape, dt):
                return P.enter_context(nc.sbuf_tensor(uq(name), list(shape), dt))
            xs = [sb("xs%d" % i, [128, D], F32) for i in range(2)]
            xb = [sb("xb%d" % i, [128, D], BF16) for i in range(2)]
            xTs = [sb("xTs%d" % i, [128, 8, 512], BF16) for i in range(2)]
            for t in range(NT):
                i = t % 2
                S.emit('sp', lambda e, t=t, i=i: e.dma_start(out=xs[i][:], in_=x[t * 128:(t + 1) * 128, :]),
                       writes=[('xs', i)], dma=('dxs', i))
                if i == 0:
                    S.emit('dve', lambda e, i=i: e.tensor_copy(out=xb[i][:], in_=xs[i][:]), reads=[('xs', i)], writes=[('xb', i)])
                else:
                    S.emit('act', lambda e, i=i: e.copy(out=xb[i][:], in_=xs[i][:]), reads=[('xs', i)], writes=[('xb', i)])
                bk = t % 2
                pv = psb(bk).rearrange("p (c n) -> p c n", n=128)
                S.emit('pe', [lambda e, c=c, i=i, pv=pv: e.transpose(out=pv[:, c, :], in_=xb[i][:, c * 128:(c + 1) * 128],
                                                                     identity=ident[:]) for c in range(8)],
                       reads=[('xb', i), 'ident'], writes=[('ps', bk)])
                g4 = (t // 4) % 2
                j = t % 4
                ce = 'dve' if i == 1 else 'act'
                if ce == 'dve':
                    S.emit('dve', lambda e, g4=g4, j=j, pv=pv: e.tensor_copy(out=xTs[g4][:, :, j * 128:(j + 1) * 128], in_=pv),
                           reads=[('ps', bk)], writes=[('xTs', g4)], partial=(j > 0))
                else:
                    S.emit('act', lambda e, g4=g4, j=j, pv=pv: e.copy(out=xTs[g4][:, :, j * 128:(j + 1) * 128], in_=pv),
                           reads=[('ps', bk)], writes=[('xTs', g4)], partial=(j > 0))
                if j == 3:
                    tt = t // 4
                    S.emit('sp', lambda e, g4=g4, tt=tt: e.dma_start(
                        out=xT_d[:, :, tt * 512:(tt + 1) * 512].rearrange("c p n -> p c n"), in_=xTs[g4][:]),
                        reads=[('xTs', g4)], dma=('dxT', g4))
            S.barrier()

        def make_xT_loader(P):
            bufs = [P.enter_context(nc.sbuf_tensor(uq("xTt%d" % i), [128, 8, 512], BF16)) for i in range(2)]
            cnt = [0]

            def load(tt):
                i = cnt[0] % 2
                cnt[0] += 1
                S.emit('sp', lambda e: e.dma_start(out=bufs[i][:],
                                                   in_=xT_d[:, :, tt * 512:(tt + 1) * 512].rearrange("c p n -> p c n")),
                       writes=[('xTt', i)], dma=('dxTt', i))
                return bufs[i], ('xTt', i)
            return load

        with ExitStack() as P:
            def sb(name, shape, dt):
                return P.enter_context(nc.sbuf_tensor(uq(name), list(shape), dt))
            wg = sb("wg", [128, 8, 2048], BF16)
            for q4 in range(4):
                load_cast(wg[:, :, q4 * 512:(q4 + 1) * 512],
                          w_in[:, 3584 + q4 * 512:3584 + (q4 + 1) * 512].rearrange("(c p) n -> p c n", p=128),
                          (8, 512), ('wg', q4))
            sgst = [sb("sgst%d" % i, [128, 8, 512], BF16) for i in range(2)]
            loadx = make_xT_loader(P)
            bk = 0
            for tt in range(8):
                xt, xk = loadx(tt)
                for gi in range(2):
                    for fc in range(8):
                        b = bk % 4
                        bk += 1
                        c0 = gi * 1024 + fc * 128
                        S.emit('pe', [lambda e, kc=kc, c0=c0, b=b, xt=xt: e.matmul(
                            PS[b][:], lhsT=wg[:, kc, c0:c0 + 128], rhs=xt[:, kc, :], start=(kc == 0), stop=(kc == 7))
                            for kc in range(8)],
                            reads=[xk, ('wg', c0 // 512)], writes=[('ps', b)])
                        S.emit('act', lambda e, gi=gi, fc=fc, b=b: e.activation(out=sgst[gi][:, fc, :], in_=PS[b][:],
                                                                               func=AF.Sigmoid),
                               reads=[('ps', b)], writes=[('sgst', gi)], partial=(fc > 0))
                    S.emit('sp', lambda e, gi=gi, tt=tt: e.dma_start(
                        out=sg_d[gi][:, :, tt * 512:(tt + 1) * 512].rearrange("c p n -> p c n"), in_=sgst[gi][:]),
                        reads=[('sgst', gi)], dma=('dsg', gi))
            S.barrier()

        def phase_H():
          with ExitStack() as P:
            def sb(name, shape, dt):
                return P.enter_context(nc.sbuf_tensor(uq(name), list(shape), dt))
            wq = sb("wq", [128, 8, 128], BF16)
            wf = sb("wf", [128, 8, 128], BF16)
            wig = sb("wig", [128, 8, 256], BF16)
            qeT = sb("qeT", [128, S_LEN], BF16)
            kdT = sb("kdT", [128, S_LEN], BF16)
            kd_tok = sb("kd_tok", [128, NT, 128], BF16)
            v_tok = sb("v_tok", [128, NT, 128], BF16)
            sgn = sb("sgn", [128, NT, 128], BF16)
            ebl = sb("ebl", [128, 64], F32)
            lbT8 = sb("lbT8", [8, 128], F32)
            lbp = sb("lbp", [128, 8], F32)
            lbv = sb("lbv", [128, 4], F32)
            omlb = sb("omlb", [128, 4], F32)
            ngA = sb("ngA", [128, 4, 128], F32)
            q32 = [sb("q32_%d" % i, [128, 512], F32) for i in range(2)]
            sg32 = [sb("sg32_%d" % i, [128, 512], F32) for i in range(2)]
            lf32 = [sb("lf32_%d" % i, [128, 512], F32) for i in range(2)]
            kk32 = [sb("kk32_%d" % i, [128, 512], F32) for i in range(2)]
            bc32 = [sb("bc32_%d" % i, [128, 512], F32) for i in range(2)]
            eb32 = [sb("eb32_%d" % i, [128, 512], F32) for i in range(2)]
            en32 = [sb("en32_%d" % i, [128, 512], F32) for i in range(2)]
            sgt = [sb("sgt%d" % i, [128, 128], F32) for i in range(2)]
            st32 = sb("st32", [128, 128], F32)
            tmp32 = sb("tmp32", [128, 128], F32)
            stbf = [sb("stbf%d" % i, [128, 128], BF16) for i in range(2)]
            attm = [sb("attm%d" % i, [128, 128], BF16) for i in range(2)]
            junk = sb("junk", [128, 128], BF16)
            ssA = sb("ssA", [128, 2], F32)
            rsA = sb("rsA", [128, 2], F32)
            oab = [sb("oab%d" % i, [128, 128], BF16) for i in range(2)]
            loadx = make_xT_loader(P)

            S.emit('sp', lambda e: e.dma_start(out=lbT8[:], in_=hg_lb.rearrange("r (h p) -> (r h) p", p=128)),
                   writes=['lbT8'], dma='dsmall0')
            S.emit('pe', lambda e: e.transpose(out=PS[7][:, 0:8], in_=lbT8[:], identity=identf[0:8, 0:8]),
                   reads=['lbT8', 'identf'], writes=[('ps', 7)])
            S.emit('dve', lambda e: e.tensor_copy(out=lbp[:], in_=PS[7][:, 0:8]), writes=['lbp', ('ps', 7)])
            S.emit('dve', lambda e: e.tensor_tensor(out=lbv[:], in0=lbp[:, 0:4], in1=lbp[:, 4:8], op=ALU.subtract),
                   reads=['lbp'], writes=['lbv'])
            S.emit('act', lambda e: e.activation(out=lbv[:], in_=lbv[:], func=AF.Sigmoid), reads=['lbv'], writes=['lbv'])
            S.emit('dve', lambda e: e.tensor_scalar(out=omlb[:], in0=lbv[:], scalar1=-1.0, scalar2=1.0, op0=ALU.mult,
                                                    op1=ALU.add), reads=['lbv'], writes=['omlb'])
            S.emit('sp', lambda e: e.dma_start(out=ngA[:].rearrange("p h n -> p (h n)"), in_=bcast_ap(hg_ng, 512)),
                   writes=['ngA'], dma='dsmall1')

            for h in range(heads if hstop >= 2 else 0):
                load_cast(wq[:], w_in[:, h * 128:(h + 1) * 128].rearrange("(c p) n -> p c n", p=128), (8, 128), 'wq')
                load_cast(wf[:], w_in[:, 512 + h * 128:512 + (h + 1) * 128].rearrange("(c p) n -> p c n", p=128),
                          (8, 128), 'wf')
                load_cast(wig[:, :, 0:128], w_in[:, 1024 + h * 128:1024 + (h + 1) * 128].rearrange("(c p) n -> p c n", p=128),
                          (8, 128), 'wig')
                load_cast(wig[:, :, 128:256], w_in[:, 1536 + h * 128:1536 + (h + 1) * 128].rearrange("(c p) n -> p c n", p=128),
                          (8, 128), 'wig')
                for tt in range(8):
                    xt, xk = loadx(tt)
                    i = tt % 2
                    tsl = slice(tt * 512, (tt + 1) * 512)
                    S.emit('pe', [lambda e, kc=kc, xt=xt: e.matmul(PS[0][:], lhsT=wq[:, kc, :], rhs=xt[:, kc, :],
                                                                  start=(kc == 0), stop=(kc == 7)) for kc in range(8)],
                           reads=[xk, 'wq'], writes=[('ps', 0)])
                    S.emit('pe', [lambda e, kc=kc, xt=xt: e.matmul(PS[1][:], lhsT=wf[:, kc, :], rhs=xt[:, kc, :],
                                                                  start=(kc == 0), stop=(kc == 7)) for kc in range(8)],
                           reads=[xk, 'wf'], writes=[('ps', 1)])
                    S.emit('act', lambda e, i=i: e.activation(out=q32[i][:], in_=PS[0][:], func=AF.Silu),
                           reads=[('ps', 0)], writes=[('q32', i)])
                    S.emit('act', lambda e, i=i: e.activation(out=sg32[i][:], in_=PS[1][:], func=AF.Sigmoid),
                           reads=[('ps', 1)], writes=[('sg32', i)])
                    if cut < 2:
                        continue
                    S.emit('dve', lambda e, i=i, h=h: e.tensor_scalar(out=sg32[i][:], in0=sg32[i][:],
                                                                      scalar1=omlb[:, h:h + 1], scalar2=lbv[:, h:h + 1],
                                                                      op0=ALU.mult, op1=ALU.add),
                           reads=[('sg32', i), 'omlb', 'lbv'], writes=[('sg32', i)])
                    S.emit('act', lambda e, i=i: e.activation(out=lf32[i][:], in_=sg32[i][:], func=AF.Ln),
                           reads=[('sg32', i)], writes=[('lf32', i)])
                    S.emit('pool', lambda e, i=i: e.tensor_scalar(out=kk32[i][:], in0=sg32[i][:], scalar1=-1.0, scalar2=1.0,
                                                                  op0=ALU.mult, op1=ALU.add),
                           reads=[('sg32', i)], writes=[('kk32', i)])
                    if cut < 3:
                        continue
                    S.emit('dve', lambda e, i=i: e.tensor_tensor_scan(out=bc32[i][:], data0=scanmask[:], data1=lf32[i][:],
                                                                      initial=0.0, op0=ALU.mult, op1=ALU.add),
                           reads=[('lf32', i), 'scanmask'], writes=[('bc32', i)])
                    S.emit('act', lambda e, i=i: e.activation(out=eb32[i][:], in_=bc32[i][:], func=AF.Exp),
                           reads=[('bc32', i)], writes=[('eb32', i)])
                    S.emit('act', lambda e, i=i: e.activation(out=en32[i][:], in_=bc32[i][:], func=AF.Exp, scale=-1.0),
                           reads=[('bc32', i)], writes=[('en32', i)])
                    if cut < 4:
                        continue
                    S.emit('dve', lambda e, i=i, tsl=tsl: e.tensor_tensor(out=qeT[:, tsl], in0=q32[i][:], in1=eb32[i][:],
                                                                          op=ALU.mult),
                           reads=[('q32', i), ('eb32', i)], writes=[('qeT', tt)])
                    S.emit('pool', lambda e, i=i, tsl=tsl: e.tensor_tensor(out=kdT[:, tsl], in0=kk32[i][:], in1=en32[i][:],
                                                                           op=ALU.mult),
                           reads=[('kk32', i), ('en32', i)], writes=[('kdT', tt)])
                    S.emit('pool', lambda e, i=i, tt=tt: e.tensor_copy(
                        out=ebl[:, tt * 8:(tt + 1) * 8],
                        in_=eb32[i][:].rearrange("p (c t) -> p c t", t=64)[:, :, 63]),
                        reads=[('eb32', i)], writes=[('ebl', tt)])
                    if cut < 5:
                        continue
                    for s4 in range(4):
                        p = tt * 4 + s4
                        b = 2 + (p % 2)
                        S.emit('pe', [lambda e, kc=kc, xt=xt, s4=s4, b=b: e.matmul(
                            PS[b][:, 0:256], lhsT=xt[:, kc, s4 * 128:(s4 + 1) * 128], rhs=wig[:, kc, :],
                            start=(kc == 0), stop=(kc == 7)) for kc in range(8)],
                            reads=[xk, 'wig'], writes=[('ps', b)])
                        S.emit('dve', lambda e, p=p, b=b: e.tensor_copy(out=v_tok[:, p, :], in_=PS[b][:, 0:128]),
                               writes=[('v_tok', p), ('ps', b)])
                        j = p % 2
                        S.emit('act', lambda e, j=j, b=b: e.activation(out=sgt[j][:], in_=PS[b][:, 128:256], func=AF.Sigmoid),
                               writes=[('sgt', j), ('ps', b)])
                        S.emit('pool', lambda e, j=j, p=p, h=h: e.tensor_tensor(out=sgn[:, p, :], in0=sgt[j][:],
                                                                                in1=ngA[:, h, :], op=ALU.mult),
                               reads=[('sgt', j), 'ngA'], writes=[('sgn', p)])
                    if cut < 6:
                        continue
                    pv = psb(4 + i).rearrange("p (c n) -> p c n", n=128)
                    S.emit('pe', [lambda e, s4=s4, tt=tt, pv=pv: e.transpose(
                        out=pv[:, s4, :], in_=kdT[:, tt * 512 + s4 * 128: tt * 512 + (s4 + 1) * 128], identity=ident[:])
                        for s4 in range(4)],
                        reads=[('kdT', tt), 'ident'], writes=[('ps', 4 + i)])
                    S.emit('dve', lambda e, tt=tt, pv=pv: e.tensor_copy(out=kd_tok[:, tt * 4:(tt + 1) * 4, :], in_=pv[:, 0:4, :]),
                           reads=[('ps', 4 + i)], writes=[('kd_tok', tt)])
                S.emit('dve', lambda e: e.memset(st32[:], 0.0), writes=['st32'])
                S.emit('dve', lambda e: e.memset(stbf[0][:], 0.0), writes=[('stbf', 0)])
                pend_T = None

                def emit_T(j, psl_):
                    tb = 2 + j
                    tv = psb(tb)[:, 0:128]
                    S.emit('pe', lambda e, j=j, tv=tv: e.transpose(out=tv, in_=oab[j][:], identity=ident[:]),
                           reads=[('oab', j), 'ident'], writes=[('ps', tb)])
                    S.emit('act', lambda e, h=h, psl_=psl_, tv=tv: e.copy(out=o_aT[:, h, psl_], in_=tv),
                           writes=[('o_aT', h, psl_.start), ('ps', tb)])
                for p in range(NT if hstop >= 3 else 0):
                    tt = p // 4
                    psl = slice(p * 128, (p + 1) * 128)
                    ba = 6 + (p % 2)
                    bo = p % 2
                    A_ps = PS[ba][:, 0:128]
                    O_ps = PS[bo][:, 0:128]
                    for hf in range(2):
                        c = 2 * p + hf
                        hs = slice(hf * 64, (hf + 1) * 64)
                        ub = 4 + (c % 2)
                        S.emit('pe', lambda e, hs=hs, p=p, ub=ub: e.matmul(
                            PS[ub][:, 0:128], lhsT=kd_tok[hs, p, :], rhs=v_tok[hs, p, :], start=True, stop=True),
                            reads=[('kd_tok', tt), ('v_tok', p)], writes=[('ps', ub)])
                    S.emit('pe', lambda e, psl=psl, A_ps=A_ps: e.matmul(A_ps, lhsT=kdT[:, psl], rhs=qeT[:, psl],
                                                                       start=True, stop=True),
                           reads=[('kdT', tt), ('qeT', tt)], writes=[('ps', ba)])
                    am = attm[p % 2]
                    S.emit('dve', lambda e, am=am, A_ps=A_ps: e.tensor_tensor(out=am[:], in0=A_ps, in1=tribd[:], op=ALU.mult),
                           reads=['tribd'], writes=[('attm', p % 2), ('ps', ba)])
                    for hf in range(2):
                        c = 2 * p + hf
                        csl = slice(c * 64, (c + 1) * 64)
                        hs = slice(hf * 64, (hf + 1) * 64)
                        sb_cur = stbf[c % 2]
                        sb_nxt = stbf[(c + 1) % 2]
                        ub = 4 + (c % 2)
                        S.emit('pe', lambda e, csl=csl, hs=hs, sb_cur=sb_cur, bo=bo: e.matmul(
                            PS[bo][hs, 0:128], lhsT=qeT[:, csl], rhs=sb_cur[:], start=True, stop=False),
                            reads=[('qeT', tt), ('stbf', c % 2)], writes=[('ps', bo)])
                        S.emit('dve', lambda e, ub=ub: e.tensor_tensor(out=tmp32[:], in0=PS[ub][:, 0:128], in1=st32[:], op=ALU.add),
                               reads=['st32'], writes=['tmp32', ('ps', ub)])
                        S.emit('dve', lambda e, c=c: e.tensor_scalar(out=st32[:], in0=tmp32[:], scalar1=ebl[:, c:c + 1],
                                                                     scalar2=None, op0=ALU.mult),
                               reads=['tmp32', ('ebl', c // 8)], writes=['st32'])
                        S.emit('act', lambda e, c=c, sb_nxt=sb_nxt: e.activation(out=sb_nxt[:], in_=tmp32[:], func=AF.Copy,
                                                                               scale=ebl[:, c:c + 1]),
                               reads=['tmp32', ('ebl', c // 8)], writes=[('stbf', (c + 1) % 2)])
                    S.emit('pe', lambda e, am=am, p=p, O_ps=O_ps: e.matmul(O_ps, lhsT=am[:], rhs=v_tok[:, p, :],
                                                                         start=False, stop=True),
                           reads=[('attm', p % 2), ('v_tok', p)], writes=[('ps', bo)])
                    if pend_T is not None:
                        emit_T(*pend_T)
                    j = p % 2
                    S.emit('act', lambda e, O_ps=O_ps, j=j: e.activation(out=junk[:], in_=O_ps, func=AF.Square,
                                                                       accum_out=ssA[:, j:j + 1]),
                           writes=[('ssA', j), 'junk', ('ps', bo)])
                    S.emit('dve', lambda e, j=j: e.tensor_scalar(out=rsA[:, j:j + 1], in0=ssA[:, j:j + 1], scalar1=1.0 / 128,
                                                                 scalar2=NORM_EPS, op0=ALU.mult, op1=ALU.add),
                           reads=[('ssA', j)], writes=[('rsA', j)])
                    S.emit('pool', lambda e, j=j: e.tensor_tensor(out=rsA[:, j:j + 1], in0=rsA[:, j:j + 1], in1=nhalf[:, 0:1], op=ALU.pow),
                           reads=[('rsA', j), 'nhalf'], writes=[('rsA', j)])
                    S.emit('dve', lambda e, j=j, p=p, O_ps=O_ps: e.scalar_tensor_tensor(
                        out=oab[j][:], in0=O_ps, scalar=rsA[:, j:j + 1], in1=sgn[:, p, :], op0=ALU.mult, op1=ALU.mult),
                        reads=[('rsA', j), ('sgn', p)], writes=[('oab', j), ('ps', bo)])
                    pend_T = (j, psl)
                if pend_T is not None:
                    emit_T(*pend_T)
            S.barrier()
            if dbg:
                S.emit('sp', lambda e: e.dma_start(out=dbg_t['oaT'], in_=o_aT[:]), dma='ddbg0')

        if stage >= 1:
            phase_H()

        def phase_D():
          with ExitStack() as P:
            def sb(name, shape, dt):
                return P.enter_context(nc.sbuf_tensor(uq(name), list(shape), dt))
            wdq = sb("wdq", [128, 8, 128], BF16)
            wdk = sb("wdk", [128, 8, 128], BF16)
            wdv = sb("wdv", [128, 8, 128], BF16)
            dqT = sb("dqT", [128, S_LEN], BF16)
            dkT = sb("dkT", [128, S_LEN], BF16)
            dv_ext = sb("dv_ext", [128, NT, 129], BF16)
            ebuf = [sb("ebuf%d" % i, [128, 2, 512], BF16) for i in range(3)]
            lamt = sb("lamt", [128, 256], F32)
            lpr = sb("lpr", [128, 128], F32)
            lsm = sb("lsm", [128, 2], F32)
            neglam = sb("neglam", [128, 1], F32)
            ngB = sb("ngB", [128, 4, 128], F32)
            rz = [sb("rz%d" % i, [128, 2], F32) for i in range(4)]
            t1 = [sb("t1_%d" % i, [128, 128], F32) for i in range(4)]
            o32 = [sb("o32_%d" % i, [128, 128], F32) for i in range(4)]
            junk = sb("junkd", [128, 128], BF16)
            ssB4 = [sb("ssB4_%d" % i, [128, 4], F32) for i in range(2)]
            obb4 = [sb("obb4_%d" % i, [128, 512], BF16) for i in range(2)]
            ssB = sb("ssB", [128, 2], F32)
            rsB = sb("rsB", [128, 2], F32)
            obb = [sb("obb%d" % i, [128, 128], BF16) for i in range(2)]
            loadx = make_xT_loader(P)

            S.emit('sp', lambda e: e.dma_start(out=lamt[:], in_=bcast_ap(da_lam, 256)), writes=['lamt'], dma='dsmall0')
            S.emit('sp', lambda e: e.dma_start(out=ngB[:].rearrange("p h n -> p (h n)"), in_=bcast_ap(da_ng, 512)),
                   writes=['ngB'], dma='dsmall1')
            S.emit('dve', lambda e: e.tensor_scalar(out=ngB[:], in0=ngB[:], scalar1=float(1.0 - LAMBDA_INIT), scalar2=None,
                                                    op0=ALU.mult), reads=['ngB'], writes=['ngB'])
            lv = lamt[:].rearrange("p (a n) -> p a n", n=64)
            S.emit('dve', lambda e: e.tensor_tensor(out=lpr[:, 0:64], in0=lv[:, 0, :], in1=lv[:, 1, :], op=ALU.mult),
                   reads=['lamt'], writes=['lpr'])
            S.emit('dve', lambda e: e.tensor_tensor(out=lpr[:, 64:128], in0=lv[:, 2, :], in1=lv[:, 3, :], op=ALU.mult),
                   reads=['lamt', 'lpr'], writes=['lpr'])
            S.emit('dve', lambda e: e.tensor_reduce(out=lsm[:], in_=lpr[:].rearrange("p (a n) -> p a n", n=64),
                                                    axis=mybir.AxisListType.X, op=ALU.add),
                   reads=['lpr'], writes=['lsm'])
            S.emit('act', lambda e: e.activation(out=lsm[:], in_=lsm[:], func=AF.Exp), reads=['lsm'], writes=['lsm'])
            S.emit('dve', lambda e: e.tensor_tensor(out=neglam[:], in0=lsm[:, 1:2], in1=lsm[:, 0:1], op=ALU.subtract),
                   reads=['lsm'], writes=['neglam'])
            S.emit('dve', lambda e: e.tensor_scalar(out=neglam[:], in0=neglam[:], scalar1=float(-LAMBDA_INIT), scalar2=None,
                                                    op0=ALU.add), reads=['neglam'], writes=['neglam'])
            S.emit('pool', lambda e: e.memset(dv_ext[:, :, 128:129], 1.0), writes=['dv_ones'])

            fin_cnt = [0]
            for h in range(heads):
                W = 256 if h == 0 else 512
                load_cast(wdq[:], w_in[:, 2048 + h * 128:2048 + (h + 1) * 128].rearrange("(c p) n -> p c n", p=128), (8, 128), 'wdq')
                load_cast(wdk[:], w_in[:, 2560 + h * 128:2560 + (h + 1) * 128].rearrange("(c p) n -> p c n", p=128), (8, 128), 'wdk')
                load_cast(wdv[:], w_in[:, 3072 + h * 128:3072 + (h + 1) * 128].rearrange("(c p) n -> p c n", p=128), (8, 128), 'wdv')
                for tt in range(8):
                    xt, xk = loadx(tt)
                    tsl = slice(tt * 512, (tt + 1) * 512)
                    S.emit('pe', [lambda e, kc=kc, xt=xt: e.matmul(PS[0][:], lhsT=wdq[:, kc, :], rhs=xt[:, kc, :],
                                                                  start=(kc == 0), stop=(kc == 7)) for kc in range(8)],
                           reads=[xk, 'wdq'], writes=[('ps', 0)])
                    S.emit('pe', [lambda e, kc=kc, xt=xt: e.matmul(PS[1][:], lhsT=wdk[:, kc, :], rhs=xt[:, kc, :],
                                                                  start=(kc == 0), stop=(kc == 7)) for kc in range(8)],
                           reads=[xk, 'wdk'], writes=[('ps', 1)])
                    S.emit('act', lambda e, tsl=tsl: e.activation(out=dqT[:, tsl], in_=PS[0][:], func=AF.Copy, scale=0.125),
                           writes=[('dqT', tt), ('ps', 0)])
                    S.emit('dve', lambda e, tsl=tsl: e.tensor_copy(out=dkT[:, tsl], in_=PS[1][:]),
                           writes=[('dkT', tt), ('ps', 1)])
                    for s4 in range(4):
                        p = tt * 4 + s4
                        b = 2 + (p % 2)
                        S.emit('pe', [lambda e, kc=kc, xt=xt, s4=s4, b=b: e.matmul(
                            PS[b][:, 0:128], lhsT=xt[:, kc, s4 * 128:(s4 + 1) * 128], rhs=wdv[:, kc, :],
                            start=(kc == 0), stop=(kc == 7)) for kc in range(8)],
                            reads=[xk, 'wdv'], writes=[('ps', b)])
                        S.emit('dve', lambda e, p=p, b=b: e.tensor_copy(out=dv_ext[:, p, 0:128], in_=PS[b][:, 0:128]),
                               reads=['dv_ones'], writes=[('dv', p), ('ps', b)])
                ecnt = 0
                for qt in range(8):
                    started = {}
                    nk = 4 * qt + 4
                    pend_av = None
                    for kt in range(nk):
                        st2 = kt % 2
                        sbk = [2 * st2, 2 * st2 + 1]
                        i0 = max(0, kt - 4 * qt)
                        c0 = i0 * 128
                        eb = ebuf[ecnt % 3]
                        ek = ('ebuf', ecnt % 3)
                        ecnt += 1
                        for j in range(2):
                            js = slice(j * 64, (j + 1) * 64)
                            S.emit('pe', lambda e, j=j, js=js, kt=kt, qt=qt, c0=c0, sbk=sbk: e.matmul(
                                PS[sbk[j]][:, c0:512], lhsT=dkT[js, kt * 128:(kt + 1) * 128],
                                rhs=dqT[js, qt * 512 + c0:(qt + 1) * 512], start=True, stop=True),
                                reads=[('dkT', kt // 4), ('dqT', qt)], writes=[('ps', sbk[j])])
                        for j in range(2):
                            fns = []
                            if W == 512:
                                col = (4 * qt - kt) + 3
                                fns.append(lambda e, j=j, c0=c0, col=col, sbk=sbk, eb=eb, h=h: e.activation(
                                    out=eb[:, j, c0:512], in_=PS[sbk[j]][:, c0:512], func=AF.Exp,
                                    bias=btab[:, h, col:col + 1], scale=1.0))
                            else:
                                for i2 in range(i0 // 2, 2):
                                    col = (4 * qt + 2 * i2 - kt) + 3
                                    cs = max(c0, i2 * 256)
                                    fns.append(lambda e, j=j, i2=i2, cs=cs, col=col, sbk=sbk, eb=eb, h=h: e.activation(
                                        out=eb[:, j, cs:(i2 + 1) * 256], in_=PS[sbk[j]][:, cs:(i2 + 1) * 256],
                                        func=AF.Exp, bias=btab[:, h, col:col + 1], scale=1.0))
                            S.emit('act', fns, reads=[('btab', h)], writes=[ek, ('ps', sbk[j])], partial=(j > 0))
                        if kt >= 4 * qt:
                            S.emit('dve', [lambda e, j=j, i0=i0, eb=eb: e.tensor_tensor(
                                out=eb[:, j, i0 * 128:(i0 + 1) * 128], in0=eb[:, j, i0 * 128:(i0 + 1) * 128], in1=tri[:],
                                op=ALU.mult) for j in range(2)],
                                reads=['tri', ek], writes=[ek])
                        fns = []
                        banks = set()
                        for i in range(i0, 4):
                            for j in range(2):
                                idx = i * 2 + j
                                bO = 4 + idx // 3
                                cc = (idx % 3) * 129
                                stt = bO not in started
                                started[bO] = True
                                banks.add(bO)
                                fns.append(lambda e, i=i, j=j, bO=bO, cc=cc, stt=stt, kt=kt, qt=qt, eb=eb: e.matmul(
                                    PS[bO][:, cc:cc + 129], lhsT=eb[:, j, i * 128:(i + 1) * 128], rhs=dv_ext[:, kt, :],
                                    start=stt, stop=(kt == 4 * qt + i), skip_group_check=True))
                        if pend_av is not None:
                            S.emit('pe', pend_av[0], reads=pend_av[1], writes=pend_av[2])
                        pend_av = (fns, [ek, ('dv', kt), 'dv_ones'], [('ps', b_) for b_ in sorted(banks)])
                    S.emit('pe', pend_av[0], reads=pend_av[1], writes=pend_av[2])
                    pend_av = None
                    for i in range(4):
                        f4 = i
                        regs = []
                        for j in range(2):
                            idx = i * 2 + j
                            regs.append((4 + idx // 3, (idx % 3) * 129))
                        (b1, c1), (b2, c2) = regs
                        S.emit('dve', lambda e, f4=f4, b1=b1, c1=c1: e.reciprocal(out=rz[f4][:, 0:1], in_=PS[b1][:, c1 + 128:c1 + 129]),
                               writes=[('rz', f4), ('ps', b1)])
                        S.emit('dve', lambda e, f4=f4, b2=b2, c2=c2: e.reciprocal(out=rz[f4][:, 1:2], in_=PS[b2][:, c2 + 128:c2 + 129]),
                               reads=[('rz', f4)], writes=[('rz', f4), ('ps', b2)])
                        S.emit('dve', lambda e, f4=f4: e.tensor_tensor(out=rz[f4][:, 1:2], in0=rz[f4][:, 1:2], in1=neglam[:], op=ALU.mult),
                               reads=[('rz', f4), 'neglam'], writes=[('rz', f4)])
                        S.emit('dve', lambda e, f4=f4, b1=b1, c1=c1: e.tensor_scalar(out=t1[f4][:], in0=PS[b1][:, c1:c1 + 128],
                                                                                    scalar1=rz[f4][:, 0:1], scalar2=None, op0=ALU.mult),
                               reads=[('rz', f4)], writes=[('t1', f4), ('ps', b1)])
                        S.emit('dve', lambda e, f4=f4, b2=b2, c2=c2: e.scalar_tensor_tensor(
                            out=o32[f4][:], in0=PS[b2][:, c2:c2 + 128], scalar=rz[f4][:, 1:2], in1=t1[f4][:], op0=ALU.mult, op1=ALU.add),
                            reads=[('rz', f4), ('t1', f4)], writes=[('o32', f4), ('ps', b2)])
                    fq = fin_cnt[0] % 2
                    fin_cnt[0] += 1
                    for i in range(4):
                        S.emit('act', lambda e, i=i, fq=fq: e.activation(out=junk[:], in_=o32[i][:], func=AF.Square,
                                                                       accum_out=ssB4[fq][:, i:i + 1]),
                               reads=[('o32', i)], writes=[('ssB4', fq), 'junkd'], partial=(i > 0))
                    S.emit('dve', lambda e, fq=fq: e.tensor_scalar(out=ssB4[fq][:], in0=ssB4[fq][:], scalar1=1.0 / 128,
                                                                 scalar2=NORM_EPS, op0=ALU.mult, op1=ALU.add),
                           reads=[('ssB4', fq)], writes=[('ssB4', fq)])
                    S.emit('pool', lambda e, fq=fq: e.tensor_tensor(out=ssB4[fq][:], in0=ssB4[fq][:], in1=nhalf[:, 0:4], op=ALU.pow),
                           reads=[('ssB4', fq), 'nhalf'], writes=[('ssB4', fq)])
                    tvq = psb(7)[:, 0:512]
                    for i in range(4):
                        S.emit('dve', lambda e, i=i, fq=fq, h=h: e.scalar_tensor_tensor(
                            out=obb4[fq][:, i * 128:(i + 1) * 128], in0=o32[i][:], scalar=ssB4[fq][:, i:i + 1], in1=ngB[:, h, :],
                            op0=ALU.mult, op1=ALU.mult),
                            reads=[('o32', i), ('ssB4', fq), 'ngB'], writes=[('obb4', fq)], partial=(i > 0))
                    S.emit('pe', [lambda e, i=i, fq=fq: e.transpose(out=tvq[:, i * 128:(i + 1) * 128],
                                                                   in_=obb4[fq][:, i * 128:(i + 1) * 128], identity=ident[:])
                                  for i in range(4)],
                           reads=[('obb4', fq), 'ident'], writes=[('ps', 7)])
                    S.emit('act', lambda e, h=h, qt=qt: e.copy(out=o_bT[:, h, qt * 512:(qt + 1) * 512], in_=tvq),
                           writes=[('o_bT', h, qt), ('ps', 7)])
            S.barrier()
            if dbg:
                S.emit('sp', lambda e: e.dma_start(out=dbg_t['obT'], in_=o_bT[:]), dma='ddbg1')

        if stage >= 2:
            phase_D()

        def layer_norm(tiles, src, src_key, dst, dst_key, gt, g_key, bt, b_key, tag):
            bnst, mv, rstd, ytmp = tiles
            yk = dst_key if ytmp is dst else tag + 'ytmp'
            S.emit('dve', [lambda e, k=k: e.bn_stats(out=bnst[:, k, :], in_=src[:, k * 512:(k + 1) * 512]) for k in range(2)],
                   reads=[src_key], writes=[tag + 'bnst'])
            S.emit('dve', lambda e: e.bn_aggr(out=mv[:], in_=bnst[:].rearrange("p a b -> p (a b)")),
                   reads=[tag + 'bnst'], writes=[tag + 'mv'])
            S.emit('dve', lambda e: e.tensor_scalar(out=rstd[:], in0=mv[:, 1:2], scalar1=LN_EPS, scalar2=None, op0=ALU.add),
                   reads=[tag + 'mv'], writes=[tag + 'rstd'])
            S.emit('pool', lambda e: e.tensor_tensor(out=rstd[:], in0=rstd[:], in1=nhalf[:, 0:1], op=ALU.pow),
                   reads=[tag + 'rstd', 'nhalf'], writes=[tag + 'rstd'])
            S.emit('dve', lambda e: e.tensor_scalar(out=ytmp[:], in0=src[:], scalar1=mv[:, 0:1], scalar2=rstd[:],
                                                    op0=ALU.subtract, op1=ALU.mult),
                   reads=[src_key, tag + 'mv', tag + 'rstd'], writes=[yk])
            S.emit('pool', lambda e: e.tensor_tensor(out=ytmp[:], in0=ytmp[:], in1=gt[:], op=ALU.mult),
                   reads=[yk, g_key], writes=[yk])
            S.emit('pool', lambda e: e.tensor_tensor(out=dst[:], in0=ytmp[:], in1=bt[:], op=ALU.add),
                   reads=[yk, b_key], writes=[dst_key])

        def phase_M():
          with ExitStack() as P:
            def sb(name, shape, dt):
                return P.enter_context(nc.sbuf_tensor(uq(name), list(shape), dt))
            wa = sb("wa", [128, 4, 1024], BF16)
            wb_ = sb("wb", [128, 4, 1024], BF16)
            wo = sb("wo", [128, 8, 1024], BF16)
            rw = sb("rw", [128, 8, NE], F32)
            rbt = sb("rbt", [128, NE], F32)
            g1 = sb("g1", [128, D], F32)
            b1 = sb("b1", [128, D], F32)
            sga = [sb("sga0", [128, 8, 512], BF16)] * 2
            sgb = [sb("sgb0", [128, 8, 512], BF16)] * 2
            mT = [sb("mT0", [128, 8, 512], BF16)] * 2
            tA = [sb("tA%d" % i, [128, 512], F32) for i in range(2)]
            tB = [sb("tB%d" % i, [128, 512], F32) for i in range(2)]
            xres = [sb("xres%d" % i, [128, D], F32) for i in range(2)]
            rr = [sb("rr%d" % i, [128, D], F32) for i in range(2)]
            h1t = [sb("h1t%d" % i, [128, D], F32) for i in range(2)]
            bnst = sb("bnst", [128, 2, 6], F32)
            mv = sb("mv", [128, 2], F32)
            rstd = sb("rstd", [128, 1], F32)
            ytmp = sb("ytmp", [128, D], F32)
            h1T32 = sb("h1T32", [128, 8, 128], F32)
            h1Tb = [sb("h1Tb%d" % i, [128, 8, 128], BF16) for i in range(2)]
            lg = sb("lg", [128, NE], F32)
            top8 = sb("top8", [128, 8], F32)
            msk = sb("msk", [128, NE], F32)
            ex = sb("ex", [128, NE], F32)
            nm = sb("nm", [128, 1], F32)
            zs = sb("zs", [128, 1], F32)

            load_cast(wa[:], w_a.rearrange("(c p) n -> p c n", p=128), (4, 1024), 'wa')
            load_cast(wb_[:], w_b.rearrange("(c p) n -> p c n", p=128), (4, 1024), 'wb')
            load_cast(wo[:, 0:4, :], w_out[0:512, :].rearrange("(c p) n -> p c n", p=128), (4, 1024), ('wo', 0))
            load_cast(wo[:, 4:8, :], w_out[512:1024, :].rearrange("(c p) n -> p c n", p=128), (4, 1024), ('wo', 1))
            S.emit('sp', lambda e: e.dma_start(out=rw[:], in_=router_w.rearrange("(c p) n -> p c n", p=128)), writes=['rw'], dma='dsmall0')
            S.emit('sp', lambda e: e.dma_start(out=rbt[:], in_=bcast_ap(router_b, NE)), writes=['rbt'], dma='dsmall1')
            S.emit('sp', lambda e: e.dma_start(out=g1[:], in_=bcast_ap(ln1_g, D)), writes=['l1g'], dma='dsmall2')
            S.emit('sp', lambda e: e.dma_start(out=b1[:], in_=bcast_ap(ln1_b, D)), writes=['l1b'], dma='dsmall3')
            for tt in range(8):
                i2 = 0
                tsl = slice(tt * 512, (tt + 1) * 512)
                S.emit('sp', lambda e, i2=i2, tsl=tsl: e.dma_start(out=sga[i2][:], in_=sg_d[0][:, :, tsl].rearrange("c p n -> p c n")),
                       writes=[('sga', i2)], dma=('dsga', i2))
                S.emit('sp', lambda e, i2=i2, tsl=tsl: e.dma_start(out=sgb[i2][:], in_=sg_d[1][:, :, tsl].rearrange("c p n -> p c n")),
                       writes=[('sgb', i2)], dma=('dsgb', i2))
                for fc in range(8):
                    k2 = fc % 2
                    fsl = slice(fc * 128, (fc + 1) * 128)
                    S.emit('pe', [lambda e, kc=kc, fsl=fsl, tsl=tsl: e.matmul(PS[0][:], lhsT=wa[:, kc, fsl], rhs=o_aT[:, kc, tsl],
                                                                             start=(kc == 0), stop=(kc == 3)) for kc in range(4)],
                           reads=['wa'], writes=[('ps', 0)])
                    S.emit('pe', [lambda e, kc=kc, fsl=fsl, tsl=tsl: e.matmul(PS[1][:], lhsT=wb_[:, kc, fsl], rhs=o_bT[:, kc, tsl],
                                                                             start=(kc == 0), stop=(kc == 3)) for kc in range(4)],
                           reads=['wb'], writes=[('ps', 1)])
                    S.emit('dve', lambda e, k2=k2, i2=i2, fc=fc: e.tensor_tensor(out=tA[k2][:], in0=PS[0][:], in1=sga[i2][:, fc, :], op=ALU.mult),
                           reads=[('sga', i2)], writes=[('tA', k2), ('ps', 0)])
                    S.emit('dve', lambda e, k2=k2, i2=i2, fc=fc: e.tensor_tensor(out=tB[k2][:], in0=PS[1][:], in1=sgb[i2][:, fc, :], op=ALU.mult),
                           reads=[('sgb', i2)], writes=[('tB', k2), ('ps', 1)])
                    S.emit('pool', lambda e, k2=k2, i2=i2, fc=fc: e.tensor_tensor(out=mT[i2][:, fc, :], in0=tA[k2][:], in1=tB[k2][:], op=ALU.add),
                           reads=[('tA', k2), ('tB', k2)], writes=[('mT', i2)], partial=(fc > 0))
                for s4 in range(4):
                    p = tt * 4 + s4
                    j = p % 2
                    S.emit('sp', lambda e, p=p, j=j: e.dma_start(out=xres[j][:], in_=x[p * 128:(p + 1) * 128, :]),
                           writes=[('xres', j)], dma=('dxres', j))
                    for hf in range(2):
                        S.emit('pe', [lambda e, kc=kc, i2=i2, s4=s4, hf=hf: e.matmul(
                            PS[2 + hf][:], lhsT=mT[i2][:, kc, s4 * 128:(s4 + 1) * 128], rhs=wo[:, kc, hf * 512:(hf + 1) * 512],
                            start=(kc == 0), stop=(kc == 7)) for kc in range(8)],
                            reads=[('mT', i2), ('wo', 0), ('wo', 1)], writes=[('ps', 2 + hf)])
                        S.emit('dve', lambda e, j=j, hf=hf: e.scalar_tensor_tensor(
                            out=rr[j][:, hf * 512:(hf + 1) * 512], in0=xres[j][:, hf * 512:(hf + 1) * 512], scalar=float(DN_ALPHA),
                            in1=PS[2 + hf][:], op0=ALU.mult, op1=ALU.add),
                            reads=[('xres', j)], writes=[('rr', j), ('ps', 2 + hf)], partial=(hf > 0))
                    layer_norm((bnst, mv, rstd, ytmp), rr[j], ('rr', j), h1t[j], ('h1t', j), g1, 'l1g', b1, 'l1b', 'm_')
                    S.emit('sp', lambda e, p=p, j=j: e.dma_start(out=h1_d[p * 128:(p + 1) * 128, :], in_=h1t[j][:]),
                           reads=[('h1t', j)], dma=('dh1', j))
                    for g in range(2):
                        S.emit('pe', [lambda e, c=c, j=j, g=g: e.transpose(out=PS[4 + g][:, (c % 4) * 128:(c % 4 + 1) * 128],
                                                                          in_=h1t[j][:, c * 128:(c + 1) * 128], identity=identf[:])
                                      for c in range(4 * g, 4 * g + 4)],
                               reads=[('h1t', j), 'identf'], writes=[('ps', 4 + g)])
                        S.emit('act', lambda e, j=j, g=g: e.copy(out=h1Tb[j][:, 4 * g:4 * g + 4, :],
                                                                 in_=PS[4 + g][:].rearrange("p (c n) -> p c n", n=128)),
                               writes=[('h1Tb', j), ('ps', 4 + g)], partial=(g > 0))
                        S.emit('dve', lambda e, g=g: e.tensor_copy(out=h1T32[:, 4 * g:4 * g + 4, :],
                                                                   in_=PS[4 + g][:].rearrange("p (c n) -> p c n", n=128)),
                               writes=['h1T32', ('ps', 4 + g)], partial=(g > 0))
                    S.emit('sp', lambda e, p=p, j=j: e.dma_start(
                        out=h1T_d[:, :, p * 128:(p + 1) * 128].rearrange("c p n -> p c n"), in_=h1Tb[j][:]),
                        reads=[('h1Tb', j)], dma=('dh1T', j))
                    S.emit('pe', [lambda e, c=c: e.matmul(PS[6][:, 0:NE], lhsT=h1T32[:, c, :], rhs=rw[:, c, :],
                                                          start=(c == 0), stop=(c == 7)) for c in range(8)],
                           reads=['h1T32', 'rw'], writes=[('ps', 6)])
                    S.emit('dve', lambda e: e.tensor_tensor(out=lg[:], in0=PS[6][:, 0:NE], in1=rbt[:], op=ALU.add),
                           reads=['rbt'], writes=['lg', ('ps', 6)])
                    S.emit('dve', lambda e: e.max(out=top8[:], in_=lg[:]), reads=['lg'], writes=['top8'])
                    S.emit('dve', lambda e: e.tensor_scalar(out=msk[:], in0=lg[:], scalar1=top8[:, 3:4], scalar2=None, op0=ALU.is_ge),
                           reads=['lg', 'top8'], writes=['msk'])
                    S.emit('dve', lambda e: e.tensor_scalar(out=nm[:], in0=top8[:, 0:1], scalar1=-1.0, scalar2=None, op0=ALU.mult),
                           reads=['top8'], writes=['nm'])
                    S.emit('act', lambda e: e.activation(out=ex[:], in_=lg[:], func=AF.Exp, bias=nm[:], scale=1.0),
                           reads=['lg', 'nm'], writes=['ex'])
                    S.emit('dve', lambda e: e.tensor_tensor(out=ex[:], in0=ex[:], in1=msk[:], op=ALU.mult),
                           reads=['ex', 'msk'], writes=['ex'])
                    S.emit('dve', lambda e: e.tensor_reduce(out=zs[:], in_=ex[:], axis=mybir.AxisListType.X, op=ALU.add),
                           reads=['ex'], writes=['zs'])
                    S.emit('dve', lambda e: e.reciprocal(out=zs[:], in_=zs[:]), reads=['zs'], writes=['zs'])
                    S.emit('dve', lambda e, p=p: e.tensor_scalar(out=comb[:, p, :], in0=ex[:], scalar1=zs[:], scalar2=None, op0=ALU.mult),
                           reads=['ex', 'zs'], writes=[('comb', p)])
            S.barrier()
            if dbg:
                S.emit('sp', lambda e: e.dma_start(out=dbg_t['h1'], in_=h1_d), dma='ddbg2')
                S.emit('sp', lambda e: e.dma_start(out=dbg_t['comb'], in_=comb[:]), dma='ddbg3')
                S.barrier()

        if stage >= 3:
            phase_M()
        MIX.close()

        def phase_E():
          with ExitStack() as P:
            def sb(name, shape, dt):
                return P.enter_context(nc.sbuf_tensor(uq(name), list(shape), dt))
            h1T = sb("h1T", [128, 8, PASS_TOK], BF16)
            acc = sb("acc", [128, 8, D], F32)
            wguh = [sb("wguh%d" % i, [128, 8, 2, 512], BF16) for i in range(3)]
            wdh = [sb("wdh%d" % i, [128, 4, D], BF16) for i in range(3)]
            est = [sb("est%d" % i, [128, 2048], F32) for i in range(3)]
            bias_all = sb("bias_all", [128, 2, 8, NE], F32)
            bd = sb("bd", [NE, D], F32)
            combT = [sb("combT%d" % i, [NE, 128], F32) for i in range(2)]
            g_t = [sb("g_t%d" % i, [128, 512], F32) for i in range(2)]
            s_t = [sb("s_t%d" % i, [128, 512], F32) for i in range(2)]
            u_t = [sb("u_t%d" % i, [128, 512], F32) for i in range(2)]
            actT = [sb("actT%d" % i, [128, 4, 512], BF16) for i in range(2)]
            comb_s = sb("comb_s", [128, NT, NE], F32)
            h1r = sb("h1r", [128, D], F32)
            r2 = sb("r2", [128, D], F32)
            yout = sb("yout", [128, D], F32)
            g2 = sb("g2", [128, D], F32)
            b2 = sb("b2", [128, D], F32)
            bnst = sb("bnst2", [128, 2, 6], F32)
            mv = sb("mv2", [128, 2], F32)
            rstd = sb("rstd2", [128, 1], F32)
            bgu32 = est[2][0:NE, :]

            S.emit('sp', lambda e: e.dma_start(out=bgu32, in_=b_gu), writes=[('est', 2)], dma='dsmall0')
            S.emit('sp', lambda e: e.dma_start(out=bd[:], in_=b_dn), writes=['bd'], dma='dsmall1')
            S.emit('sp', lambda e: e.dma_start(out=g2[:], in_=bcast_ap(ln2_g, D)), writes=['l2g'], dma='dsmall2')
            S.emit('sp', lambda e: e.dma_start(out=b2[:], in_=bcast_ap(ln2_b, D)), writes=['l2b'], dma='dsmall3')
            S.emit('pe', [lambda e, g=g, fc=fc: e.transpose(
                out=PS[7][:, (g * 8 + fc) * NE:(g * 8 + fc + 1) * NE],
                in_=est[2][0:NE, fc * 256 + g:(fc + 1) * 256:2], identity=identf[0:NE, 0:NE])
                for g in range(2) for fc in range(8)],
                reads=[('est', 2), 'identf'], writes=[('ps', 7)])
            S.emit('dve', lambda e: e.tensor_copy(out=bias_all[:].rearrange("p a b c -> p (a b c)"), in_=PS[7][:]),
                   writes=['bias_all', ('ps', 7)])
            S.emit('dve', lambda e: e.tensor_scalar(out=bias_all[:, 1, :, :], in0=bias_all[:, 1, :, :], scalar1=1.0, scalar2=None,
                                                    op0=ALU.add), reads=['bias_all'], writes=['bias_all'])

            S.emit('dve', lambda e: e.tensor_scalar(out=comb_s[:], in0=comb[:], scalar1=float(1.0 / 1.702), scalar2=None, op0=ALU.mult),
                   writes=['comb_s'])
            jobs = []
            ptr = {'d': 0, 'c': 0}

            def make_jobs(e_, hfu, slot):
                for q in range(4):
                    jobs.append(('gu', e_, hfu, slot, q))
                for q in range(2):
                    jobs.append(('dn', e_, hfu, slot, q))

            def emit_dma(k):
                kind, e_, hfu, slot, q = jobs[k]
                i = k % 3
                if kind == 'gu':
                    src = w_gu[e_, q * 256:(q + 1) * 256, hfu * 1024:(hfu + 1) * 1024].rearrange("(c p) n -> p c n", p=128)
                else:
                    src = w_dn[e_, hfu * 512 + q * 256:hfu * 512 + (q + 1) * 256, :].rearrange("(c p) n -> p c n", p=128)
                S.emit('sp', lambda e, i=i, src=src: e.dma_start(out=est[i][:].rearrange("p (c n) -> p c n", n=1024), in_=src),
                       writes=[('est', i)], dma=('dest', i))

            def emit_cast(k):
                kind, e_, hfu, slot, q = jobs[k]
                i = k % 3
                if kind == 'gu':
                    for c in range(2):
                        kc = q * 2 + c
                        src = est[i][:, c * 1024:(c + 1) * 1024].rearrange("p (f g) -> p g f", g=2)
                        S.emit('act', lambda e, kc=kc, src=src, slot=slot: e.copy(out=wguh[slot][:, kc, :, :], in_=src),
                               reads=[('est', i)], writes=[('wguh', slot)], partial=not (q == 0 and c == 0))
                else:
                    src = est[i][:].rearrange("p (c n) -> p c n", n=1024)
                    S.emit('act', lambda e, q=q, src=src, slot=slot: e.copy(out=wdh[slot][:, 2 * q:2 * q + 2, :], in_=src),
                           reads=[('est', i)], writes=[('wdh', slot)], partial=(q > 0))

            def pump(n):
                for _ in range(n):
                    if ptr['c'] < len(jobs) and ptr['c'] < ptr['d']:
                        emit_cast(ptr['c'])
                        ptr['c'] += 1
                    while ptr['d'] < min(len(jobs), ptr['c'] + 3):
                        emit_dma(ptr['d'])
                        ptr['d'] += 1

            gcnt = [0]
            ycnt = [0]

            def emit_gu(ps_, e_, hfu, slot, tile, ai):
                tl = slice(tile * 512, (tile + 1) * 512)

                def finish(fcl, k):
                    S.emit('dve', lambda e, k=k, fcl=fcl: e.scalar_tensor_tensor(
                        out=actT[ai][:, fcl, :], in0=u_t[k][:], scalar=-6.0, in1=s_t[k][:], op0=ALU.max, op1=ALU.mult),
                        reads=[('u_t', k), ('s_t', k)], writes=[('actT', ai)], partial=(fcl > 0))
                pend = None
                for fcl in range(4):
                    fc = hfu * 4 + fcl
                    k = gcnt[0] % 2
                    gcnt[0] += 1
                    bg, bu = 2 * k, 2 * k + 1
                    fs = slice(fcl * 128, (fcl + 1) * 128)
                    S.emit('pe', [lambda e, kc=kc, fs=fs, tl=tl, bg=bg: e.matmul(
                        PS[bg][:], lhsT=wguh[slot][:, kc, 0, fs], rhs=h1T[:, kc, tl], start=(kc == 0), stop=(kc == 7))
                        for kc in range(8)], reads=[('wguh', slot), 'h1T'], writes=[('ps', bg)])
                    S.emit('pe', [lambda e, kc=kc, fs=fs, tl=tl, bu=bu: e.matmul(
                        PS[bu][:], lhsT=wguh[slot][:, kc, 1, fs], rhs=h1T[:, kc, tl], start=(kc == 0), stop=(kc == 7))
                        for kc in range(8)], reads=[('wguh', slot), 'h1T'], writes=[('ps', bu)])
                    S.emit('dve', lambda e, k=k, bg=bg, fc=fc: e.tensor_scalar(
                        out=g_t[k][:], in0=PS[bg][:], scalar1=bias_all[:, 0, fc, e_:e_ + 1], scalar2=7.0, op0=ALU.add, op1=ALU.min),
                        reads=['bias_all'], writes=[('g_t', k), ('ps', bg)])
                    S.emit('act', lambda e, k=k: e.activation(out=s_t[k][:], in_=g_t[k][:], func=AF.Silu, scale=1.702),
                           reads=[('g_t', k)], writes=[('s_t', k)])
                    S.emit('dve', lambda e, k=k, bu=bu, fc=fc: e.tensor_scalar(
                        out=u_t[k][:], in0=PS[bu][:], scalar1=bias_all[:, 1, fc, e_:e_ + 1], scalar2=8.0, op0=ALU.add, op1=ALU.min),
                        reads=['bias_all'], writes=[('u_t', k), ('ps', bu)])
                    if pend is not None:
                        finish(*pend)
                    pend = (fcl, k)
                    if tile == 0 or fcl < 2:
                        pump(1)
                finish(*pend)

            def emit_down(ps_, e_, hfu, slot, tile, ai):
                for s4 in range(4):
                    st = tile * 4 + s4
                    p = ps_ * 8 + st
                    for d2 in range(2):
                        by = 4 + (ycnt[0] % 2)
                        ycnt[0] += 1
                        ds = slice(d2 * 512, (d2 + 1) * 512)
                        S.emit('pe', [lambda e, fcl=fcl, s4=s4, ds=ds, by=by: e.matmul(
                            PS[by][:], lhsT=actT[ai][:, fcl, s4 * 128:(s4 + 1) * 128], rhs=wdh[slot][:, fcl, ds],
                            start=(fcl == 0), stop=(fcl == 3)) for fcl in range(4)],
                            reads=[('actT', ai), ('wdh', slot)], writes=[('ps', by)])
                        S.emit('dve', lambda e, st=st, p=p, ds=ds, by=by: e.scalar_tensor_tensor(
                            out=acc[:, st, ds], in0=PS[by][:], scalar=comb_s[:, p, e_:e_ + 1], in1=acc[:, st, ds],
                            op0=ALU.mult, op1=ALU.add),
                            reads=[('acc', st, d2)], writes=[('acc', st, d2), ('ps', by)])

            def acc_init(ps_):
                for st in range(8):
                    p = ps_ * 8 + st
                    ci = st % 2
                    S.emit('pe', lambda e, p=p: e.transpose(out=PS[7][0:NE, 0:128], in_=comb[:, p, :], identity=identf[:]),
                           reads=['identf'], writes=[('ps', 7)])
                    S.emit('dve', lambda e, ci=ci: e.tensor_copy(out=combT[ci][:], in_=PS[7][0:NE, 0:128]),
                           writes=[('combT', ci), ('ps', 7)])
                    for d2 in range(2):
                        ds = slice(d2 * 512, (d2 + 1) * 512)
                        S.emit('pe', lambda e, ci=ci, ds=ds: e.matmul(PS[6][:], lhsT=combT[ci][:], rhs=bd[:, ds], start=True, stop=True),
                               reads=[('combT', ci), 'bd'], writes=[('ps', 6)])
                        S.emit('act', lambda e, st=st, ds=ds: e.copy(out=acc[:, st, ds], in_=PS[6][:]),
                               writes=[('acc', st, d2), ('ps', 6)])

            units = [(ps_, e_, hfu) for ps_ in range(NPASS) for e_ in range(NE) for hfu in range(2)]
            if stage == 4 and ne_decl < NE:
                units = [(ps_, e_, hfu) for ps_ in range(NPASS) for e_ in range(ne_decl) for hfu in range(2)]
            nun = len(units)
            per_pass = nun // NPASS
            for u0 in range(min(2, nun)):
                make_jobs(units[u0][1], units[u0][2], u0 % 3)
            pump(len(jobs) + 1)
            prev = None
            tcount = 0
            need_acc_init = None
            for u, (ps_, e_, hfu) in enumerate(units):
                slot = u % 3
                if u % per_pass == 0:
                    tok0 = ps_ * PASS_TOK
                    S.emit('sp', lambda e, tok0=tok0: e.dma_start(
                        out=h1T[:], in_=h1T_d[:, :, tok0:tok0 + PASS_TOK].rearrange("c p n -> p c n")),
                        writes=['h1T'], dma='dh1Tp')
                    need_acc_init = ps_
                if u + 2 < nun:
                    make_jobs(units[u + 2][1], units[u + 2][2], (u + 2) % 3)
                for tile in range(2):
                    ai = tcount % 2
                    tcount += 1
                    emit_gu(ps_, e_, hfu, slot, tile, ai)
                    if need_acc_init is not None:
                        acc_init(need_acc_init)
                        need_acc_init = None
                    if prev is not None:
                        emit_down(*prev)
                    prev = (ps_, e_, hfu, slot, tile, ai)
                if (u + 1) % per_pass == 0:
                    emit_down(*prev)
                    prev = None
                    for st in range(8):
                        p = ps_ * 8 + st
                        S.emit('sp', lambda e, p=p: e.dma_start(out=h1r[:], in_=h1_d[p * 128:(p + 1) * 128, :]),
                               writes=['h1r'], dma='dh1r')
                        S.emit('dve', lambda e, st=st: e.scalar_tensor_tensor(
                            out=r2[:], in0=h1r[:], scalar=float(DN_ALPHA), in1=acc[:, st, :], op0=ALU.mult, op1=ALU.add),
                            reads=['h1r', ('acc', st, 0), ('acc', st, 1)], writes=['r2'])
                        layer_norm((bnst, mv, rstd, yout), r2, 'r2', yout, 'yout', g2, 'l2g', b2, 'l2b', 'f_')
                        S.emit('sp', lambda e, p=p: e.dma_start(out=y[p * 128:(p + 1) * 128, :], in_=yout[:]),
                               reads=['yout'], writes=['yout_dma'], dma='dyout')
            S.barrier()

        if stage >= 4:
            phase_E()

        S.barrier()
        S.run()
    return nc


_NC_CACHE = {}


def _get_nc():
    if 'nc' not in _NC_CACHE:
        _NC_CACHE['nc'] = build()
    return _NC_CACHE['nc']


def kernel(**inputs):
    nc = _get_nc()
    n = 8
    names = ["w_in", "hg_lb_logits", "hg_norm_g", "da_lambda", "da_norm_g", "w_branch_a", "w_branch_b", "w_out",
             "ln1_g", "ln1_b", "router_w", "router_b", "w_gate_up", "b_gate_up", "w_down", "b_down", "ln2_g", "ln2_b"]
    shared = {}
    for k in names:
        a = np.ascontiguousarray(np.asarray(inputs[k], dtype=np.float32))
        shared[k] = a[0] if k != "hg_lb_logits" else a
    xx = np.asarray(inputs["x"], dtype=np.float32)
    in_maps = []
    for c in range(n):
        m = dict(shared)
        m["x"] = np.ascontiguousarray(xx[c])
        in_maps.append(m)
    res = run_bass_kernel_spmd(nc, in_maps, core_ids=list(range(n)))
    return np.stack([np.asarray(r["y"]) for r in res.results], axis=0).astype(np.float32)
```
